# Optimizing a Trainium2 kernel written in Bass

```python
import math
import jax
import jax.numpy as jnp
from jax import lax
import numpy as np

D_MODEL = 2048
BATCH = 4
SEQ = 4096
DEPTH = 1

SSD_WIDTH = D_MODEL
SSD_HEADDIM = 64
SSD_HEADS = SSD_WIDTH // SSD_HEADDIM
SSD_GROUPS = 4
SSD_STATE = 128
SSD_CONV_K = 3
SSD_CHUNK = 128
SC_WIDTH = D_MODEL
SC_GROUPS = 32
SC_CONV_K = 3
MIX_WIDTH = SSD_WIDTH + SC_WIDTH
XBC_WIDTH = SSD_WIDTH + 2 * SSD_GROUPS * SSD_STATE
IN_COLS = SSD_WIDTH + XBC_WIDTH + 2 * SSD_HEADS + 3 * SC_WIDTH

PEER_HEADS = 8
N_KEYS = 128
N_EXPERTS = N_KEYS * N_KEYS
PEER_TOPK = 16
D_QUERY = 512
PEER_TOKEN_BLOCK = 128

DEEPNORM_ALPHA = (2.0 * DEPTH) ** 0.25
DEEPNORM_BETA = (8.0 * DEPTH) ** -0.25
NORM_EPS = 1e-5
DT_MIN = 0.001
DT_MAX = 0.1
A_MIN = 1.0
A_MAX = 16.0

kernel_name = 'hymba_bissd_shortconv_peer_deepnorm_adaln'


def layer_norm(x, g, b):
    xf = x.astype(jnp.float32)
    mu = jnp.mean(xf, axis=-1, keepdims=True)
    xc = xf - mu
    var = jnp.mean(xc * xc, axis=-1, keepdims=True)
    return (xc * lax.rsqrt(var + NORM_EPS) * g + b).astype(x.dtype)


def group_rms_norm(y, g, n_groups):
    yf = y.astype(jnp.float32).reshape(y.shape[:-1] + (n_groups, y.shape[-1] // n_groups))
    yf = yf * lax.rsqrt(jnp.mean(yf * yf, axis=-1, keepdims=True) + NORM_EPS)
    return (yf.reshape(y.shape) * g).astype(y.dtype)


def centred_depthwise_conv(u, w):
    k = w.shape[0]
    pad = k // 2
    s = u.shape[1]
    up = jnp.pad(u, ((0, 0), (pad, pad), (0, 0)))
    out = up[:, 0:s] * w[0]
    for j in range(1, k):
        out = out + up[:, j:j + s] * w[j]
    return out


def ssd_chunked(x, dt, a, bm, cm):
    b, l, h, p = x.shape
    g, n = bm.shape[-2:]
    r = h // g
    cs = SSD_CHUNK
    nc = l // cs
    xd = (x * dt[..., None]).reshape(b, nc, cs, g, r, p)
    adt = (dt * a).reshape(b, nc, cs, g, r)
    acs = jnp.moveaxis(jnp.cumsum(adt, axis=2), 2, -1)
    bc = bm.reshape(b, nc, cs, g, n)
    cc = cm.reshape(b, nc, cs, g, n)
    mask = jnp.tril(jnp.ones((cs, cs), dtype=bool))
    seg = acs[..., :, None] - acs[..., None, :]
    lmat = jnp.exp(jnp.where(mask, seg, -jnp.inf))
    cb = jnp.einsum('bclgn,bcsgn->bcgls', cc, bc)
    y_diag = jnp.einsum('bcgls,bcgrls,bcsgrp->bclgrp', cb, lmat, xd)
    decay_states = jnp.exp(acs[..., -1:] - acs)
    states = jnp.einsum('bclgn,bcgrl,bclgrp->bcgrpn', bc, decay_states, xd)
    chunk_decay = jnp.exp(acs[..., -1])

    def step(h_prev, inp):
        dec, st = inp
        return dec[..., None, None] * h_prev + st, h_prev

    h0 = jnp.zeros((b, g, r, p, n), dtype=states.dtype)
    _, h_in = lax.scan(step, h0, (jnp.moveaxis(chunk_decay, 1, 0), jnp.moveaxis(states, 1, 0)))
    h_in = jnp.moveaxis(h_in, 0, 1)
    y_off = jnp.einsum('bclgn,bcgrpn,bcgrl->bclgrp', cc, h_in, jnp.exp(acs))
    return (y_diag + y_off).reshape(b, l, h, p)


def ssd_bidirectional(xs, bm, cm, dt_f, dt_b, a_f, a_b, d_skip):
    y_f = ssd_chunked(xs, dt_f, a_f, bm, cm)
    y_b = jnp.flip(ssd_chunked(jnp.flip(xs, 1), jnp.flip(dt_b, 1), a_b, jnp.flip(bm, 1), jnp.flip(cm, 1)), 1)
    return y_f + y_b + xs * d_skip[:, None]


def hybrid_mixer(h, w_in, conv_ssd_w, conv_ssd_b, dt_bias_f, dt_bias_b, a_log_f, a_log_b,
                 d_skip, ssd_norm_g, short_conv_w, sc_norm_g, w_out):
    b, s, _ = h.shape
    proj = h @ w_in
    o1 = SSD_WIDTH
    o2 = o1 + XBC_WIDTH
    o3 = o2 + 2 * SSD_HEADS
    o4 = o3 + SC_WIDTH
    o5 = o4 + SC_WIDTH
    z, xbc, dt_raw, g_b, g_c, v = jnp.split(proj, [o1, o2, o3, o4, o5], axis=-1)
    xbc = jax.nn.silu(centred_depthwise_conv(xbc, conv_ssd_w) + conv_ssd_b)
    gn = SSD_GROUPS * SSD_STATE
    xs, bm, cm = jnp.split(xbc, [SSD_WIDTH, SSD_WIDTH + gn], axis=-1)
    xs = xs.reshape(b, s, SSD_HEADS, SSD_HEADDIM)
    bm = bm.reshape(b, s, SSD_GROUPS, SSD_STATE)
    cm = cm.reshape(b, s, SSD_GROUPS, SSD_STATE)
    dt_raw = dt_raw.astype(jnp.float32)
    dt_f = jax.nn.softplus(dt_raw[..., :SSD_HEADS] + dt_bias_f)
    dt_b = jax.nn.softplus(dt_raw[..., SSD_HEADS:] + dt_bias_b)
    a_f = -jnp.exp(a_log_f.astype(jnp.float32))
    a_b = -jnp.exp(a_log_b.astype(jnp.float32))
    y_ssd = ssd_bidirectional(xs, bm, cm, dt_f, dt_b, a_f, a_b, d_skip).reshape(b, s, SSD_WIDTH)
    y_ssd = group_rms_norm(y_ssd * jax.nn.silu(z), ssd_norm_g, SSD_GROUPS)
    y_sc = g_b * centred_depthwise_conv(g_c * v, short_conv_w)
    y_sc = group_rms_norm(y_sc, sc_norm_g, SC_GROUPS)
    return jnp.concatenate([y_ssd, y_sc], axis=-1) @ w_out


def peer(h, w_query, sub_keys, expert_u, expert_v):
    b, s, d = h.shape
    t = b * s
    half = D_QUERY // 2
    q = (h @ w_query).reshape(b, s, PEER_HEADS, 2, half)
    s1 = jnp.einsum('bshd,hkd->bshk', q[..., 0, :], sub_keys[:, 0])
    s2 = jnp.einsum('bshd,hkd->bshk', q[..., 1, :], sub_keys[:, 1])
    v1, i1 = lax.top_k(s1, PEER_TOPK)
    v2, i2 = lax.top_k(s2, PEER_TOPK)
    cand = (v1[..., :, None] + v2[..., None, :]).reshape(b, s, PEER_HEADS, PEER_TOPK * PEER_TOPK)
    score, flat = lax.top_k(cand, PEER_TOPK)
    e1 = jnp.take_along_axis(i1, flat // PEER_TOPK, axis=-1)
    e2 = jnp.take_along_axis(i2, flat % PEER_TOPK, axis=-1)
    expert_idx = e1 * N_KEYS + e2
    gate = jax.nn.softmax(score.astype(jnp.float32), axis=-1)
    nb = t // PEER_TOKEN_BLOCK
    hk = PEER_HEADS * PEER_TOPK
    idx_blocks = expert_idx.reshape(nb, PEER_TOKEN_BLOCK, hk)
    gate_blocks = gate.reshape(nb, PEER_TOKEN_BLOCK, hk)
    x_blocks = h.reshape(nb, PEER_TOKEN_BLOCK, d)

    def block(args):
        xb, ib, gb = args
        u_sel = jnp.take(expert_u, ib, axis=0)
        act = jax.nn.gelu(jnp.einsum('tkd,td->tk', u_sel, xb), approximate=False)
        v_sel = jnp.take(expert_v, ib, axis=0)
        return jnp.einsum('tk,tkd->td', act * gb, v_sel)

    out = lax.map(block, (x_blocks, idx_blocks, gate_blocks))
    return out.reshape(b, s, d)


def setup_inputs(seed: int = 0) -> dict:
    key = jax.random.key(seed)
    ks = jax.random.split(key, 24)
    f32 = jnp.float32
    L = DEPTH

    def nrm(k, shape, std):
        return jax.random.normal(k, shape, f32) * std

    def dt_bias(k):
        dt = jnp.exp(jax.random.uniform(k, (L, SSD_HEADS), f32, math.log(DT_MIN), math.log(DT_MAX)))
        return dt + jnp.log(-jnp.expm1(-dt))

    return {
        'x': nrm(ks[0], (BATCH, SEQ, D_MODEL), 1.0),
        'c': nrm(ks[1], (BATCH, D_MODEL), 1.0),
        'w_ada': nrm(ks[2], (L, D_MODEL, 6 * D_MODEL), D_MODEL ** -0.5),
        'b_ada': nrm(ks[3], (L, 6 * D_MODEL), 0.01),
        'w_in': nrm(ks[4], (L, D_MODEL, IN_COLS), D_MODEL ** -0.5),
        'conv_ssd_w': nrm(ks[5], (L, SSD_CONV_K, XBC_WIDTH), SSD_CONV_K ** -0.5),
        'conv_ssd_b': nrm(ks[6], (L, XBC_WIDTH), 0.01),
        'dt_bias_f': dt_bias(ks[7]),
        'dt_bias_b': dt_bias(ks[8]),
        'a_log_f': jnp.log(jax.random.uniform(ks[9], (L, SSD_HEADS), f32, A_MIN, A_MAX)),
        'a_log_b': jnp.log(jax.random.uniform(ks[10], (L, SSD_HEADS), f32, A_MIN, A_MAX)),
        'd_skip': 1.0 + nrm(ks[11], (L, SSD_HEADS), 0.02),
        'ssd_norm_g': 1.0 + nrm(ks[12], (L, SSD_WIDTH), 0.02),
        'short_conv_w': nrm(ks[13], (L, SC_CONV_K, SC_WIDTH), SC_CONV_K ** -0.5),
        'sc_norm_g': 1.0 + nrm(ks[14], (L, SC_WIDTH), 0.02),
        'w_out': nrm(ks[15], (L, MIX_WIDTH, D_MODEL), DEEPNORM_BETA * MIX_WIDTH ** -0.5),
        'ln1_g': 1.0 + nrm(ks[16], (L, D_MODEL), 0.02),
        'ln1_b': nrm(ks[17], (L, D_MODEL), 0.01),
        'w_query': nrm(ks[18], (L, D_MODEL, PEER_HEADS * D_QUERY), D_MODEL ** -0.5),
        'sub_keys': nrm(ks[19], (L, PEER_HEADS, 2, N_KEYS, D_QUERY // 2), (D_QUERY // 2) ** -0.5),
        'expert_u': nrm(ks[20], (L, N_EXPERTS, D_MODEL), D_MODEL ** -0.5),
        'expert_v': nrm(ks[21], (L, N_EXPERTS, D_MODEL), DEEPNORM_BETA),
        'ln2_g': 1.0 + nrm(ks[22], (L, D_MODEL), 0.02),
        'ln2_b': nrm(ks[23], (L, D_MODEL), 0.01),
    }


def reference(x, c, w_ada, b_ada, w_in, conv_ssd_w, conv_ssd_b, dt_bias_f, dt_bias_b,
              a_log_f, a_log_b, d_skip, ssd_norm_g, short_conv_w, sc_norm_g, w_out,
              ln1_g, ln1_b, w_query, sub_keys, expert_u, expert_v, ln2_g, ln2_b):
    for i in range(DEPTH):
        mod = jax.nn.silu(c) @ w_ada[i] + b_ada[i]
        sh1, sc1, g1, sh2, sc2, g2 = jnp.split(mod[:, None, :], 6, axis=-1)
        h = x * (1.0 + sc1) + sh1
        mix = hybrid_mixer(h, w_in[i], conv_ssd_w[i], conv_ssd_b[i], dt_bias_f[i], dt_bias_b[i],
                           a_log_f[i], a_log_b[i], d_skip[i], ssd_norm_g[i], short_conv_w[i],
                           sc_norm_g[i], w_out[i])
        x = layer_norm(DEEPNORM_ALPHA * x + g1 * mix, ln1_g[i], ln1_b[i])
        h = x * (1.0 + sc2) + sh2
        ffn = peer(h, w_query[i], sub_keys[i], expert_u[i], expert_v[i])
        x = layer_norm(DEEPNORM_ALPHA * x + g2 * ffn, ln2_g[i], ln2_b[i])
    return x
```

```python
import numpy as np
import concourse.bass as bass
import concourse.mybir as mybir
from concourse.bass_utils import run_bass_kernel_spmd
from contextlib import ExitStack

F32 = mybir.dt.float32
BF16 = mybir.dt.bfloat16
I32 = mybir.dt.int32
U32 = mybir.dt.uint32
AF = mybir.ActivationFunctionType
ALU = mybir.AluOpType
AX = mybir.AxisListType

ENGS = ['pe', 'act', 'dve', 'pool', 'sp']
ALPHA = 2.0 ** 0.25
EPS = 1e-5
NT = 16
NEG = -1.0e30


class Tracker:
    def __init__(self, nc, es):
        self.nc = nc
        self.es = es
        self.stream = {e: [] for e in ENGS}
        self.sems = {}
        self.cnt = {}
        self.seen = {e: {} for e in ENGS}
        self.lastw = {}
        self.readers = {}
        self.chan_keys = {}
        for e in ENGS:
            self._sem('E_' + e)

    def _sem(self, name):
        if name not in self.sems:
            self.sems[name] = self.es.enter_context(self.nc.semaphore(name))
            self.cnt[name] = 0
        return name

    def _deps(self, reads, writes):
        deps = []
        for k in reads:
            if k in self.lastw:
                deps.append(self.lastw[k])
        for k in writes:
            if k in self.lastw:
                deps.append(self.lastw[k])
            deps.extend(self.readers.get(k, {}).items())
        return deps

    def _emit_waits(self, eng, deps, skip_sem=None):
        best = {}
        for (s, v) in deps:
            if s == skip_sem:
                continue
            if v > best.get(s, 0):
                best[s] = v
        for s, v in best.items():
            if self.seen[eng].get(s, 0) < v:
                self.stream[eng].append(('wait', s, v))
                self.seen[eng][s] = v

    def _commit(self, token, reads, writes):
        for k in writes:
            self.lastw[k] = token
            self.readers[k] = {}
        for k in reads:
            d = self.readers.setdefault(k, {})
            if d.get(token[0], 0) < token[1]:
                d[token[0]] = token[1]

    def op(self, eng, fn, reads=(), writes=(), inc=True):
        s = 'E_' + eng
        deps = self._deps(reads, writes)
        self._emit_waits(eng, deps, skip_sem=(s if eng == 'pe' else None))
        if inc:
            self.cnt[s] += 1
            token = (s, self.cnt[s])
        else:
            token = (s, self.cnt[s] + 1)
        self.stream[eng].append(('op', fn, (s, 1) if inc else None))
        self._commit(token, reads, writes)
        return token

    def dma(self, q, fn, chan, reads=(), writes=()):
        s = self._sem('D_' + chan)
        deps = self._deps(reads, writes)
        self._emit_waits(q, deps)
        self.cnt[s] += 16
        token = (s, self.cnt[s])
        self.stream[q].append(('op', fn, (s, 16)))
        self._commit(token, reads, writes)
        self.chan_keys.setdefault(chan, set()).update(writes)
        return token

    def seal(self, chan):
        s = 'D_' + chan
        if s not in self.cnt:
            return
        tok = (s, self.cnt[s])
        for k in self.chan_keys.get(chan, ()):
            if k in self.lastw and self.lastw[k][0] == s:
                self.lastw[k] = tok

    def barrier(self):
        for e in ENGS:
            for s, c in self.cnt.items():
                if c > 0 and self.seen[e].get(s, 0) < c:
                    self.stream[e].append(('wait', s, c))
                    self.seen[e][s] = c

    def getreg(self, eng, val):
        if not hasattr(self, '_regs'):
            self._regs = {}
        if val not in self._regs:
            self._regs[val] = eng.to_reg(val)
        return self._regs[val]

    def emit(self):
        nc = self.nc
        tk = self

        def run(engname):
            def body(eng):
                for it in tk.stream[engname]:
                    if it[0] == 'wait':
                        eng.wait_ge(tk.sems[it[1]], it[2])
                    else:
                        ins = it[1](eng)
                        if it[2] is not None:
                            ins.then_inc(tk.sems[it[2][0]], it[2][1])
            return body

        with nc.Block() as block:
            block.tensor(run('pe'))
            block.scalar(run('act'))
            block.vector(run('dve'))
            block.gpsimd(run('pool'))
            block.sync(run('sp'))


class Arena:
    def __init__(self, ap):
        self.ap = ap
        self.off = 0
        self.N = ap.shape[1]

    def f32(self, n):
        a = self.ap[:, self.off:self.off + n]
        self.off += n
        assert self.off <= self.N, ("arena overflow", self.off, self.N)
        return a

    def bf16(self, n):
        m = (n + 1) // 2
        return self.f32(m).bitcast(BF16)

    def i32(self, n):
        return self.f32(n).bitcast(I32)

    def u32(self, n):
        return self.f32(n).bitcast(U32)


OZ, OXBC, ODT, OGB, OGC, OV = 0, 2048, 5120, 5184, 7232, 9280
NBLK = 58


def blk_xbc(cb, tap):
    return cb * 3 + tap


def blk_z(g):
    return 18 + g


def blk_sc(j, k):
    return 22 + j * 5 + k


def blk_out(nb, hf):
    return 42 + nb * 2 + hf


def blk_q(hb):
    return 50 + hb


def build(stage=99):
    import os
    SUB = int(os.environ.get('K_SUB', '9'))
    DBG = int(os.environ.get('K_DBG', '0'))
    nc = bass.Bass("TRN2", target_bir_lowering=False)

    def din(name, shape, dt=F32):
        return nc.dram_tensor(name, shape, dt, kind="ExternalInput").ap()

    xp = din("xp", [4098, 2048])
    cT = din("cT", [128, 16])
    w_ada = din("w_ada", [2048, 12288])
    b_ada = din("b_ada", [1, 12288])
    w_in = din("w_in", [2048, 11328])
    w_dt = din("w_dt", [2048, 64])
    cwb = din("cwb", [128, 9216])
    scwb = din("scwb", [128, 6144])
    cbias = din("cbias", [1, 3072])
    dtb_b = din("dtb_b", [128, 64])
    alog_b = din("alog_b", [128, 64])
    dsk_b = din("dsk_b", [128, 32])
    ssdgT = din("ssdgT", [128, 16])
    scgT = din("scgT", [128, 16])
    w_out = din("w_out", [4096, 2048])
    ln1g_b = din("ln1g_b", [128, 2048])
    ln1b_b = din("ln1b_b", [128, 2048])
    w_query = din("w_query", [2048, 4096])
    kT = din("kT", [128, 4096])
    eu = din("eu", [16384, 2048])
    ev = din("ev", [16384, 2048])
    ln2g_b = din("ln2g_b", [128, 2048])
    ln2b_b = din("ln2b_b", [128, 2048])
    out = nc.dram_tensor("out", [2048, 2048], F32, kind="ExternalOutput").ap()
    WS = nc.dram_tensor("WS", [NBLK, 128, 16 * 512], BF16, kind="Internal").ap()
    modd = nc.dram_tensor("modd", [1, 12288], F32, kind="Internal").ap()
    HFS = nc.dram_tensor("HFS", [NT, 128, 2048], BF16, kind="Internal").ap()
    X1S = nc.dram_tensor("X1S", [2048, 2048], F32, kind="Internal").ap()
    EUb = nc.dram_tensor("EUb", [16384, 2048], BF16, kind="Internal").ap()
    EVb = nc.dram_tensor("EVb", [16384, 2048], BF16, kind="Internal").ap()

    es = ExitStack()
    with es:
        tk = Tracker(nc, es)
        ARN = es.enter_context(nc.sbuf_tensor("arena", [128, 52900], F32))
        PSM = es.enter_context(nc.psum_tensor("psm", [128, 4096], F32))
        ar = Arena(ARN[:, :])

        def bank(i, n=512):
            return PSM[:, i * 512:i * 512 + n]

        ident = ar.f32(128)
        ones_f = ar.f32(128)
        triF = ar.f32(128)
        triR = ar.f32(128)
        ones_b = ar.bf16(128)
        sc1pT = ar.f32(16)
        sh1T = ar.f32(16)
        dtb = ar.f32(64)
        a_neg = ar.f32(64)
        dsk = ar.f32(32)
        ssdg = ar.f32(16)
        scg = ar.f32(16)
        iota16 = ar.f32(16)
        bias2 = ar.bf16(3072)
        wdt_sb = ar.bf16(16 * 64)
        PERSIST = ar.off

        tk.op('pool', lambda e: e.memset(ident, 0.0), writes=['ident'])
        tk.op('pool', lambda e: e.affine_select(out=ident, in_=ident, pattern=[[-1, 128]], compare_op=ALU.not_equal,
                                                fill=tk.getreg(e, 1.0), base=0, channel_multiplier=1), reads=['ident'], writes=['ident'])
        tk.op('pool', lambda e: e.memset(ones_f, 1.0), writes=['ones_f'])
        tk.op('pool', lambda e: e.memset(ones_b, 1.0), writes=['ones_b'])
        tk.op('pool', lambda e: e.memset(triF, 1.0), writes=['triF'])
        tk.op('pool', lambda e: e.affine_select(out=triF, in_=triF, pattern=[[1, 128]], compare_op=ALU.is_ge,
                                                fill=tk.getreg(e, 0.0), base=0, channel_multiplier=-1), reads=['triF'], writes=['triF'])
        tk.op('pool', lambda e: e.memset(triR, 1.0), writes=['triR'])
        tk.op('pool', lambda e: e.affine_select(out=triR, in_=triR, pattern=[[-1, 128]], compare_op=ALU.is_ge,
                                                fill=tk.getreg(e, 0.0), base=0, channel_multiplier=1), reads=['triR'], writes=['triR'])
        tk.op('pool', lambda e: e.iota(iota16, pattern=[[1, 16]], base=0, channel_multiplier=0,
                                       allow_small_or_imprecise_dtypes=True), writes=['iota16'])
        for (dst, src, nm) in [(dtb, dtb_b, 'dtb'), (dsk, dsk_b, 'dsk'), (ssdg, ssdgT, 'ssdg'), (scg, scgT, 'scg')]:
            tk.dma('sp', lambda e, dst=dst, src=src: e.dma_start(out=dst, in_=src[:, :]), 'par', writes=[nm])
        tk.dma('sp', lambda e: e.dma_start(out=a_neg, in_=alog_b[:, :]), 'par', writes=['a_neg'])
        tk.seal('par')
        tk.op('act', lambda e: e.activation(out=a_neg, in_=a_neg, func=AF.Exp), reads=['a_neg'], writes=['a_neg'])
        tk.op('dve', lambda e: e.tensor_scalar(out=a_neg, in0=a_neg, scalar1=-1.0, scalar2=None, op0=ALU.mult),
              reads=['a_neg'], writes=['a_neg'])

        P0 = ar.off
        cs_ = ar.f32(16)
        bada = ar.f32(12288)
        wa = [ar.f32(4096), ar.f32(4096)]
        stg = ar.f32(4096)
        tk.dma('sp', lambda e: e.dma_start(out=cs_, in_=cT[:, :]), 'p0', writes=['cs_'])
        tk.dma('sp', lambda e: e.dma_start(out=bada[0:1, :], in_=b_ada[:, :]), 'p0', writes=['bada'])
        tk.seal('p0')
        tk.op('act', lambda e: e.activation(out=cs_, in_=cs_, func=AF.Silu), reads=['cs_'], writes=['cs_'])
        it = 0
        for g in range(3):
            for kc in range(16):
                b = it % 2
                it += 1
                tk.dma('sp', lambda e, b=b, kc=kc, g=g: e.dma_start(
                    out=wa[b], in_=w_ada[kc * 128:(kc + 1) * 128, g * 4096:(g + 1) * 4096]), 'wa%d' % b, writes=[('wa', b)])
                for j in range(8):
                    tk.op('pe', lambda e, b=b, kc=kc, j=j: e.matmul(bank(j)[0:1, :], lhsT=cs_[:, kc:kc + 1],
                                                                   rhs=wa[b][:, j * 512:(j + 1) * 512],
                                                                   start=(kc == 0), stop=(kc == 15)),
                          reads=['cs_', ('wa', b)], writes=[('ps', j)], inc=(j == 7))
            for j in range(8):
                tk.op('dve', lambda e, j=j, g=g: e.tensor_tensor(out=stg[0:1, j * 512:(j + 1) * 512], in0=bank(j)[0:1, :],
                                                                 in1=bada[0:1, g * 4096 + j * 512:g * 4096 + (j + 1) * 512],
                                                                 op=ALU.add),
                      reads=[('ps', j), 'bada'], writes=['stg'])
            tk.dma('sp', lambda e, g=g: e.dma_start(out=modd[0:1, g * 4096:(g + 1) * 4096], in_=stg[0:1, :]), 'modst',
                   reads=['stg'], writes=['modd'])
        tk.seal('modst')
        t16 = ar.f32(128)
        for (dst, off, nm, addone) in [(sh1T, 0, 'sh1T', False), (sc1pT, 2048, 'sc1pT', True)]:
            tk.dma('sp', lambda e, off=off: e.dma_start(out=t16[0:16, :],
                                                        in_=modd[0, off:off + 2048].rearrange("(k p) -> k p", p=128)),
                   'm16', reads=['modd'], writes=['t16'])
            tk.op('pe', lambda e: e.transpose(out=bank(0)[:, 0:16], in_=t16[0:16, :], identity=ident[0:16, 0:16]),
                  reads=['t16', 'ident'], writes=[('ps', 0)])
            if addone:
                tk.op('dve', lambda e, dst=dst: e.tensor_scalar(out=dst, in0=bank(0)[:, 0:16], scalar1=1.0, scalar2=None,
                                                                op0=ALU.add), reads=[('ps', 0)], writes=[nm])
            else:
                tk.op('dve', lambda e, dst=dst: e.tensor_copy(out=dst, in_=bank(0)[:, 0:16]), reads=[('ps', 0)], writes=[nm])
        cb32 = ar.f32(3072)
        cbt = ar.f32(3072)
        tk.dma('sp', lambda e: e.dma_start(out=cb32[0:1, :], in_=cbias[:, :]), 'p0b', writes=['cb32'])
        tk.op('dve', lambda e: e.tensor_copy(out=bias2[0:1, :], in_=cb32[0:1, :]), reads=['cb32'], writes=['bias2'])
        tk.op('dve', lambda e: e.tensor_tensor(out=cbt[0:1, :], in0=cb32[0:1, :], in1=bias2[0:1, :], op=ALU.subtract),
              reads=['cb32', 'bias2'], writes=['cbt'])
        lo_d = nc.dram_tensor("lo_d", [1, 3072], BF16, kind="Internal").ap()
        lo16 = ar.bf16(3072)
        tk.op('dve', lambda e: e.tensor_copy(out=lo16[0:1, :], in_=cbt[0:1, :]), reads=['cbt'], writes=['lo16'])
        tk.dma('sp', lambda e: e.dma_start(out=lo_d[:, :], in_=lo16[0:1, :]), 'p0c', reads=['lo16'], writes=['lo_d'])
        tk.dma('sp', lambda e: e.dma_start(out=bias2[1:2, :], in_=lo_d[:, :]), 'p0d', reads=['lo_d', 'bias2'], writes=['bias2'])
        wdt32 = ar.f32(16 * 64)
        tk.dma('sp', lambda e: e.dma_start(out=wdt32.rearrange("p (k n) -> p k n", n=64),
                                           in_=w_dt.rearrange("(k p) n -> p k n", p=128)), 'p0e', writes=['wdt32'])
        tk.op('dve', lambda e: e.tensor_copy(out=wdt_sb, in_=wdt32), reads=['wdt32'], writes=['wdt_sb'])

        tk.barrier()
        ar.off = PERSIST
        cw_sb = ar.f32(9216)
        scw_sb = ar.f32(6144)
        wst = [ar.f32(8192), ar.f32(8192)]
        wcs = [ar.bf16(8192), ar.bf16(8192)]
        tk.dma('sp', lambda e: e.dma_start(out=cw_sb, in_=cwb[:, :]), 'pa', writes=['cw_sb'])
        tk.dma('sp', lambda e: e.dma_start(out=scw_sb, in_=scwb[:, :]), 'pa', writes=['scw_sb'])
        tk.seal('pa')
        bases = []
        for cb in range(6):
            bases.append((w_in[:, OXBC + cb * 512:OXBC + (cb + 1) * 512],
                          [(blk_xbc(cb, t), cw_sb[:, t * 3072 + cb * 512:t * 3072 + (cb + 1) * 512]) for t in range(3)]))
        for g in range(4):
            bases.append((w_in[:, OZ + g * 512:OZ + (g + 1) * 512], [(blk_z(g), None)]))
        for j in range(4):
            bases.append((w_in[:, OGB + j * 512:OGB + (j + 1) * 512], [(blk_sc(j, 0), None)]))
            bases.append((w_in[:, OGC + j * 512:OGC + (j + 1) * 512],
                          [(blk_sc(j, 1 + t), scw_sb[:, t * 2048 + j * 512:t * 2048 + (j + 1) * 512]) for t in range(3)]))
            bases.append((w_in[:, OV + j * 512:OV + (j + 1) * 512], [(blk_sc(j, 4), None)]))
        for nb in range(4):
            for hf in range(2):
                bases.append((w_out[hf * 2048:(hf + 1) * 2048, nb * 512:(nb + 1) * 512], [(blk_out(nb, hf), None)]))
        for hb in range(8):
            bases.append((w_query[:, hb * 512:(hb + 1) * 512], [(blk_q(hb), None)]))
        ci = 0
        for bi, (src, ders) in enumerate(bases if stage >= 1 else []):
            sb_ = bi % 2
            n_ = src.shape[1]
            tk.dma('sp', lambda e, sb_=sb_, src=src, n_=n_: e.dma_start(out=wst[sb_].rearrange("p (k n) -> p k n", n=n_),
                                                                 in_=src.rearrange("(k p) n -> p k n", p=128)),
                   'wst%d' % sb_, writes=[('wst', sb_)])
            for (bid, scl) in ders:
                cbuf = ci % 2
                ci += 1
                if scl is None:
                    eng = ['act', 'dve', 'pool'][ci % 3] if isinstance(bid, tuple) else 'act'
                    if eng == 'act':
                        tk.op('act', lambda e, sb_=sb_, cbuf=cbuf: e.activation(out=wcs[cbuf], in_=wst[sb_], func=AF.Copy),
                              reads=[('wst', sb_)], writes=[('wcs', cbuf)])
                    else:
                        tk.op(eng, lambda e, sb_=sb_, cbuf=cbuf: e.tensor_copy(out=wcs[cbuf], in_=wst[sb_]),
                              reads=[('wst', sb_)], writes=[('wcs', cbuf)])
                else:
                    eng = 'dve' if (ci % 2 == 0) else 'pool'
                    tk.op(eng, lambda e, sb_=sb_, cbuf=cbuf, scl=scl: e.tensor_tensor(
                        out=wcs[cbuf].rearrange("p (k n) -> p k n", n=512),
                        in0=wst[sb_].rearrange("p (k n) -> p k n", n=512),
                        in1=scl.unsqueeze(1).to_broadcast([128, 16, 512]), op=ALU.mult),
                        reads=[('wst', sb_), 'cw_sb', 'scw_sb'], writes=[('wcs', cbuf)])
                if isinstance(bid, tuple):
                    dst, nm = bid
                    tk.dma('act', lambda e, dst=dst, cbuf=cbuf, n_=n_: e.dma_start(
                        out=dst.rearrange("(k p) n -> p k n", p=128), in_=wcs[cbuf].rearrange("p (k n) -> p k n", n=n_)), 'wsst%d' % cbuf,
                        reads=[('wcs', cbuf)], writes=[nm])
                else:
                    tk.dma('act', lambda e, bid=bid, cbuf=cbuf: e.dma_start(out=WS[bid], in_=wcs[cbuf]), 'wsst%d' % cbuf,
                           reads=[('wcs', cbuf)], writes=[('WS', bid)])
        tk.seal('wsst0')
        tk.seal('wsst1')

        tk.barrier()
        ar.off = PERSIST
        NWB = 2
        WB = [ar.bf16(8192) for _ in range(NWB)]
        XT = [ar.f32(2048), ar.f32(2048)]
        XH = [ar.f32(256), ar.f32(256)]
        hT = [ar.bf16(16 * 130) for _ in range(4)]
        xbc0 = ar.f32(3072)
        xbc1 = ar.f32(3072)
        cur = {'j': 0, 'xb': xbc0}
        ysum = ar.f32(2048)
        xd_b = ar.bf16(2048)
        xdd_b = ar.bf16(2048)
        B_b = ar.bf16(512)
        BT_b = ar.bf16(512)
        CT_b = ar.bf16(512)
        HR = ar.f32(2048)
        HRb = ar.bf16(2048)
        HFb = ar.bf16(2048)
        off_x3 = ar.off
        X3 = ar.f32(1024)
        D3 = ar.f32(1024)
        E3 = ar.f32(1024)
        MT_b = ar.bf16(1024)
        yT = ar.bf16(32 * 128)
        R6 = ar.f32(6144)
        g1b, l1g, l1b = R6[:, 0:2048], R6[:, 2048:4096], R6[:, 4096:6144]
        xbcG = [xbc0, xbc1, R6[:, 0:3072], R6[:, 3072:6144]]
        off_ta = ar.off
        tA = ar.f32(512)
        tB = ar.f32(512)
        vt = [ar.f32(512) for _ in range(3)]
        u_t = ar.f32(2048)
        dt64 = ar.f32(64)
        dtA = ar.f32(64)
        sm = ar.f32(512)
        cs32, dst32, ecs32, cdec32, w232 = sm[:, 0:32], sm[:, 32:64], sm[:, 64:96], sm[:, 96:128], sm[:, 128:160]
        t32 = sm[:, 160:192]
        st8 = sm[:, 192:208]
        rs8 = sm[:, 208:224]
        e64 = sm[:, 256:320]

        PA, PB, PT, PS_, PY, PO = bank(0), bank(1), bank(2), bank(3), bank(4), bank(5)
        PC = PSM[:, 6 * 512:8 * 512]
        pacc = [PA, PB]
        pacc_k = [('ps', 0), ('ps', 1)]
        gacc = [PA, PB, PY, PO]
        gacc_k = [('ps', 0), ('ps', 1), 'py', 'po']
        state = {'wi': 0, 'acc': 0, 'hb': 0}

        class WStream:
            def __init__(self, seq):
                self.seq = seq
                self.issued = 0
                self.pos = 0

            def _issue(self):
                if self.issued < len(self.seq):
                    bid = self.seq[self.issued]
                    slot = state['wi'] % NWB
                    state['wi'] += 1
                    tk.dma('sp', lambda e, bid=bid, slot=slot: e.dma_start(out=WB[slot], in_=WS[bid]), 'wb%d' % slot,
                           reads=[('WS', bid)], writes=[('WB', slot)])
                    self.slots = getattr(self, 'slots', []) + [slot]
                    self.issued += 1

            def next(self):
                while self.issued < min(self.pos + NWB, len(self.seq)):
                    self._issue()
                slot = self.slots[self.pos]
                self.pos += 1
                return WB[slot].rearrange("p (k n) -> p k n", n=512), ('WB', slot)

        def load_tile(pos0, zero_lo, zero_hi, xt=None, ht=None):
            xb_ = xt
            hb = ht
            tk.dma('sp', lambda e: e.dma_start(out=XT[xb_], in_=xp[pos0 + 1:pos0 + 129, :]), 'xt%d' % xb_, writes=[('XT', xb_)])
            tk.dma('sp', lambda e: e.dma_start(out=XH[xb_][0:16, 0:128], in_=xp[pos0, :].rearrange("(k p) -> k p", p=128)),
                   'xh%d' % xb_, writes=[('XH', xb_)])
            tk.dma('sp', lambda e: e.dma_start(out=XH[xb_][0:16, 128:256], in_=xp[pos0 + 129, :].rearrange("(k p) -> k p", p=128)),
                   'xh%d' % xb_, writes=[('XH', xb_)])
            h3 = hT[hb].rearrange("p (k t) -> p k t", t=130)
            for k4 in range(4):
                for q in range(4):
                    kc = k4 * 4 + q
                    tk.op('pe', lambda e, kc=kc, q=q: e.transpose(out=PT[:, q * 128:(q + 1) * 128],
                                                                 in_=XT[xb_][:, kc * 128:(kc + 1) * 128], identity=ident),
                          reads=[('XT', xb_), 'ident'], writes=['pt'], inc=(q == 3))
                for q in range(4):
                    kc = k4 * 4 + q
                    tk.op('act', lambda e, kc=kc, q=q: e.activation(out=h3[:, kc, 1:129], in_=PT[:, q * 128:(q + 1) * 128],
                                                                   func=AF.Identity, scale=sc1pT[:, kc:kc + 1],
                                                                   bias=sh1T[:, kc:kc + 1]),
                          reads=['pt', 'sc1pT', 'sh1T'], writes=[('hT', hb, kc)])
            for hi in range(2):
                tk.op('pe', lambda e, hi=hi: e.transpose(out=PS_[:, 256 + hi * 16:256 + hi * 16 + 16],
                                                        in_=XH[xb_][0:16, hi * 128:(hi + 1) * 128], identity=ident[0:16, 0:16]),
                      reads=[('XH', xb_), 'ident'], writes=['ps_'], inc=(hi == 1))
            tk.op('dve', lambda e: e.tensor_tensor(out=t32.rearrange("p (t k) -> p t k", k=16),
                                                   in0=PS_[:, 256:288].rearrange("p (t k) -> p t k", k=16),
                                                   in1=sc1pT.unsqueeze(1).to_broadcast([128, 2, 16]), op=ALU.mult),
                  reads=['ps_', 'sc1pT'], writes=['t32'])
            tk.op('dve', lambda e: e.tensor_tensor(out=h3[:, :, 0:130:129].rearrange("p k t -> p t k"),
                                                   in0=t32.rearrange("p (t k) -> p t k", k=16),
                                                   in1=sh1T.unsqueeze(1).to_broadcast([128, 2, 16]), op=ALU.add),
                  reads=['t32', 'sh1T'], writes=[('hTh', hb)])
            if zero_lo:
                tk.op('dve', lambda e: e.memset(h3[:, :, 0:1], 0.0), reads=[('hTh', hb)], writes=[('hTh', hb)])
            if zero_hi:
                tk.op('dve', lambda e: e.memset(h3[:, :, 129:130], 0.0), reads=[('hTh', hb)], writes=[('hTh', hb)])
            return hb, h3

        def hkeys(hb):
            return [('hT', hb, kc) for kc in range(16)] + [('hTh', hb)]

        def proj(ps, pskey, hb, h3, taps_blocks, ncols=512, bias_cols=None, first=True, last=True):
            n = len(taps_blocks)
            for ti, (tap, wv, wkey) in enumerate(taps_blocks):
                for kc in range(16):
                    st = first and ti == 0 and kc == 0
                    sp_ = last and (bias_cols is None) and ti == n - 1 and kc == 15
                    tk.op('pe', lambda e, tap=tap, wv=wv, kc=kc, st=st, sp_=sp_: e.matmul(
                        ps[:, 0:ncols], lhsT=h3[:, kc, tap:tap + 128], rhs=wv[:, kc, 0:ncols], start=st, stop=sp_),
                        reads=hkeys(hb) + [wkey], writes=[pskey], inc=(kc == 15))
            if bias_cols is not None:
                c0 = bias_cols
                tk.op('pe', lambda e: e.matmul(ps[:, 0:ncols], lhsT=ones_b[0:2, :], rhs=bias2[0:2, c0:c0 + ncols],
                                               start=False, stop=True),
                      reads=['ones_b', 'bias2'], writes=[pskey])

        wdt3 = wdt_sb.rearrange("p (k n) -> p k n", n=64)

        def xbc_blocks_group(ws, infos, cbs, hook=None):
            for cb in cbs:
                for tap in range(3):
                    wv, wkey = ws.next()
                    for j, (hb, h3) in enumerate(infos):
                        proj(gacc[j], gacc_k[j], hb, h3, [(tap, wv, wkey)], bias_cols=(cb * 512 if tap == 2 else None),
                             first=(tap == 0), last=(tap == 2))
                    if hook is not None:
                        hook()
                for j, (hb, h3) in enumerate(infos):
                    tk.op('act', lambda e, j=j, cb=cb: e.activation(out=xbcG[j][:, cb * 512:(cb + 1) * 512], in_=gacc[j], func=AF.Silu),
                          reads=[gacc_k[j]], writes=[('xbc', j, cb)])

        def dt_for_tile(hb, h3):
            a = state['acc'] % 2
            state['acc'] += 1
            proj(pacc[a], pacc_k[a], hb, h3, [(1, wdt3, 'wdt_sb')], ncols=64)
            tk.op('dve', lambda e, a=a: e.tensor_tensor(out=dt64, in0=pacc[a][:, 0:64], in1=dtb, op=ALU.add),
                  reads=[pacc_k[a], 'dtb'], writes=['dt64'])
            tk.op('act', lambda e: e.activation(out=e64, in_=dt64, func=AF.Exp), reads=['dt64'], writes=['e64'])
            tk.op('act', lambda e: e.activation(out=dt64, in_=e64, func=AF.Ln, bias=1.0), reads=['e64'], writes=['dt64'])
            tk.op('dve', lambda e: e.tensor_tensor(out=dtA, in0=dt64, in1=a_neg, op=ALU.mult),
                  reads=['dt64', 'a_neg'], writes=['dtA'])

        def chunk_scalars(dr, full):
            xbc = cur['xb']
            jj = cur['j']
            sl = slice(dr * 32, dr * 32 + 32)
            tri, trik = (triF, 'triF') if dr == 0 else (triR, 'triR')
            tk.op('pe', lambda e: e.matmul(PS_[:, 0:32], lhsT=tri, rhs=dtA[:, sl], start=True, stop=True),
                  reads=[trik, 'dtA'], writes=['ps_'], inc=False)
            tk.op('pe', lambda e: e.matmul(PS_[:, 32:64], lhsT=ones_f, rhs=dtA[:, sl], start=True, stop=True),
                  reads=['ones_f', 'dtA'], writes=['ps_'])
            tk.op('dve', lambda e: e.tensor_copy(out=cs32, in_=PS_[:, 0:32]), reads=['ps_'], writes=['cs32'])
            tk.op('dve', lambda e: e.tensor_tensor(out=dst32, in0=PS_[:, 32:64], in1=cs32, op=ALU.subtract),
                  reads=['ps_', 'cs32'], writes=['dst32'])
            tk.op('act', lambda e: e.activation(out=dst32, in_=dst32, func=AF.Exp), reads=['dst32'], writes=['dst32'])
            tk.op('act', lambda e: e.activation(out=cdec32, in_=PS_[:, 32:64], func=AF.Exp), reads=['ps_'], writes=['cdec32'])
            if full:
                tk.op('act', lambda e: e.activation(out=ecs32, in_=cs32, func=AF.Exp), reads=['cs32'], writes=['ecs32'])
            tk.op('dve', lambda e: e.tensor_tensor(out=w232, in0=dt64[:, sl], in1=dst32, op=ALU.mult),
                  reads=['dt64', 'dst32'], writes=['w232'])
            xs3 = xbc[:, 0:2048].rearrange("p (h d) -> p h d", d=64)
            tk.op('pool', lambda e: e.tensor_tensor(out=xdd_b.rearrange("p (h d) -> p h d", d=64), in0=xs3,
                                                    in1=w232.unsqueeze(2).to_broadcast([128, 32, 64]), op=ALU.mult),
                  reads=[('xbc', jj, i) for i in range(4)] + ['w232'], writes=['xdd_b'])
            if full:
                tk.op('pool', lambda e: e.tensor_tensor(out=xd_b.rearrange("p (h d) -> p h d", d=64), in0=xs3,
                                                        in1=dt64[:, sl].unsqueeze(2).to_broadcast([128, 32, 64]), op=ALU.mult),
                      reads=[('xbc', jj, i) for i in range(4)] + ['dt64'], writes=['xd_b'])

        def state_update(H, Hk):
            for g in range(4):
                tk.op('pe', lambda e, g=g: e.matmul(PO, lhsT=B_b[:, g * 128:(g + 1) * 128], rhs=xdd_b[:, g * 512:(g + 1) * 512],
                                                    start=True, stop=True),
                      reads=['B_b', 'xdd_b'], writes=['po'])
                Hg = H[:, g * 512:(g + 1) * 512].rearrange("p (h d) -> p h d", d=64)
                tk.op('dve', lambda e, g=g, Hg=Hg: e.tensor_tensor(out=Hg, in0=Hg,
                                                                   in1=cdec32[:, g * 8:(g + 1) * 8].unsqueeze(2).to_broadcast([128, 8, 64]),
                                                                   op=ALU.mult),
                      reads=[(Hk, g), 'cdec32'], writes=[(Hk, g)])
                tk.op('dve', lambda e, g=g: e.tensor_tensor(out=H[:, g * 512:(g + 1) * 512], in0=H[:, g * 512:(g + 1) * 512],
                                                            in1=PO, op=ALU.add),
                      reads=[(Hk, g), 'po'], writes=[(Hk, g)])

        def make_Bb():
            xbc = cur['xb']
            tk.op('act', lambda e: e.activation(out=B_b, in_=xbc[:, 2048:2560], func=AF.Copy), reads=[('xbc', cur['j'], 4)], writes=['B_b'])

        Hkeys = [('HR', g) for g in range(4)]

        cin = [ARN[:, off_x3:off_x3 + 2048], ARN[:, off_x3 + 2048:off_x3 + 4096]]
        cout = [ARN[:, off_ta:off_ta + 1024].bitcast(BF16), ARN[:, off_ta + 1024:off_ta + 2048].bitcast(BF16)]

        def conv_gen():
            def src(k):
                tab = eu if k < 128 else ev
                r0 = (k % 128) * 128
                return tab[r0:r0 + 128, :]

            def dst(k):
                tab = EUb if k < 128 else EVb
                r0 = (k % 128) * 128
                return tab[r0:r0 + 128, :], ('EUb' if k < 128 else 'EVb')

            def load(k):
                i = k % 2
                sk = src(k)
                tk.dma('pool', lambda e: e.dma_start(out=cin[i], in_=sk), 'cvl%d' % i, writes=[('cin', i)])
            load(0)
            for k in range(256):
                if k + 1 < 256:
                    load(k + 1)
                i = k % 2
                tk.op('dve', lambda e, i=i: e.tensor_copy(out=cout[i], in_=cin[i]), reads=[('cin', i)], writes=[('cout', i)])
                dk, nm = dst(k)
                tk.dma('pool', lambda e, i=i, dk=dk: e.dma_start(out=dk, in_=cout[i]), 'cvs%d' % i, reads=[('cout', i)], writes=[nm])
                yield

        cvg = conv_gen()

        def state_sweep(tiles, dr, H, Hk, spill):
            groups = [tiles[i:i + 4] for i in range(0, len(tiles), 4)]
            seq = []
            for _ in groups:
                for cb in range(5):
                    for tap in range(3):
                        seq.append(blk_xbc(cb, tap))
            ws = WStream(seq)
            for grp in groups:
                infos = []
                for j, T in enumerate(grp):
                    infos.append(load_tile(T * 128, T == 0, T == 31, xt=j % 2, ht=j))
                xbc_blocks_group(ws, infos, range(5), hook=lambda: [next(cvg, None) for _c in range(3)])
                for j, T in enumerate(grp):
                    if spill:
                        tk.op('act', lambda e: e.activation(out=HFb, in_=H, func=AF.Copy), reads=[(Hk, g) for g in range(4)], writes=['HFb'])
                        tk.dma('sp', lambda e, T=T: e.dma_start(out=HFS[T], in_=HFb), 'hfs', reads=['HFb'], writes=[('HFS', T)])
                    cur['j'] = j
                    cur['xb'] = xbcG[j]
                    dt_for_tile(*infos[j])
                    make_Bb()
                    chunk_scalars(dr, False)
                    state_update(H, Hk)

        tk.op('dve', lambda e: e.memset(HR, 0.0), writes=Hkeys)
        if stage >= 2:
            state_sweep(list(range(31, 15, -1)), 1, HR, 'HR', False)
        HF = ysum
        HFk = [('HF', g) for g in range(4)]
        tk.op('dve', lambda e: e.memset(HF, 0.0), writes=HFk)
        if stage >= 2:
            state_sweep(list(range(15)), 0, HF, 'HF', True)
            tk.op('act', lambda e: e.activation(out=HFb, in_=HF, func=AF.Copy), reads=HFk, writes=['HFb'])
            tk.dma('sp', lambda e: e.dma_start(out=HFS[15], in_=HFb), 'hfs', reads=['HFb'], writes=[('HFS', 15)])
        tk.seal('hfs')
        for _ in cvg:
            pass
        tk.barrier()
        tk.dma('sp', lambda e: e.dma_start(out=g1b, in_=modd[0:1, 4096:6144].partition_broadcast(128)), 'pm',
               reads=['modd'], writes=['g1b'])
        tk.dma('sp', lambda e: e.dma_start(out=l1g, in_=ln1g_b[:, :]), 'pm', writes=['l1g'])
        tk.dma('sp', lambda e: e.dma_start(out=l1b, in_=ln1b_b[:, :]), 'pm', writes=['l1b'])
        tk.seal('pm')

        seqC = []
        for pi in range(8):
            for cb in range(6):
                for tap in range(3):
                    seqC.append(blk_xbc(cb, tap))
            for _t in range(2):
                for j in range(4):
                    seqC += [blk_sc(j, 4), blk_sc(j, 1), blk_sc(j, 2), blk_sc(j, 3), blk_sc(j, 0)]
                for g in range(4):
                    seqC.append(blk_z(g))
                for nb in range(4):
                    seqC += [blk_out(nb, 0), blk_out(nb, 1)]
        ws = WStream(seqC)
        yT3 = yT.rearrange("p (k t) -> p k t", t=128)

        def transposes_to_yT(src, srckey, kbase, gT):
            for cc in range(4):
                q = cc
                tk.op('pe', lambda e, cc=cc, q=q: e.transpose(out=PT[:, q * 128:(q + 1) * 128],
                                                             in_=src[:, cc * 128:(cc + 1) * 128], identity=ident),
                      reads=[srckey, 'ident'], writes=['pt'], inc=(q == 3))
            for cc in range(4):
                q = cc
                col = kbase + cc - (kbase // 16) * 16
                tk.op('act', lambda e, cc=cc, q=q, col=col: e.activation(out=yT3[:, kbase + cc, :], in_=PT[:, q * 128:(q + 1) * 128],
                                                                        func=AF.Copy, scale=gT[:, col:col + 1]),
                      reads=['pt', 'ssdg', 'scg'], writes=[('yT', kbase + cc)])

        def rstd_from_ss(ss_ap, n_elems, width, key_in, key_out, out_ap):
            tk.op('dve', lambda e: e.tensor_scalar(out=out_ap, in0=ss_ap, scalar1=1.0 / n_elems, scalar2=EPS, op0=ALU.mult,
                                                   op1=ALU.add), reads=[key_in], writes=[key_out])
            tk.op('act', lambda e: e.activation(out=out_ap, in_=out_ap, func=AF.Sqrt), reads=[key_out], writes=[key_out])
            tk.op('dve', lambda e: e.reciprocal(out=out_ap, in_=out_ap), reads=[key_out], writes=[key_out])

        def sc_gen(hb, h3):
            tC = u_t[:, 0:512]
            tD = u_t[:, 512:1024]
            st8s = sm[:, 320:328]
            rs8s = sm[:, 328:336]
            for j in range(4):
                wv, wkey = ws.next()
                for tap in range(3):
                    a = state['acc'] % 2
                    state['acc'] += 1
                    proj(pacc[a], pacc_k[a], hb, h3, [(tap, wv, wkey)])
                    tk.op('act', lambda e, a=a, tap=tap: e.activation(out=vt[tap], in_=pacc[a], func=AF.Copy),
                          reads=[pacc_k[a]], writes=[('vt', tap)])
                    yield
                for tap in range(3):
                    a = state['acc'] % 2
                    state['acc'] += 1
                    wv, wkey = ws.next()
                    proj(pacc[a], pacc_k[a], hb, h3, [(tap, wv, wkey)])
                    if tap == 0:
                        tk.op('dve', lambda e, a=a: e.tensor_tensor(out=tD, in0=pacc[a], in1=vt[0], op=ALU.mult),
                              reads=[pacc_k[a], ('vt', 0)], writes=['tD', 'u_t'])
                    else:
                        tk.op('dve', lambda e, a=a, tap=tap: e.tensor_tensor(out=tC, in0=pacc[a], in1=vt[tap], op=ALU.mult),
                              reads=[pacc_k[a], ('vt', tap)], writes=['tC', 'u_t'])
                        tk.op('pool', lambda e: e.tensor_tensor(out=tD, in0=tD, in1=tC, op=ALU.add),
                              reads=['tC', 'tD'], writes=['tD'])
                    yield
                a = state['acc'] % 2
                state['acc'] += 1
                wv, wkey = ws.next()
                proj(pacc[a], pacc_k[a], hb, h3, [(1, wv, wkey)])
                tk.op('dve', lambda e, a=a: e.tensor_tensor(out=tD, in0=pacc[a], in1=tD, op=ALU.mult),
                      reads=[pacc_k[a], 'tD'], writes=['tD'])
                tk.op('act', lambda e: e.activation(out=tC, in_=tD, func=AF.Square), reads=['tD'], writes=['tC'])
                tk.op('dve', lambda e: e.tensor_reduce(out=st8s, in_=tC.rearrange("p (g d) -> p g d", d=64), axis=AX.X,
                                                       op=ALU.add), reads=['tC'], writes=['st8s'])
                rstd_from_ss(st8s, 64.0, 8, 'st8s', 'rs8s', rs8s)
                tk.op('dve', lambda e: e.tensor_tensor(out=tD.rearrange("p (g d) -> p g d", d=64),
                                                       in0=tD.rearrange("p (g d) -> p g d", d=64),
                                                       in1=rs8s.unsqueeze(2).to_broadcast([128, 8, 64]), op=ALU.mult),
                      reads=['tD', 'rs8s'], writes=['tD'])
                transposes_to_yT(tD, 'tD', 16 + j * 4, scg)
                yield

        def tile_C(T, hb, h3, xbc, jj):
            scg_ = sc_gen(hb, h3)
            XBK = [('xbc', jj, i) for i in range(4)]
            xs3 = xbc[:, 0:2048].rearrange("p (h d) -> p h d", d=64)
            dt_for_tile(hb, h3)
            make_Bb()
            tk.dma('sp', lambda e, T=T: e.dma_start(out=HFb, in_=HFS[T]), 'hfl', reads=[('HFS', T)], writes=['HFb'])
            for (srcoff, dstb, dk) in [(2048, BT_b, 'BT_b'), (2560, CT_b, 'CT_b')]:
                for g in range(4):
                    tk.op('pe', lambda e, g=g, srcoff=srcoff: e.transpose(out=PT[:, g * 128:(g + 1) * 128],
                                                                         in_=xbc[:, srcoff + g * 128:srcoff + (g + 1) * 128],
                                                                         identity=ident),
                          reads=[('xbc', jj, 4), ('xbc', jj, 5), 'ident'], writes=['pt'], inc=(g == 3))
                tk.op('act', lambda e, dstb=dstb: e.activation(out=dstb, in_=PT, func=AF.Copy),
                      reads=['pt' for g in range(4)], writes=[dk])
            tk.op('pool', lambda e: e.tensor_tensor(out=ysum.rearrange("p (h d) -> p h d", d=64), in0=xs3,
                                                    in1=dsk.unsqueeze(2).to_broadcast([128, 32, 64]), op=ALU.mult),
                  reads=XBK + ['dsk'], writes=['ysum'] + HFk)
            for dr in (1, 0):
                sl0 = dr * 32
                chunk_scalars(dr, True)
                Hb, Hbk = (HRb, 'HRb') if dr == 1 else (HFb, 'HFb')
                if dr == 1:
                    tk.op('act', lambda e: e.activation(out=HRb, in_=HR, func=AF.Copy), reads=Hkeys, writes=['HRb'])
                tri, trik = (triF, 'triF') if dr == 0 else (triR, 'triR')
                for g in range(4):
                    hs = slice(sl0 + g * 8, sl0 + g * 8 + 8)
                    tk.op('pe', lambda e, g=g: e.matmul(PS_[:, 64:192], lhsT=BT_b[:, g * 128:(g + 1) * 128],
                                                        rhs=CT_b[:, g * 128:(g + 1) * 128], start=True, stop=True),
                          reads=['BT_b', 'CT_b'], writes=['ps_'])
                    X3v = X3.rearrange("p (h l) -> p h l", l=128)
                    tk.op('pool', lambda e, hs=hs, tri=tri: e.tensor_tensor(
                        out=X3v, in0=dtA[:, hs].unsqueeze(2).to_broadcast([128, 8, 128]),
                        in1=tri.unsqueeze(1).to_broadcast([128, 8, 128]), op=ALU.mult),
                        reads=['dtA', trik], writes=['X3'])
                    for hf in range(2):
                        tk.op('pe', lambda e, hf=hf: e.matmul(PC[:, hf * 512:(hf + 1) * 512], lhsT=ones_f,
                                                              rhs=X3[:, hf * 512:(hf + 1) * 512], start=True, stop=True),
                              reads=['ones_f', 'X3'], writes=['pc'], inc=(hf == 1))
                    D3v = D3.rearrange("p (h l) -> p h l", l=128)
                    tk.op('dve', lambda e, g=g: e.tensor_tensor(
                        out=D3v, in0=PC.rearrange("p (h l) -> p h l", l=128),
                        in1=cs32[:, g * 8:(g + 1) * 8].unsqueeze(2).to_broadcast([128, 8, 128]), op=ALU.subtract),
                        reads=['pc', 'cs32'], writes=['D3'])
                    if dr == 0:
                        patt, cm = [[0, 8], [1, 128]], -1
                    else:
                        patt, cm = [[0, 8], [-1, 128]], 1
                    tk.op('pool', lambda e, patt=patt, cm=cm: e.affine_select(out=D3v, in_=D3v, pattern=patt, compare_op=ALU.is_ge,
                                                                             fill=tk.getreg(e, -200.0), base=0, channel_multiplier=cm),
                          reads=['D3'], writes=['D3'])
                    tk.op('act', lambda e: e.activation(out=E3, in_=D3, func=AF.Exp), reads=['D3'], writes=['E3'])
                    tk.op('dve', lambda e: e.tensor_tensor(
                        out=MT_b.rearrange("p (h l) -> p h l", l=128), in0=E3.rearrange("p (h l) -> p h l", l=128),
                        in1=PS_[:, 64:192].unsqueeze(1).to_broadcast([128, 8, 128]), op=ALU.mult),
                        reads=['E3', 'ps_'], writes=['MT_b'])
                    for _i in range(4):
                        next(scg_, None)
                    for hh in range(8):
                        hd = g * 8 + hh
                        tk.op('pe', lambda e, hh=hh, hd=hd: e.matmul(PY[:, hh * 64:(hh + 1) * 64], lhsT=MT_b[:, hh * 128:(hh + 1) * 128],
                                                                     rhs=xd_b[:, hd * 64:(hd + 1) * 64], start=True, stop=True),
                              reads=['MT_b', 'xd_b'], writes=['py'], inc=(hh == 7))
                    tk.op('pe', lambda e, g=g, Hb=Hb: e.matmul(PO, lhsT=CT_b[:, g * 128:(g + 1) * 128],
                                                               rhs=Hb[:, g * 512:(g + 1) * 512], start=True, stop=True),
                          reads=['CT_b', Hbk], writes=['po'])
                    tk.op('dve', lambda e, g=g: e.tensor_tensor(
                        out=tA.rearrange("p (h d) -> p h d", d=64), in0=PO.rearrange("p (h d) -> p h d", d=64),
                        in1=ecs32[:, g * 8:(g + 1) * 8].unsqueeze(2).to_broadcast([128, 8, 64]), op=ALU.mult),
                        reads=['po', 'ecs32'], writes=['tA'])
                    tk.op('dve', lambda e: e.tensor_tensor(out=tA, in0=tA, in1=PY, op=ALU.add), reads=['tA', 'py'], writes=['tA'])
                    tk.op('pool', lambda e, g=g: e.tensor_tensor(out=ysum[:, g * 512:(g + 1) * 512],
                                                                 in0=ysum[:, g * 512:(g + 1) * 512], in1=tA, op=ALU.add),
                          reads=['ysum', 'tA'], writes=['ysum'])
                if dr == 1:
                    state_update(HR, 'HR')
            for g in range(4):
                a = state['acc'] % 2
                state['acc'] += 1
                wv, wkey = ws.next()
                proj(pacc[a], pacc_k[a], hb, h3, [(1, wv, wkey)])
                tk.op('act', lambda e, a=a: e.activation(out=tB, in_=pacc[a], func=AF.Silu), reads=[pacc_k[a]], writes=['tB'])
                tk.op('dve', lambda e, g=g: e.tensor_tensor(out=tB, in0=tB, in1=ysum[:, g * 512:(g + 1) * 512], op=ALU.mult),
                      reads=['tB', 'ysum'], writes=['tB'])
                tk.op('act', lambda e: e.activation(out=tA, in_=tB, func=AF.Square, accum_out=st8[:, 0:1]),
                      reads=['tB'], writes=['tA', 'st8'])
                rstd_from_ss(st8[:, 0:1], 512.0, 1, 'st8', 'rs8', rs8[:, 0:1])
                tk.op('dve', lambda e: e.tensor_scalar(out=tB, in0=tB, scalar1=rs8[:, 0:1], scalar2=None, op0=ALU.mult),
                      reads=['tB', 'rs8'], writes=['tB'])
                transposes_to_yT(tB, 'tB', g * 4, ssdg)
            for _ in scg_:
                pass
            ypk = [('yT', k) for k in range(32)]
            for nb in range(4):
                a = state['acc'] % 2
                state['acc'] += 1
                for hf in range(2):
                    wv, wkey = ws.next()
                    for kc in range(16):
                        tk.op('pe', lambda e, a=a, hf=hf, kc=kc, wv=wv: e.matmul(
                            pacc[a], lhsT=yT3[:, hf * 16 + kc, :], rhs=wv[:, kc, :], start=(hf == 0 and kc == 0),
                            stop=(hf == 1 and kc == 15)), reads=ypk + [wkey], writes=[pacc_k[a]], inc=(kc == 15))
                tk.op('dve', lambda e, a=a, nb=nb: e.tensor_tensor(out=tA, in0=pacc[a], in1=g1b[:, nb * 512:(nb + 1) * 512], op=ALU.mult),
                      reads=[pacc_k[a], 'g1b'], writes=['tA'])
                if DBG == 1:
                    tk.op('dve', lambda e, a=a, nb=nb: e.tensor_copy(out=u_t[:, nb * 512:(nb + 1) * 512], in_=pacc[a]),
                          reads=[pacc_k[a], 'tA'], writes=['u_t'])
                else:
                    tk.op('dve', lambda e, nb=nb, hb=hb: e.scalar_tensor_tensor(out=u_t[:, nb * 512:(nb + 1) * 512],
                                                                                in0=XT[hb][:, nb * 512:(nb + 1) * 512], scalar=ALPHA, in1=tA,
                                                                                op0=ALU.mult, op1=ALU.add),
                          reads=[('XT', hb), 'tA'], writes=['u_t'])
            if DBG == 3:
                tk.op('dve', lambda e: e.tensor_copy(out=u_t[:, 0:1024], in_=xbc[:, 2048:3072]), reads=[('xbc', jj, 4), ('xbc', jj, 5), 'u_t'], writes=['u_t'])
                tk.op('dve', lambda e: e.tensor_copy(out=u_t[:, 1024:1088], in_=dt64), reads=['dt64', 'u_t'], writes=['u_t'])
                tk.op('dve', lambda e: e.tensor_copy(out=u_t[:, 1088:2048], in_=xbc[:, 0:960]), reads=XBK + ['u_t'], writes=['u_t'])
            if DBG == 2:
                tk.op('dve', lambda e: e.tensor_copy(out=u_t, in_=ysum), reads=['ysum', 'u_t'], writes=['u_t'])
            if DBG == 0:
                layer_norm(tk, u_t, 'u_t', sm, l1g, 'l1g', l1b, 'l1b', xbc[:, 0:2048], XBK)
            tk.dma('pool', lambda e, T=T: e.dma_start(out=X1S[T * 128:(T + 1) * 128, :], in_=u_t), 'x1s',
                   reads=['u_t'], writes=[('X1S', T)])

        for pi in range(8):
            if stage < 3:
                break
            pair = [15 - 2 * pi, 14 - 2 * pi]
            infos = [load_tile(T * 128, T == 0, False, xt=j, ht=j) for j, T in enumerate(pair)]
            xbc_blocks_group(ws, infos, range(6))
            for j, T in enumerate(pair):
                cur['j'] = j
                cur['xb'] = xbcG[j]
                tile_C(T, infos[j][0], infos[j][1], xbcG[j], j)
        tk.seal('x1s')

        tk.barrier()
        ar.off = PERSIST
        if stage >= 4:
            peer_phase(nc, tk, ar, bank, PSM, WS, X1S, modd, kT, EUb, EVb, ln2g_b, ln2b_b, out, ident, iota16)
        else:
            tb = ar.f32(2048)
            for T in range(16):
                tk.dma('sp', lambda e, T=T: e.dma_start(out=tb, in_=X1S[T * 128:(T + 1) * 128, :]), 'dbl',
                       reads=[('X1S', T)], writes=['tb'])
                tk.dma('sp', lambda e, T=T: e.dma_start(out=out[T * 128:(T + 1) * 128, :], in_=tb), 'outst',
                       reads=['tb'], writes=[('out', T)])
        tk.barrier()
        tk.emit()
    return nc


def layer_norm(tk, u, uk, sm, g, gk, b, bk, scratch, scratch_keys):
    mean = sm[:, 224:225]
    ssq = sm[:, 225:226]
    rstd = sm[:, 226:227]
    tk.op('act', lambda e: e.activation(out=scratch, in_=u, func=AF.Identity, accum_out=mean),
          reads=[uk], writes=list(scratch_keys) + ['ln_mean'])
    tk.op('dve', lambda e: e.tensor_scalar(out=mean, in0=mean, scalar1=1.0 / 2048.0, scalar2=None, op0=ALU.mult),
          reads=['ln_mean'], writes=['ln_mean'])
    tk.op('dve', lambda e: e.tensor_scalar(out=u, in0=u, scalar1=mean, scalar2=None, op0=ALU.subtract),
          reads=[uk, 'ln_mean'], writes=[uk])
    tk.op('act', lambda e: e.activation(out=scratch, in_=u, func=AF.Square, accum_out=ssq),
          reads=[uk], writes=list(scratch_keys) + ['ln_ssq'])
    tk.op('dve', lambda e: e.tensor_scalar(out=rstd, in0=ssq, scalar1=1.0 / 2048.0, scalar2=EPS, op0=ALU.mult, op1=ALU.add),
          reads=['ln_ssq'], writes=['ln_rstd'])
    tk.op('act', lambda e: e.activation(out=rstd, in_=rstd, func=AF.Sqrt), reads=['ln_rstd'], writes=['ln_rstd'])
    tk.op('dve', lambda e: e.reciprocal(out=rstd, in_=rstd), reads=['ln_rstd'], writes=['ln_rstd'])
    tk.op('dve', lambda e: e.tensor_scalar(out=u, in0=u, scalar1=rstd, scalar2=None, op0=ALU.mult),
          reads=[uk, 'ln_rstd'], writes=[uk])
    tk.op('dve', lambda e: e.tensor_tensor(out=u, in0=u, in1=g, op=ALU.mult), reads=[uk, gk], writes=[uk])
    tk.op('dve', lambda e: e.tensor_tensor(out=u, in0=u, in1=b, op=ALU.add), reads=[uk, bk], writes=[uk])


def peer_phase(nc, tk, ar, bank, PSM, WS, X1S, modd, kT, eu, ev, ln2g_b, ln2b_b, out, ident, iota16):
    NB = 12
    sc2p = ar.f32(2048)
    sh2 = ar.f32(2048)
    g2b = ar.f32(2048)
    l2g = ar.f32(2048)
    l2b = ar.f32(2048)
    KT_b = ar.bf16(4096)
    acc = ar.f32(2048)
    x1 = [acc, ar.f32(2048)]
    h2 = [ar.f32(2048), ar.f32(2048)]
    h2T = ar.bf16(16 * 128)
    qT = ar.bf16(32 * 128)
    WQ = [ar.bf16(8192)]
    S = ar.f32(2048)
    S2 = ar.f32(2048)
    V16 = ar.f32(256)
    I16 = ar.u32(256)
    I16f = ar.f32(256)
    CAND = ar.f32(2048)
    CAND2 = S2
    SCV = ar.f32(128)
    FL = ar.u32(128)
    ABf = ar.f32(256)
    OH = CAND
    E12 = ar.f32(256)
    IDX = [ar.i32(128), ar.i32(128), ar.i32(128)]
    IDXf = ar.f32(128)
    GATE = [ar.f32(128), ar.f32(128), ar.f32(128)]
    ACT_ = [ar.f32(128), ar.f32(128)]
    COEF = [ar.f32(128), ar.f32(128)]
    sm = ar.f32(512)
    thr16 = sm[:, 16:32]
    UBR = ar.f32(NB * 1024)
    UB = [UBR[:, b * 1024:(b + 1) * 1024].bitcast(BF16) for b in range(NB)]
    DG = [ar.bf16(128) for _ in range(4)]
    junk = OH

    tk.dma('sp', lambda e: e.dma_start(out=sh2, in_=modd[0:1, 6144:8192].partition_broadcast(128)), 'pd', reads=['modd'], writes=['sh2'])
    tk.dma('sp', lambda e: e.dma_start(out=sc2p, in_=modd[0:1, 8192:10240].partition_broadcast(128)), 'pd', reads=['modd'], writes=['sc2p'])
    tk.dma('sp', lambda e: e.dma_start(out=g2b, in_=modd[0:1, 10240:12288].partition_broadcast(128)), 'pd', reads=['modd'], writes=['g2b'])
    tk.dma('sp', lambda e: e.dma_start(out=l2g, in_=ln2g_b[:, :]), 'pd', writes=['l2g'])
    tk.dma('sp', lambda e: e.dma_start(out=l2b, in_=ln2b_b[:, :]), 'pd', writes=['l2b'])
    tk.seal('pd')
    tk.op('dve', lambda e: e.tensor_scalar(out=thr16, in0=iota16, scalar1=16.0, scalar2=16.0, op0=ALU.mult, op1=ALU.add),
          reads=['iota16'], writes=['thr16'])
    tk.op('dve', lambda e: e.tensor_scalar(out=sc2p, in0=sc2p, scalar1=1.0, scalar2=None, op0=ALU.add), reads=['sc2p'], writes=['sc2p'])
    for i in range(2):
        k32 = UBR[:, i * 2048:(i + 1) * 2048]
        tk.dma('sp', lambda e, i=i, k32=k32: e.dma_start(out=k32, in_=kT[:, i * 2048:(i + 1) * 2048]), 'pd2_%d' % i,
               writes=[('UB', 2 * i), ('UB', 2 * i + 1)])
        tk.op('act', lambda e, i=i, k32=k32: e.activation(out=KT_b[:, i * 2048:(i + 1) * 2048], in_=k32, func=AF.Copy),
              reads=[('UB', 2 * i), ('UB', 2 * i + 1)], writes=['KT_b'])
    h2T3 = h2T.rearrange("p (k t) -> p k t", t=128)
    qT3 = qT.rearrange("p (k t) -> p k t", t=128)
    KT3 = KT_b.rearrange("p (k n) -> p k n", n=128)
    PT = bank(2)
    PQ = [bank(0), bank(1)]
    PS3 = bank(3)
    PSC = PSM[:, 4 * 512:8 * 512]
    st = {'qi': 0, 'gi': 0}
    V3 = V16.rearrange("p (h k) -> p h k", k=16)
    I3 = I16.rearrange("p (h k) -> p h k", k=16)
    S3 = S.rearrange("p (h k) -> p h k", k=128)
    S23 = S2.rearrange("p (h k) -> p h k", k=128)
    V4 = V16.rearrange("p (h s k) -> p h s k", s=2, k=16)
    C4 = CAND.rearrange("p (h a b) -> p h a b", a=16, b=16)
    C3 = CAND.rearrange("p (h c) -> p h c", c=256)
    C23 = CAND2.rearrange("p (h c) -> p h c", c=256)
    SC3 = SCV.rearrange("p (h k) -> p h k", k=16)
    FL3 = FL.rearrange("p (h k) -> p h k", k=16)
    I4f = I16f.rearrange("p (h s k) -> p h s k", s=2, k=16)
    OH4 = OH.rearrange("p (h k a) -> p h k a", k=16, a=16)

    def top16(vals, idxs, src, src2, hh, keys):
        kv, ki, ks, ks2 = keys
        tk.op('dve', lambda e: e.max(out=vals[:, hh, 0:8], in_=src[:, hh, :]), reads=[ks], writes=[kv])
        tk.op('dve', lambda e: e.max_index(out=idxs[:, hh, 0:8], in_max=vals[:, hh, 0:8], in_values=src[:, hh, :]),
              reads=[ks, kv], writes=[ki])
        tk.op('dve', lambda e: e.match_replace(out=src2[:, hh, :], in_to_replace=vals[:, hh, 0:8], in_values=src[:, hh, :],
                                               imm_value=NEG), reads=[ks, kv], writes=[ks2])
        tk.op('dve', lambda e: e.max(out=vals[:, hh, 8:16], in_=src2[:, hh, :]), reads=[ks2], writes=[kv])
        tk.op('dve', lambda e: e.max_index(out=idxs[:, hh, 8:16], in_max=vals[:, hh, 8:16], in_values=src2[:, hh, :]),
              reads=[ks2, kv], writes=[ki])

    def stage_A(T):
        p = T % 2
        p3 = T % 3
        x1p, h2p, IDXp, GATEp = x1[0], h2[p], IDX[p3], GATE[p3]
        kx, kh, ki_, kg = 'acc', ('h2', p), ('IDX', p3), ('GATE', p3)
        tk.dma('sp', lambda e: e.dma_start(out=x1p, in_=X1S[T * 128:(T + 1) * 128, :]), 'x1l0', reads=[('X1S', T)], writes=[kx])
        tk.op('dve', lambda e: e.tensor_tensor(out=h2p, in0=x1p, in1=sc2p, op=ALU.mult), reads=[kx, 'sc2p'], writes=[kh])
        tk.op('dve', lambda e: e.tensor_tensor(out=h2p, in0=h2p, in1=sh2, op=ALU.add), reads=[kh, 'sh2'], writes=[kh])
        yield
        for k4 in range(4):
            for q in range(4):
                kc = k4 * 4 + q
                tk.op('pe', lambda e, kc=kc, q=q: e.transpose(out=PT[:, q * 128:(q + 1) * 128], in_=h2p[:, kc * 128:(kc + 1) * 128],
                                                             identity=ident), reads=[kh, 'ident'], writes=['pt'], inc=(q == 3))
            for q in range(4):
                kc = k4 * 4 + q
                tk.op('act', lambda e, kc=kc, q=q: e.activation(out=h2T3[:, kc, :], in_=PT[:, q * 128:(q + 1) * 128], func=AF.Copy),
                      reads=['pt'], writes=[('h2T', kc)])
            yield
        h2Tk = [('h2T', kc) for kc in range(16)]
        for hb in range(8):
            tk.dma('sp', lambda e, hb=hb: e.dma_start(out=WQ[0], in_=WS[blk_q(hb)]), 'wq0',
                   reads=[('WS', blk_q(hb))], writes=[('WQ', 0)])
            wv = WQ[0].rearrange("p (k n) -> p k n", n=512)
            for cc in range(4):
                a = st['qi'] % 2
                st['qi'] += 1
                for kc in range(16):
                    tk.op('pe', lambda e, a=a, kc=kc, cc=cc, wv=wv: e.matmul(PQ[a][:, 0:128], lhsT=wv[:, kc, cc * 128:(cc + 1) * 128],
                                                                           rhs=h2T3[:, kc, :], start=(kc == 0), stop=(kc == 15)),
                          reads=h2Tk + [('WQ', 0)], writes=[('ps', a)], inc=(kc == 15))
                tk.op('act', lambda e, a=a, hb=hb, cc=cc: e.activation(out=qT3[:, hb * 4 + cc, :], in_=PQ[a][:, 0:128], func=AF.Copy),
                      reads=[('ps', a)], writes=[('qT', hb * 4 + cc)])
                yield
        qTk = [('qT', i) for i in range(32)]
        for b4 in range(4):
            for h4 in range(4):
                hh = b4 * 4 + h4
                for jc in range(2):
                    tk.op('pe', lambda e, hh=hh, h4=h4, jc=jc: e.matmul(PS3[:, h4 * 128:(h4 + 1) * 128], lhsT=qT3[:, hh * 2 + jc, :],
                                                                      rhs=KT3[:, hh * 2 + jc, :], start=(jc == 0), stop=(jc == 1)),
                          reads=qTk + ['KT_b'], writes=['ps3'], inc=(h4 == 3 and jc == 1))
            tk.op('act', lambda e, b4=b4: e.activation(out=S[:, b4 * 512:(b4 + 1) * 512], in_=PS3, func=AF.Copy),
                  reads=['ps3'], writes=['S'])
            yield
        for hh in range(16):
            top16(V3, I3, S3, S23, hh, ('V16', 'I16', 'S', 'S2'))
            yield
        tk.op('dve', lambda e: e.tensor_tensor(out=C4, in0=V4[:, :, 0, :].unsqueeze(3).to_broadcast([128, 8, 16, 16]),
                                               in1=V4[:, :, 1, :].unsqueeze(2).to_broadcast([128, 8, 16, 16]), op=ALU.add),
              reads=['V16'], writes=['CAND'])
        yield
        for h in range(8):
            top16(SC3, FL3, C3, C23, h, ('SCV', 'FL', 'CAND', 'S2'))
            yield
        tk.op('dve', lambda e: e.tensor_copy(out=ABf[:, 128:256], in_=FL), reads=['FL'], writes=['ABf'])
        tk.op('dve', lambda e: e.tensor_tensor(out=OH.rearrange("p (hk a) -> p hk a", a=16),
                                               in0=ABf[:, 128:256].unsqueeze(2).to_broadcast([128, 128, 16]),
                                               in1=thr16.unsqueeze(1).to_broadcast([128, 128, 16]), op=ALU.is_ge),
              reads=['ABf', 'thr16'], writes=['CAND'])
        tk.op('dve', lambda e: e.tensor_reduce(out=ABf[:, 0:128], in_=OH.rearrange("p (hk a) -> p hk a", a=16), axis=AX.X,
                                               op=ALU.add), reads=['CAND'], writes=['ABf'])
        tk.op('dve', lambda e: e.scalar_tensor_tensor(out=ABf[:, 128:256], in0=ABf[:, 0:128], scalar=-16.0, in1=ABf[:, 128:256],
                                                      op0=ALU.mult, op1=ALU.add), reads=['ABf'], writes=['ABf'])
        tk.op('dve', lambda e: e.tensor_copy(out=I16f, in_=I16), reads=['I16'], writes=['I16f'])
        yield
        for s_ in range(2):
            ab = ABf[:, s_ * 128:(s_ + 1) * 128].rearrange("p (h k) -> p h k", k=16)
            tk.op('dve', lambda e, ab=ab: e.tensor_tensor(out=OH4, in0=ab.unsqueeze(3).to_broadcast([128, 8, 16, 16]),
                                                          in1=iota16.unsqueeze(1).unsqueeze(1).to_broadcast([128, 8, 16, 16]),
                                                          op=ALU.is_equal), reads=['ABf', 'iota16'], writes=['CAND'])
            tk.op('dve', lambda e, s_=s_: e.tensor_tensor(out=OH4, in0=OH4,
                                                          in1=I4f[:, :, s_, :].unsqueeze(2).to_broadcast([128, 8, 16, 16]),
                                                          op=ALU.mult), reads=['CAND', 'I16f'], writes=['CAND'])
            tk.op('dve', lambda e, s_=s_: e.tensor_reduce(out=E12[:, s_ * 128:(s_ + 1) * 128],
                                                          in_=OH.rearrange("p (hk a) -> p hk a", a=16), axis=AX.X, op=ALU.add),
                  reads=['CAND'], writes=['E12'])
            yield
        tk.op('dve', lambda e: e.scalar_tensor_tensor(out=IDXf, in0=E12[:, 0:128], scalar=128.0, in1=E12[:, 128:256],
                                                      op0=ALU.mult, op1=ALU.add), reads=['E12'], writes=['IDXf'])
        tk.op('dve', lambda e: e.tensor_copy(out=IDXp, in_=IDXf), reads=['IDXf'], writes=[ki_])
        G3 = GATEp.rearrange("p (h k) -> p h k", k=16)
        tk.op('dve', lambda e: e.tensor_tensor(out=G3, in0=SC3, in1=SC3[:, :, 0:1].to_broadcast([128, 8, 16]), op=ALU.subtract),
              reads=['SCV'], writes=[kg])
        tk.op('act', lambda e: e.activation(out=GATEp, in_=GATEp, func=AF.Exp), reads=[kg], writes=[kg])
        tk.op('dve', lambda e: e.tensor_reduce(out=sm[:, 0:8], in_=G3, axis=AX.X, op=ALU.add), reads=[kg], writes=['gsum'])
        tk.op('dve', lambda e: e.reciprocal(out=sm[:, 0:8], in_=sm[:, 0:8]), reads=['gsum'], writes=['gsum'])
        tk.op('dve', lambda e: e.tensor_tensor(out=G3, in0=G3, in1=sm[:, 0:8].unsqueeze(2).to_broadcast([128, 8, 16]), op=ALU.mult),
              reads=[kg, 'gsum'], writes=[kg])
        yield

    def u_slot(T, sl):
        p, p3 = T % 2, T % 3
        b = st['gi'] % NB
        st['gi'] += 1
        IDXp, h2p, ACTp = IDX[p3], h2[p], ACT_[p]
        tk.dma('pool', lambda e: e.indirect_dma_start(
            out=UB[b], out_offset=None, in_=eu[:, :], in_offset=bass.IndirectOffsetOnAxis(ap=IDXp[:, sl:sl + 1], axis=0)),
            'ub%d' % b, reads=[('IDX', p3)], writes=[('UB', b)])
        tk.op('dve', lambda e: e.scalar_tensor_tensor(out=UB[b], in0=UB[b], scalar=1.0, in1=h2p, op0=ALU.mult,
                                                      op1=ALU.mult, accum_out=ACTp[:, sl:sl + 1]),
              reads=[('UB', b), ('h2', p)], writes=[('UB', b), ('ACT', p, sl)])

    def u_finish(T):
        p, p3 = T % 2, T % 3
        ACTp, COEFp, GATEp = ACT_[p], COEF[p], GATE[p3]
        tk.op('act', lambda e: e.activation(out=COEFp, in_=ACTp, func=AF.Gelu), reads=[('ACT', p, sl) for sl in range(128)],
              writes=[('COEF', p)])
        tk.op('dve', lambda e: e.tensor_tensor(out=COEFp, in0=COEFp, in1=GATEp, op=ALU.mult),
              reads=[('COEF', p), ('GATE', p3)], writes=[('COEF', p)])

    def v_slot(T, sl):
        p, p3 = T % 2, T % 3
        b = st['gi'] % NB
        st['gi'] += 1
        IDXp, COEFp = IDX[p3], COEF[p]
        tk.dma('pool', lambda e: e.indirect_dma_start(
            out=UB[b], out_offset=None, in_=ev[:, :], in_offset=bass.IndirectOffsetOnAxis(ap=IDXp[:, sl:sl + 1], axis=0)),
            'ub%d' % b, reads=[('IDX', p3)], writes=[('UB', b)])
        if sl % 4 == 3:
            accd = x1[1]
            if sl == 3:
                tk.op('dve', lambda e: e.tensor_scalar(out=accd, in0=UB[b], scalar1=COEFp[:, sl:sl + 1], scalar2=None, op0=ALU.mult),
                      reads=[('UB', b), ('COEF', p)], writes=[('x1', 1)])
            else:
                tk.op('dve', lambda e: e.scalar_tensor_tensor(out=accd, in0=UB[b], scalar=COEFp[:, sl:sl + 1], in1=accd,
                                                              op0=ALU.mult, op1=ALU.add),
                      reads=[('UB', b), ('COEF', p), ('x1', 1)], writes=[('x1', 1)])
            return
        dj = sl % 4
        tk.op('act', lambda e: e.activation(out=DG[dj], in_=ident, func=AF.Copy, scale=COEFp[:, sl:sl + 1]),
              reads=['ident', ('COEF', p)], writes=[('DG', dj)])
        for nb in range(4):
            tk.op('pe', lambda e, nb=nb: e.matmul(PSC[:, nb * 512:(nb + 1) * 512], lhsT=DG[dj],
                                                  rhs=UB[b][:, nb * 512:(nb + 1) * 512], start=(sl == 0), stop=(sl == 126)),
                  reads=[('DG', dj), ('UB', b)], writes=['psc'], inc=(nb == 3))

    def v_finish(T):
        x1f = x1[1]
        tk.op('dve', lambda e: e.tensor_tensor(out=acc, in0=PSC, in1=x1f, op=ALU.add), reads=['psc', ('x1', 1)], writes=['acc'])
        tk.dma('sp', lambda e: e.dma_start(out=x1f, in_=X1S[T * 128:(T + 1) * 128, :]), 'x1l1', reads=[('X1S', T)], writes=[('x1', 1)])
        tk.op('dve', lambda e: e.tensor_tensor(out=acc, in0=acc, in1=g2b, op=ALU.mult), reads=['acc', 'g2b'], writes=['acc'])
        tk.op('dve', lambda e: e.scalar_tensor_tensor(out=acc, in0=x1f, scalar=ALPHA, in1=acc, op0=ALU.mult, op1=ALU.add),
              reads=[('x1', 1), 'acc'], writes=['acc'])
        layer_norm(tk, acc, 'acc', sm, l2g, 'l2g', l2b, 'l2b', PSC, ['psc'])
        tk.dma('sp', lambda e: e.dma_start(out=out[T * 128:(T + 1) * 128, :], in_=acc), 'outst',
               reads=['acc'], writes=[('out', T)])

    for i in range(NT + 2):
        genA = stage_A(i) if i < NT else iter(())
        tu = i - 1 if 0 <= i - 1 < NT else None
        tv = i - 2 if 0 <= i - 2 < NT else None
        if tu is None and tv is None:
            for _ in genA:
                pass
            continue
        for sl in range(128):
            if tv is not None:
                v_slot(tv, sl)
            if tu is not None:
                u_slot(tu, sl)
            if sl % 2 == 1 or (sl % 10 == 0 and sl > 0):
                next(genA, None)
        for _ in genA:
            pass
        if tu is not None:
            u_finish(tu)
        if tv is not None:
            v_finish(tv)


_CACHE = {}


def make_inputs(inputs, core):
    b, half = core // 2, core % 2
    flip = half == 1
    f = np.float32
    x = np.asarray(inputs['x'])[b]
    if flip:
        x = x[::-1]
    xp = np.zeros((4098, 2048), f)
    xp[1:4097] = x
    w_in = np.ascontiguousarray(np.asarray(inputs['w_in'])[0], dtype=f)
    wd = w_in[:, ODT:ODT + 64]
    cw = np.asarray(inputs['conv_ssd_w'])[0]
    scw = np.asarray(inputs['short_conv_w'])[0]
    dbf, dbb = np.asarray(inputs['dt_bias_f'])[0], np.asarray(inputs['dt_bias_b'])[0]
    alf, alb = np.asarray(inputs['a_log_f'])[0], np.asarray(inputs['a_log_b'])[0]
    if flip:
        wd = np.concatenate([wd[:, 32:64], wd[:, 0:32]], axis=1)
        cw = cw[::-1]
        scw = scw[::-1]
        dtb = np.concatenate([dbb, dbf])
        alog = np.concatenate([alb, alf])
    else:
        dtb = np.concatenate([dbf, dbb])
        alog = np.concatenate([alf, alb])

    def bc(v):
        v = np.asarray(v, dtype=f).reshape(1, -1)
        return np.ascontiguousarray(np.broadcast_to(v, (128, v.shape[1])))

    def fm(v):
        return np.ascontiguousarray(np.asarray(v, dtype=f).reshape(16, 128).T)

    sk = np.asarray(inputs['sub_keys'])[0]
    kT = sk.reshape(8, 2, 128, 2, 128).transpose(4, 0, 1, 3, 2).reshape(128, 4096)
    return {
        'xp': xp,
        'cT': fm(np.asarray(inputs['c'])[b]),
        'w_ada': np.ascontiguousarray(np.asarray(inputs['w_ada'])[0], dtype=f),
        'b_ada': np.ascontiguousarray(np.asarray(inputs['b_ada'])[0:1], dtype=f),
        'w_in': w_in,
        'w_dt': np.ascontiguousarray(wd, dtype=f),
        'cwb': bc(np.ascontiguousarray(cw).reshape(-1)),
        'scwb': bc(np.ascontiguousarray(scw).reshape(-1)),
        'cbias': np.ascontiguousarray(np.asarray(inputs['conv_ssd_b'])[0:1], dtype=f),
        'dtb_b': bc(dtb),
        'alog_b': bc(alog),
        'dsk_b': bc(np.asarray(inputs['d_skip'])[0]),
        'ssdgT': fm(np.asarray(inputs['ssd_norm_g'])[0]),
        'scgT': fm(np.asarray(inputs['sc_norm_g'])[0]),
        'w_out': np.ascontiguousarray(np.asarray(inputs['w_out'])[0], dtype=f),
        'ln1g_b': bc(np.asarray(inputs['ln1_g'])[0]),
        'ln1b_b': bc(np.asarray(inputs['ln1_b'])[0]),
        'w_query': np.ascontiguousarray(np.asarray(inputs['w_query'])[0], dtype=f),
        'kT': np.ascontiguousarray(kT, dtype=f),
        'eu': np.ascontiguousarray(np.asarray(inputs['expert_u'])[0], dtype=f),
        'ev': np.ascontiguousarray(np.asarray(inputs['expert_v'])[0], dtype=f),
        'ln2g_b': bc(np.asarray(inputs['ln2_g'])[0]),
        'ln2b_b': bc(np.asarray(inputs['ln2_b'])[0]),
    }


def kernel(_stage=99, _cores=8, **inputs):
    if _stage not in _CACHE:
        _CACHE[_stage] = build(_stage)
    nc = _CACHE[_stage]
    in_maps = [make_inputs(inputs, c) for c in range(_cores)]
    res = run_bass_kernel_spmd(nc, in_maps, core_ids=list(range(_cores)))
    outp = np.zeros((4, 4096, 2048), np.float32)
    for c in range(_cores):
        b, half = c // 2, c % 2
        o = np.asarray(res.results[c]['out'])
        if half == 0:
            outp[b, 0:2048] = o
        else:
            outp[b, 2048:4096] = o[::-1]
    return outp
```

```python
import numpy as np
import concourse.bass as bass
import concourse.mybir as mybir
from concourse.bass_utils import run_bass_kernel_spmd
from contextlib import ExitStack

F32 = mybir.dt.float32
BF16 = mybir.dt.bfloat16
I32 = mybir.dt.int32
U32 = mybir.dt.uint32
AF = mybir.ActivationFunctionType
ALU = mybir.AluOpType
AX = mybir.AxisListType

ENGS = ['pe', 'act', 'dve', 'pool', 'sp']
ALPHA = 2.0 ** 0.25
EPS = 1e-5
NT = 16
NEG = -1.0e30


class Tracker:
    def __init__(self, nc, es):
        self.nc = nc
        self.es = es
        self.stream = {e: [] for e in ENGS}
        self.sems = {}
        self.cnt = {}
        self.seen = {e: {} for e in ENGS}
        self.lastw = {}
        self.readers = {}
        self.chan_keys = {}
        for e in ENGS:
            self._sem('E_' + e)

    def _sem(self, name):
        if name not in self.sems:
            self.sems[name] = self.es.enter_context(self.nc.semaphore(name))
            self.cnt[name] = 0
        return name

    def _deps(self, reads, writes):
        deps = []
        for k in reads:
            if k in self.lastw:
                deps.append(self.lastw[k])
        for k in writes:
            if k in self.lastw:
                deps.append(self.lastw[k])
            deps.extend(self.readers.get(k, {}).items())
        return deps

    def _emit_waits(self, eng, deps, skip_sem=None):
        best = {}
        for (s, v) in deps:
            if s == skip_sem:
                continue
            if v > best.get(s, 0):
                best[s] = v
        for s, v in best.items():
            if self.seen[eng].get(s, 0) < v:
                self.stream[eng].append(('wait', s, v))
                self.seen[eng][s] = v

    def _commit(self, token, reads, writes):
        for k in writes:
            self.lastw[k] = token
            self.readers[k] = {}
        for k in reads:
            d = self.readers.setdefault(k, {})
            if d.get(token[0], 0) < token[1]:
                d[token[0]] = token[1]

    def op(self, eng, fn, reads=(), writes=(), inc=True):
        s = 'E_' + eng
        deps = self._deps(reads, writes)
        self._emit_waits(eng, deps, skip_sem=(s if eng == 'pe' else None))
        if inc:
            self.cnt[s] += 1
            token = (s, self.cnt[s])
        else:
            token = (s, self.cnt[s] + 1)
        self.stream[eng].append(('op', fn, (s, 1) if inc else None))
        self._commit(token, reads, writes)
        return token

    def dma(self, q, fn, chan, reads=(), writes=()):
        s = self._sem('D_' + chan)
        deps = self._deps(reads, writes)
        self._emit_waits(q, deps)
        self.cnt[s] += 16
        token = (s, self.cnt[s])
        self.stream[q].append(('op', fn, (s, 16)))
        self._commit(token, reads, writes)
        self.chan_keys.setdefault(chan, set()).update(writes)
        return token

    def seal(self, chan):
        s = 'D_' + chan
        if s not in self.cnt:
            return
        tok = (s, self.cnt[s])
        for k in self.chan_keys.get(chan, ()):
            if k in self.lastw and self.lastw[k][0] == s:
                self.lastw[k] = tok

    def barrier(self):
        for e in ENGS:
            for s, c in self.cnt.items():
                if c > 0 and self.seen[e].get(s, 0) < c:
                    self.stream[e].append(('wait', s, c))
                    self.seen[e][s] = c

    def getreg(self, eng, val):
        if not hasattr(self, '_regs'):
            self._regs = {}
        if val not in self._regs:
            self._regs[val] = eng.to_reg(val)
        return self._regs[val]

    def emit(self):
        nc = self.nc
        tk = self

        def run(engname):
            def body(eng):
                for it in tk.stream[engname]:
                    if it[0] == 'wait':
                        eng.wait_ge(tk.sems[it[1]], it[2])
                    else:
                        ins = it[1](eng)
                        if it[2] is not None:
                            ins.then_inc(tk.sems[it[2][0]], it[2][1])
            return body

        with nc.Block() as block:
            block.tensor(run('pe'))
            block.scalar(run('act'))
            block.vector(run('dve'))
            block.gpsimd(run('pool'))
            block.sync(run('sp'))


class Arena:
    def __init__(self, ap):
        self.ap = ap
        self.off = 0
        self.N = ap.shape[1]

    def f32(self, n):
        a = self.ap[:, self.off:self.off + n]
        self.off += n
        assert self.off <= self.N, ("arena overflow", self.off, self.N)
        return a

    def bf16(self, n):
        m = (n + 1) // 2
        return self.f32(m).bitcast(BF16)

    def i32(self, n):
        return self.f32(n).bitcast(I32)

    def u32(self, n):
        return self.f32(n).bitcast(U32)


OZ, OXBC, ODT, OGB, OGC, OV = 0, 2048, 5120, 5184, 7232, 9280
NBLK = 58


def blk_xbc(cb, tap):
    return cb * 3 + tap


def blk_z(g):
    return 18 + g


def blk_sc(j, k):
    return 22 + j * 5 + k


def blk_out(nb, hf):
    return 42 + nb * 2 + hf


def blk_q(hb):
    return 50 + hb


def build(stage=99):
    import os
    SUB = int(os.environ.get('K_SUB', '9'))
    DBG = int(os.environ.get('K_DBG', '0'))
    nc = bass.Bass("TRN2", target_bir_lowering=False)

    def din(name, shape, dt=F32):
        return nc.dram_tensor(name, shape, dt, kind="ExternalInput").ap()

    xp = din("xp", [4098, 2048])
    cT = din("cT", [128, 16])
    w_ada = din("w_ada", [2048, 12288])
    b_ada = din("b_ada", [1, 12288])
    w_in = din("w_in", [2048, 11328])
    w_dt = din("w_dt", [2048, 64])
    cwb = din("cwb", [128, 9216])
    scwb = din("scwb", [128, 6144])
    cbias = din("cbias", [1, 3072])
    dtb_b = din("dtb_b", [128, 64])
    alog_b = din("alog_b", [128, 64])
    dsk_b = din("dsk_b", [128, 32])
    ssdgT = din("ssdgT", [128, 16])
    scgT = din("scgT", [128, 16])
    w_out = din("w_out", [4096, 2048])
    ln1g_b = din("ln1g_b", [128, 2048])
    ln1b_b = din("ln1b_b", [128, 2048])
    w_query = din("w_query", [2048, 4096])
    kT = din("kT", [128, 4096])
    eu = din("eu", [16384, 2048])
    ev = din("ev", [16384, 2048])
    ln2g_b = din("ln2g_b", [128, 2048])
    ln2b_b = din("ln2b_b", [128, 2048])
    out = nc.dram_tensor("out", [2048, 2048], F32, kind="ExternalOutput").ap()
    WS = nc.dram_tensor("WS", [NBLK, 128, 16 * 512], BF16, kind="Internal").ap()
    modd = nc.dram_tensor("modd", [1, 12288], F32, kind="Internal").ap()
    HFS = nc.dram_tensor("HFS", [NT, 128, 2048], BF16, kind="Internal").ap()
    X1S = nc.dram_tensor("X1S", [2048, 2048], F32, kind="Internal").ap()
    EUb = nc.dram_tensor("EUb", [16384, 2048], BF16, kind="Internal").ap()
    EVb = nc.dram_tensor("EVb", [16384, 2048], BF16, kind="Internal").ap()

    es = ExitStack()
    with es:
        tk = Tracker(nc, es)
        ARN = es.enter_context(nc.sbuf_tensor("arena", [128, 52900], F32))
        PSM = es.enter_context(nc.psum_tensor("psm", [128, 4096], F32))
        ar = Arena(ARN[:, :])

        def bank(i, n=512):
            return PSM[:, i * 512:i * 512 + n]

        ident = ar.f32(128)
        ones_f = ar.f32(128)
        triF = ar.f32(128)
        triR = ar.f32(128)
        ones_b = ar.bf16(128)
        sc1pT = ar.f32(16)
        sh1T = ar.f32(16)
        dtb = ar.f32(64)
        a_neg = ar.f32(64)
        dsk = ar.f32(32)
        ssdg = ar.f32(16)
        scg = ar.f32(16)
        iota16 = ar.f32(16)
        bias2 = ar.bf16(3072)
        wdt_sb = ar.bf16(16 * 64)
        PERSIST = ar.off

        tk.op('pool', lambda e: e.memset(ident, 0.0), writes=['ident'])
        tk.op('pool', lambda e: e.affine_select(out=ident, in_=ident, pattern=[[-1, 128]], compare_op=ALU.not_equal,
                                                fill=tk.getreg(e, 1.0), base=0, channel_multiplier=1), reads=['ident'], writes=['ident'])
        tk.op('pool', lambda e: e.memset(ones_f, 1.0), writes=['ones_f'])
        tk.op('pool', lambda e: e.memset(ones_b, 1.0), writes=['ones_b'])
        tk.op('pool', lambda e: e.memset(triF, 1.0), writes=['triF'])
        tk.op('pool', lambda e: e.affine_select(out=triF, in_=triF, pattern=[[1, 128]], compare_op=ALU.is_ge,
                                                fill=tk.getreg(e, 0.0), base=0, channel_multiplier=-1), reads=['triF'], writes=['triF'])
        tk.op('pool', lambda e: e.memset(triR, 1.0), writes=['triR'])
        tk.op('pool', lambda e: e.affine_select(out=triR, in_=triR, pattern=[[-1, 128]], compare_op=ALU.is_ge,
                                                fill=tk.getreg(e, 0.0), base=0, channel_multiplier=1), reads=['triR'], writes=['triR'])
        tk.op('pool', lambda e: e.iota(iota16, pattern=[[1, 16]], base=0, channel_multiplier=0,
                                       allow_small_or_imprecise_dtypes=True), writes=['iota16'])
        for (dst, src, nm) in [(dtb, dtb_b, 'dtb'), (dsk, dsk_b, 'dsk'), (ssdg, ssdgT, 'ssdg'), (scg, scgT, 'scg')]:
            tk.dma('sp', lambda e, dst=dst, src=src: e.dma_start(out=dst, in_=src[:, :]), 'par', writes=[nm])
        tk.dma('sp', lambda e: e.dma_start(out=a_neg, in_=alog_b[:, :]), 'par', writes=['a_neg'])
        tk.seal('par')
        tk.op('act', lambda e: e.activation(out=a_neg, in_=a_neg, func=AF.Exp), reads=['a_neg'], writes=['a_neg'])
        tk.op('dve', lambda e: e.tensor_scalar(out=a_neg, in0=a_neg, scalar1=-1.0, scalar2=None, op0=ALU.mult),
              reads=['a_neg'], writes=['a_neg'])

        P0 = ar.off
        cs_ = ar.f32(16)
        bada = ar.f32(12288)
        wa = [ar.f32(4096), ar.f32(4096)]
        stg = ar.f32(4096)
        tk.dma('sp', lambda e: e.dma_start(out=cs_, in_=cT[:, :]), 'p0', writes=['cs_'])
        tk.dma('sp', lambda e: e.dma_start(out=bada[0:1, :], in_=b_ada[:, :]), 'p0', writes=['bada'])
        tk.seal('p0')
        tk.op('act', lambda e: e.activation(out=cs_, in_=cs_, func=AF.Silu), reads=['cs_'], writes=['cs_'])
        it = 0
        for g in range(3):
            for kc in range(16):
                b = it % 2
                it += 1
                tk.dma('sp', lambda e, b=b, kc=kc, g=g: e.dma_start(
                    out=wa[b], in_=w_ada[kc * 128:(kc + 1) * 128, g * 4096:(g + 1) * 4096]), 'wa%d' % b, writes=[('wa', b)])
                for j in range(8):
                    tk.op('pe', lambda e, b=b, kc=kc, j=j: e.matmul(bank(j)[0:1, :], lhsT=cs_[:, kc:kc + 1],
                                                                   rhs=wa[b][:, j * 512:(j + 1) * 512],
                                                                   start=(kc == 0), stop=(kc == 15)),
                          reads=['cs_', ('wa', b)], writes=[('ps', j)], inc=(j == 7))
            for j in range(8):
                tk.op('dve', lambda e, j=j, g=g: e.tensor_tensor(out=stg[0:1, j * 512:(j + 1) * 512], in0=bank(j)[0:1, :],
                                                                 in1=bada[0:1, g * 4096 + j * 512:g * 4096 + (j + 1) * 512],
                                                                 op=ALU.add),
                      reads=[('ps', j), 'bada'], writes=['stg'])
            tk.dma('sp', lambda e, g=g: e.dma_start(out=modd[0:1, g * 4096:(g + 1) * 4096], in_=stg[0:1, :]), 'modst',
                   reads=['stg'], writes=['modd'])
        tk.seal('modst')
        t16 = ar.f32(128)
        for (dst, off, nm, addone) in [(sh1T, 0, 'sh1T', False), (sc1pT, 2048, 'sc1pT', True)]:
            tk.dma('sp', lambda e, off=off: e.dma_start(out=t16[0:16, :],
                                                        in_=modd[0, off:off + 2048].rearrange("(k p) -> k p", p=128)),
                   'm16', reads=['modd'], writes=['t16'])
            tk.op('pe', lambda e: e.transpose(out=bank(0)[:, 0:16], in_=t16[0:16, :], identity=ident[0:16, 0:16]),
                  reads=['t16', 'ident'], writes=[('ps', 0)])
            if addone:
                tk.op('dve', lambda e, dst=dst: e.tensor_scalar(out=dst, in0=bank(0)[:, 0:16], scalar1=1.0, scalar2=None,
                                                                op0=ALU.add), reads=[('ps', 0)], writes=[nm])
            else:
                tk.op('dve', lambda e, dst=dst: e.tensor_copy(out=dst, in_=bank(0)[:, 0:16]), reads=[('ps', 0)], writes=[nm])
        cb32 = ar.f32(3072)
        cbt = ar.f32(3072)
        tk.dma('sp', lambda e: e.dma_start(out=cb32[0:1, :], in_=cbias[:, :]), 'p0b', writes=['cb32'])
        tk.op('dve', lambda e: e.tensor_copy(out=bias2[0:1, :], in_=cb32[0:1, :]), reads=['cb32'], writes=['bias2'])
        tk.op('dve', lambda e: e.tensor_tensor(out=cbt[0:1, :], in0=cb32[0:1, :], in1=bias2[0:1, :], op=ALU.subtract),
              reads=['cb32', 'bias2'], writes=['cbt'])
        lo_d = nc.dram_tensor("lo_d", [1, 3072], BF16, kind="Internal").ap()
        lo16 = ar.bf16(3072)
        tk.op('dve', lambda e: e.tensor_copy(out=lo16[0:1, :], in_=cbt[0:1, :]), reads=['cbt'], writes=['lo16'])
        tk.dma('sp', lambda e: e.dma_start(out=lo_d[:, :], in_=lo16[0:1, :]), 'p0c', reads=['lo16'], writes=['lo_d'])
        tk.dma('sp', lambda e: e.dma_start(out=bias2[1:2, :], in_=lo_d[:, :]), 'p0d', reads=['lo_d', 'bias2'], writes=['bias2'])
        wdt32 = ar.f32(16 * 64)
        tk.dma('sp', lambda e: e.dma_start(out=wdt32.rearrange("p (k n) -> p k n", n=64),
                                           in_=w_dt.rearrange("(k p) n -> p k n", p=128)), 'p0e', writes=['wdt32'])
        tk.op('dve', lambda e: e.tensor_copy(out=wdt_sb, in_=wdt32), reads=['wdt32'], writes=['wdt_sb'])

        tk.barrier()
        ar.off = PERSIST
        cw_sb = ar.f32(9216)
        scw_sb = ar.f32(6144)
        wst = [ar.f32(8192), ar.f32(8192)]
        wcs = [ar.bf16(8192), ar.bf16(8192)]
        tk.dma('sp', lambda e: e.dma_start(out=cw_sb, in_=cwb[:, :]), 'pa', writes=['cw_sb'])
        tk.dma('sp', lambda e: e.dma_start(out=scw_sb, in_=scwb[:, :]), 'pa', writes=['scw_sb'])
        tk.seal('pa')
        bases = []
        for cb in range(6):
            bases.append((w_in[:, OXBC + cb * 512:OXBC + (cb + 1) * 512],
                          [(blk_xbc(cb, t), cw_sb[:, t * 3072 + cb * 512:t * 3072 + (cb + 1) * 512]) for t in range(3)]))
        for g in range(4):
            bases.append((w_in[:, OZ + g * 512:OZ + (g + 1) * 512], [(blk_z(g), None)]))
        for j in range(4):
            bases.append((w_in[:, OGB + j * 512:OGB + (j + 1) * 512], [(blk_sc(j, 0), None)]))
            bases.append((w_in[:, OGC + j * 512:OGC + (j + 1) * 512],
                          [(blk_sc(j, 1 + t), scw_sb[:, t * 2048 + j * 512:t * 2048 + (j + 1) * 512]) for t in range(3)]))
            bases.append((w_in[:, OV + j * 512:OV + (j + 1) * 512], [(blk_sc(j, 4), None)]))
        for nb in range(4):
            for hf in range(2):
                bases.append((w_out[hf * 2048:(hf + 1) * 2048, nb * 512:(nb + 1) * 512], [(blk_out(nb, hf), None)]))
        for hb in range(8):
            bases.append((w_query[:, hb * 512:(hb + 1) * 512], [(blk_q(hb), None)]))
        ci = 0
        for bi, (src, ders) in enumerate(bases if stage >= 1 else []):
            sb_ = bi % 2
            n_ = src.shape[1]
            tk.dma('sp', lambda e, sb_=sb_, src=src, n_=n_: e.dma_start(out=wst[sb_].rearrange("p (k n) -> p k n", n=n_),
                                                                 in_=src.rearrange("(k p) n -> p k n", p=128)),
                   'wst%d' % sb_, writes=[('wst', sb_)])
            for (bid, scl) in ders:
                cbuf = ci % 2
                ci += 1
                if scl is None:
                    eng = ['act', 'dve', 'pool'][ci % 3] if isinstance(bid, tuple) else 'act'
                    if eng == 'act':
                        tk.op('act', lambda e, sb_=sb_, cbuf=cbuf: e.activation(out=wcs[cbuf], in_=wst[sb_], func=AF.Copy),
                              reads=[('wst', sb_)], writes=[('wcs', cbuf)])
                    else:
                        tk.op(eng, lambda e, sb_=sb_, cbuf=cbuf: e.tensor_copy(out=wcs[cbuf], in_=wst[sb_]),
                              reads=[('wst', sb_)], writes=[('wcs', cbuf)])
                else:
                    eng = 'dve' if (ci % 2 == 0) else 'pool'
                    tk.op(eng, lambda e, sb_=sb_, cbuf=cbuf, scl=scl: e.tensor_tensor(
                        out=wcs[cbuf].rearrange("p (k n) -> p k n", n=512),
                        in0=wst[sb_].rearrange("p (k n) -> p k n", n=512),
                        in1=scl.unsqueeze(1).to_broadcast([128, 16, 512]), op=ALU.mult),
                        reads=[('wst', sb_), 'cw_sb', 'scw_sb'], writes=[('wcs', cbuf)])
                if isinstance(bid, tuple):
                    dst, nm = bid
                    tk.dma('act', lambda e, dst=dst, cbuf=cbuf, n_=n_: e.dma_start(
                        out=dst.rearrange("(k p) n -> p k n", p=128), in_=wcs[cbuf].rearrange("p (k n) -> p k n", n=n_)), 'wsst%d' % cbuf,
                        reads=[('wcs', cbuf)], writes=[nm])
                else:
                    tk.dma('act', lambda e, bid=bid, cbuf=cbuf: e.dma_start(out=WS[bid], in_=wcs[cbuf]), 'wsst%d' % cbuf,
                           reads=[('wcs', cbuf)], writes=[('WS', bid)])
        tk.seal('wsst0')
        tk.seal('wsst1')

        tk.barrier()
        ar.off = PERSIST
        NWB = 2
        WB = [ar.bf16(8192) for _ in range(NWB)]
        XT = [ar.f32(2048), ar.f32(2048)]
        XH = [ar.f32(256), ar.f32(256)]
        hT = [ar.bf16(16 * 130) for _ in range(4)]
        xbc0 = ar.f32(3072)
        xbc1 = ar.f32(3072)
        cur = {'j': 0, 'xb': xbc0}
        ysum = ar.f32(2048)
        xd_b = ar.bf16(2048)
        xdd_b = ar.bf16(2048)
        B_b = ar.bf16(512)
        BT_b = ar.bf16(512)
        CT_b = ar.bf16(512)
        HR = ar.f32(2048)
        HRb = ar.bf16(2048)
        HFb = ar.bf16(2048)
        off_x3 = ar.off
        X3 = ar.f32(1024)
        D3 = ar.f32(1024)
        E3 = ar.f32(1024)
        MT_b = ar.bf16(1024)
        yT = ar.bf16(32 * 128)
        R6 = ar.f32(6144)
        g1b, l1g, l1b = R6[:, 0:2048], R6[:, 2048:4096], R6[:, 4096:6144]
        xbcG = [xbc0, xbc1, R6[:, 0:3072], R6[:, 3072:6144]]
        off_ta = ar.off
        tA = ar.f32(512)
        tB = ar.f32(512)
        vt = [ar.f32(512) for _ in range(3)]
        u_t = ar.f32(2048)
        dt64 = ar.f32(64)
        dtA = ar.f32(64)
        sm = ar.f32(512)
        cs32, dst32, ecs32, cdec32, w232 = sm[:, 0:32], sm[:, 32:64], sm[:, 64:96], sm[:, 96:128], sm[:, 128:160]
        t32 = sm[:, 160:192]
        st8 = sm[:, 192:208]
        rs8 = sm[:, 208:224]
        e64 = sm[:, 256:320]

        PA, PB, PT, PS_, PY, PO = bank(0), bank(1), bank(2), bank(3), bank(4), bank(5)
        PC = PSM[:, 6 * 512:8 * 512]
        pacc = [PA, PB]
        pacc_k = [('ps', 0), ('ps', 1)]
        gacc = [PA, PB, PY, PO]
        gacc_k = [('ps', 0), ('ps', 1), 'py', 'po']
        state = {'wi': 0, 'acc': 0, 'hb': 0}

        class WStream:
            def __init__(self, seq):
                self.seq = seq
                self.issued = 0
                self.pos = 0

            def _issue(self):
                if self.issued < len(self.seq):
                    bid = self.seq[self.issued]
                    slot = state['wi'] % NWB
                    state['wi'] += 1
                    tk.dma('sp', lambda e, bid=bid, slot=slot: e.dma_start(out=WB[slot], in_=WS[bid]), 'wb%d' % slot,
                           reads=[('WS', bid)], writes=[('WB', slot)])
                    self.slots = getattr(self, 'slots', []) + [slot]
                    self.issued += 1

            def next(self):
                while self.issued < min(self.pos + NWB, len(self.seq)):
                    self._issue()
                slot = self.slots[self.pos]
                self.pos += 1
                return WB[slot].rearrange("p (k n) -> p k n", n=512), ('WB', slot)

        def load_tile(pos0, zero_lo, zero_hi, xt=None, ht=None):
            xb_ = xt
            hb = ht
            tk.dma('sp', lambda e: e.dma_start(out=XT[xb_], in_=xp[pos0 + 1:pos0 + 129, :]), 'xt%d' % xb_, writes=[('XT', xb_)])
            tk.dma('sp', lambda e: e.dma_start(out=XH[xb_][0:16, 0:128], in_=xp[pos0, :].rearrange("(k p) -> k p", p=128)),
                   'xh%d' % xb_, writes=[('XH', xb_)])
            tk.dma('sp', lambda e: e.dma_start(out=XH[xb_][0:16, 128:256], in_=xp[pos0 + 129, :].rearrange("(k p) -> k p", p=128)),
                   'xh%d' % xb_, writes=[('XH', xb_)])
            h3 = hT[hb].rearrange("p (k t) -> p k t", t=130)
            for k4 in range(4):
                for q in range(4):
                    kc = k4 * 4 + q
                    tk.op('pe', lambda e, kc=kc, q=q: e.transpose(out=PT[:, q * 128:(q + 1) * 128],
                                                                 in_=XT[xb_][:, kc * 128:(kc + 1) * 128], identity=ident),
                          reads=[('XT', xb_), 'ident'], writes=['pt'], inc=(q == 3))
                for q in range(4):
                    kc = k4 * 4 + q
                    tk.op('act', lambda e, kc=kc, q=q: e.activation(out=h3[:, kc, 1:129], in_=PT[:, q * 128:(q + 1) * 128],
                                                                   func=AF.Identity, scale=sc1pT[:, kc:kc + 1],
                                                                   bias=sh1T[:, kc:kc + 1]),
                          reads=['pt', 'sc1pT', 'sh1T'], writes=[('hT', hb, kc)])
            for hi in range(2):
                tk.op('pe', lambda e, hi=hi: e.transpose(out=PS_[:, 256 + hi * 16:256 + hi * 16 + 16],
                                                        in_=XH[xb_][0:16, hi * 128:(hi + 1) * 128], identity=ident[0:16, 0:16]),
                      reads=[('XH', xb_), 'ident'], writes=['ps_'], inc=(hi == 1))
            tk.op('dve', lambda e: e.tensor_tensor(out=t32.rearrange("p (t k) -> p t k", k=16),
                                                   in0=PS_[:, 256:288].rearrange("p (t k) -> p t k", k=16),
                                                   in1=sc1pT.unsqueeze(1).to_broadcast([128, 2, 16]), op=ALU.mult),
                  reads=['ps_', 'sc1pT'], writes=['t32'])
            tk.op('dve', lambda e: e.tensor_tensor(out=h3[:, :, 0:130:129].rearrange("p k t -> p t k"),
                                                   in0=t32.rearrange("p (t k) -> p t k", k=16),
                                                   in1=sh1T.unsqueeze(1).to_broadcast([128, 2, 16]), op=ALU.add),
                  reads=['t32', 'sh1T'], writes=[('hTh', hb)])
            if zero_lo:
                tk.op('dve', lambda e: e.memset(h3[:, :, 0:1], 0.0), reads=[('hTh', hb)], writes=[('hTh', hb)])
            if zero_hi:
                tk.op('dve', lambda e: e.memset(h3[:, :, 129:130], 0.0), reads=[('hTh', hb)], writes=[('hTh', hb)])
            return hb, h3

        def hkeys(hb):
            return [('hT', hb, kc) for kc in range(16)] + [('hTh', hb)]

        def proj(ps, pskey, hb, h3, taps_blocks, ncols=512, bias_cols=None, first=True, last=True):
            n = len(taps_blocks)
            for ti, (tap, wv, wkey) in enumerate(taps_blocks):
                for kc in range(16):
                    st = first and ti == 0 and kc == 0
                    sp_ = last and (bias_cols is None) and ti == n - 1 and kc == 15
                    tk.op('pe', lambda e, tap=tap, wv=wv, kc=kc, st=st, sp_=sp_: e.matmul(
                        ps[:, 0:ncols], lhsT=h3[:, kc, tap:tap + 128], rhs=wv[:, kc, 0:ncols], start=st, stop=sp_),
                        reads=hkeys(hb) + [wkey], writes=[pskey], inc=(kc == 15))
            if bias_cols is not None:
                c0 = bias_cols
                tk.op('pe', lambda e: e.matmul(ps[:, 0:ncols], lhsT=ones_b[0:2, :], rhs=bias2[0:2, c0:c0 + ncols],
                                               start=False, stop=True),
                      reads=['ones_b', 'bias2'], writes=[pskey])

        wdt3 = wdt_sb.rearrange("p (k n) -> p k n", n=64)

        def xbc_blocks_group(ws, infos, cbs, hook=None):
            for cb in cbs:
                for tap in range(3):
                    wv, wkey = ws.next()
                    for j, (hb, h3) in enumerate(infos):
                        proj(gacc[j], gacc_k[j], hb, h3, [(tap, wv, wkey)], bias_cols=(cb * 512 if tap == 2 else None),
                             first=(tap == 0), last=(tap == 2))
                    if hook is not None:
                        hook()
                for j, (hb, h3) in enumerate(infos):
                    tk.op('act', lambda e, j=j, cb=cb: e.activation(out=xbcG[j][:, cb * 512:(cb + 1) * 512], in_=gacc[j], func=AF.Silu),
                          reads=[gacc_k[j]], writes=[('xbc', j, cb)])

        def dt_for_tile(hb, h3):
            a = state['acc'] % 2
            state['acc'] += 1
            proj(pacc[a], pacc_k[a], hb, h3, [(1, wdt3, 'wdt_sb')], ncols=64)
            tk.op('dve', lambda e, a=a: e.tensor_tensor(out=dt64, in0=pacc[a][:, 0:64], in1=dtb, op=ALU.add),
                  reads=[pacc_k[a], 'dtb'], writes=['dt64'])
            tk.op('act', lambda e: e.activation(out=e64, in_=dt64, func=AF.Exp), reads=['dt64'], writes=['e64'])
            tk.op('act', lambda e: e.activation(out=dt64, in_=e64, func=AF.Ln, bias=1.0), reads=['e64'], writes=['dt64'])
            tk.op('dve', lambda e: e.tensor_tensor(out=dtA, in0=dt64, in1=a_neg, op=ALU.mult),
                  reads=['dt64', 'a_neg'], writes=['dtA'])

        def chunk_scalars(dr, full):
            xbc = cur['xb']
            jj = cur['j']
            sl = slice(dr * 32, dr * 32 + 32)
            tri, trik = (triF, 'triF') if dr == 0 else (triR, 'triR')
            tk.op('pe', lambda e: e.matmul(PS_[:, 0:32], lhsT=tri, rhs=dtA[:, sl], start=True, stop=True),
                  reads=[trik, 'dtA'], writes=['ps_'], inc=False)
            tk.op('pe', lambda e: e.matmul(PS_[:, 32:64], lhsT=ones_f, rhs=dtA[:, sl], start=True, stop=True),
                  reads=['ones_f', 'dtA'], writes=['ps_'])
            tk.op('dve', lambda e: e.tensor_copy(out=cs32, in_=PS_[:, 0:32]), reads=['ps_'], writes=['cs32'])
            tk.op('dve', lambda e: e.tensor_tensor(out=dst32, in0=PS_[:, 32:64], in1=cs32, op=ALU.subtract),
                  reads=['ps_', 'cs32'], writes=['dst32'])
            tk.op('act', lambda e: e.activation(out=dst32, in_=dst32, func=AF.Exp), reads=['dst32'], writes=['dst32'])
            tk.op('act', lambda e: e.activation(out=cdec32, in_=PS_[:, 32:64], func=AF.Exp), reads=['ps_'], writes=['cdec32'])
            if full:
                tk.op('act', lambda e: e.activation(out=ecs32, in_=cs32, func=AF.Exp), reads=['cs32'], writes=['ecs32'])
            tk.op('dve', lambda e: e.tensor_tensor(out=w232, in0=dt64[:, sl], in1=dst32, op=ALU.mult),
                  reads=['dt64', 'dst32'], writes=['w232'])
            xs3 = xbc[:, 0:2048].rearrange("p (h d) -> p h d", d=64)
            tk.op('pool', lambda e: e.tensor_tensor(out=xdd_b.rearrange("p (h d) -> p h d", d=64), in0=xs3,
                                                    in1=w232.unsqueeze(2).to_broadcast([128, 32, 64]), op=ALU.mult),
                  reads=[('xbc', jj, i) for i in range(4)] + ['w232'], writes=['xdd_b'])
            if full:
                tk.op('pool', lambda e: e.tensor_tensor(out=xd_b.rearrange("p (h d) -> p h d", d=64), in0=xs3,
                                                        in1=dt64[:, sl].unsqueeze(2).to_broadcast([128, 32, 64]), op=ALU.mult),
                      reads=[('xbc', jj, i) for i in range(4)] + ['dt64'], writes=['xd_b'])

        def state_update(H, Hk):
            for g in range(4):
                tk.op('pe', lambda e, g=g: e.matmul(PO, lhsT=B_b[:, g * 128:(g + 1) * 128], rhs=xdd_b[:, g * 512:(g + 1) * 512],
                                                    start=True, stop=True),
                      reads=['B_b', 'xdd_b'], writes=['po'])
                Hg = H[:, g * 512:(g + 1) * 512].rearrange("p (h d) -> p h d", d=64)
                tk.op('dve', lambda e, g=g, Hg=Hg: e.tensor_tensor(out=Hg, in0=Hg,
                                                                   in1=cdec32[:, g * 8:(g + 1) * 8].unsqueeze(2).to_broadcast([128, 8, 64]),
                                                                   op=ALU.mult),
                      reads=[(Hk, g), 'cdec32'], writes=[(Hk, g)])
                tk.op('dve', lambda e, g=g: e.tensor_tensor(out=H[:, g * 512:(g + 1) * 512], in0=H[:, g * 512:(g + 1) * 512],
                                                            in1=PO, op=ALU.add),
                      reads=[(Hk, g), 'po'], writes=[(Hk, g)])

        def make_Bb():
            xbc = cur['xb']
            tk.op('act', lambda e: e.activation(out=B_b, in_=xbc[:, 2048:2560], func=AF.Copy), reads=[('xbc', cur['j'], 4)], writes=['B_b'])

        Hkeys = [('HR', g) for g in range(4)]

        cin = [ARN[:, off_x3:off_x3 + 2048], ARN[:, off_x3 + 2048:off_x3 + 4096]]
        cout = [ARN[:, off_ta:off_ta + 1024].bitcast(BF16), ARN[:, off_ta + 1024:off_ta + 2048].bitcast(BF16)]

        def conv_gen():
            def src(k):
                tab = eu if k < 128 else ev
                r0 = (k % 128) * 128
                return tab[r0:r0 + 128, :]

            def dst(k):
                tab = EUb if k < 128 else EVb
                r0 = (k % 128) * 128
                return tab[r0:r0 + 128, :], ('EUb' if k < 128 else 'EVb')

            def load(k):
                i = k % 2
                sk = src(k)
                tk.dma('pool', lambda e: e.dma_start(out=cin[i], in_=sk), 'cvl%d' % i, writes=[('cin', i)])
            load(0)
            for k in range(256):
                if k + 1 < 256:
                    load(k + 1)
                i = k % 2
                tk.op('dve', lambda e, i=i: e.tensor_copy(out=cout[i], in_=cin[i]), reads=[('cin', i)], writes=[('cout', i)])
                dk, nm = dst(k)
                tk.dma('pool', lambda e, i=i, dk=dk: e.dma_start(out=dk, in_=cout[i]), 'cvs%d' % i, reads=[('cout', i)], writes=[nm])
                yield

        cvg = conv_gen()

        def state_sweep(tiles, dr, H, Hk, spill):
            groups = [tiles[i:i + 4] for i in range(0, len(tiles), 4)]
            seq = []
            for _ in groups:
                for cb in range(5):
                    for tap in range(3):
                        seq.append(blk_xbc(cb, tap))
            ws = WStream(seq)
            for grp in groups:
                infos = []
                for j, T in enumerate(grp):
                    infos.append(load_tile(T * 128, T == 0, T == 31, xt=j % 2, ht=j))
                xbc_blocks_group(ws, infos, range(5), hook=lambda: [next(cvg, None) for _c in range(3)])
                for j, T in enumerate(grp):
                    if spill:
                        tk.op('act', lambda e: e.activation(out=HFb, in_=H, func=AF.Copy), reads=[(Hk, g) for g in range(4)], writes=['HFb'])
                        tk.dma('sp', lambda e, T=T: e.dma_start(out=HFS[T], in_=HFb), 'hfs', reads=['HFb'], writes=[('HFS', T)])
                    cur['j'] = j
                    cur['xb'] = xbcG[j]
                    dt_for_tile(*infos[j])
                    make_Bb()
                    chunk_scalars(dr, False)
                    state_update(H, Hk)

        tk.op('dve', lambda e: e.memset(HR, 0.0), writes=Hkeys)
        if stage >= 2:
            state_sweep(list(range(31, 15, -1)), 1, HR, 'HR', False)
        HF = ysum
        HFk = [('HF', g) for g in range(4)]
        tk.op('dve', lambda e: e.memset(HF, 0.0), writes=HFk)
        if stage >= 2:
            state_sweep(list(range(15)), 0, HF, 'HF', True)
            tk.op('act', lambda e: e.activation(out=HFb, in_=HF, func=AF.Copy), reads=HFk, writes=['HFb'])
            tk.dma('sp', lambda e: e.dma_start(out=HFS[15], in_=HFb), 'hfs', reads=['HFb'], writes=[('HFS', 15)])
        tk.seal('hfs')
        for _ in cvg:
            pass
        tk.barrier()
        tk.dma('sp', lambda e: e.dma_start(out=g1b, in_=modd[0:1, 4096:6144].partition_broadcast(128)), 'pm',
               reads=['modd'], writes=['g1b'])
        tk.dma('sp', lambda e: e.dma_start(out=l1g, in_=ln1g_b[:, :]), 'pm', writes=['l1g'])
        tk.dma('sp', lambda e: e.dma_start(out=l1b, in_=ln1b_b[:, :]), 'pm', writes=['l1b'])
        tk.seal('pm')

        seqC = []
        for pi in range(8):
            for cb in range(6):
                for tap in range(3):
                    seqC.append(blk_xbc(cb, tap))
            for _t in range(2):
                for j in range(4):
                    seqC += [blk_sc(j, 4), blk_sc(j, 1), blk_sc(j, 2), blk_sc(j, 3), blk_sc(j, 0)]
                for g in range(4):
                    seqC.append(blk_z(g))
                for nb in range(4):
                    seqC += [blk_out(nb, 0), blk_out(nb, 1)]
        ws = WStream(seqC)
        yT3 = yT.rearrange("p (k t) -> p k t", t=128)

        def transposes_to_yT(src, srckey, kbase, gT):
            for cc in range(4):
                q = cc
                tk.op('pe', lambda e, cc=cc, q=q: e.transpose(out=PT[:, q * 128:(q + 1) * 128],
                                                             in_=src[:, cc * 128:(cc + 1) * 128], identity=ident),
                      reads=[srckey, 'ident'], writes=['pt'], inc=(q == 3))
            for cc in range(4):
                q = cc
                col = kbase + cc - (kbase // 16) * 16
                tk.op('act', lambda e, cc=cc, q=q, col=col: e.activation(out=yT3[:, kbase + cc, :], in_=PT[:, q * 128:(q + 1) * 128],
                                                                        func=AF.Copy, scale=gT[:, col:col + 1]),
                      reads=['pt', 'ssdg', 'scg'], writes=[('yT', kbase + cc)])

        def rstd_from_ss(ss_ap, n_elems, width, key_in, key_out, out_ap):
            tk.op('dve', lambda e: e.tensor_scalar(out=out_ap, in0=ss_ap, scalar1=1.0 / n_elems, scalar2=EPS, op0=ALU.mult,
                                                   op1=ALU.add), reads=[key_in], writes=[key_out])
            tk.op('act', lambda e: e.activation(out=out_ap, in_=out_ap, func=AF.Sqrt), reads=[key_out], writes=[key_out])
            tk.op('dve', lambda e: e.reciprocal(out=out_ap, in_=out_ap), reads=[key_out], writes=[key_out])

        def sc_gen(hb, h3):
            tC = u_t[:, 0:512]
            tD = u_t[:, 512:1024]
            st8s = sm[:, 320:328]
            rs8s = sm[:, 328:336]
            for j in range(4):
                wv, wkey = ws.next()
                for tap in range(3):
                    a = state['acc'] % 2
                    state['acc'] += 1
                    proj(pacc[a], pacc_k[a], hb, h3, [(tap, wv, wkey)])
                    tk.op('act', lambda e, a=a, tap=tap: e.activation(out=vt[tap], in_=pacc[a], func=AF.Copy),
                          reads=[pacc_k[a]], writes=[('vt', tap)])
                    yield
                for tap in range(3):
                    a = state['acc'] % 2
                    state['acc'] += 1
                    wv, wkey = ws.next()
                    proj(pacc[a], pacc_k[a], hb, h3, [(tap, wv, wkey)])
                    if tap == 0:
                        tk.op('dve', lambda e, a=a: e.tensor_tensor(out=tD, in0=pacc[a], in1=vt[0], op=ALU.mult),
                              reads=[pacc_k[a], ('vt', 0)], writes=['tD', 'u_t'])
                    else:
                        tk.op('dve', lambda e, a=a, tap=tap: e.tensor_tensor(out=tC, in0=pacc[a], in1=vt[tap], op=ALU.mult),
                              reads=[pacc_k[a], ('vt', tap)], writes=['tC', 'u_t'])
                        tk.op('pool', lambda e: e.tensor_tensor(out=tD, in0=tD, in1=tC, op=ALU.add),
                              reads=['tC', 'tD'], writes=['tD'])
                    yield
                a = state['acc'] % 2
                state['acc'] += 1
                wv, wkey = ws.next()
                proj(pacc[a], pacc_k[a], hb, h3, [(1, wv, wkey)])
                tk.op('dve', lambda e, a=a: e.tensor_tensor(out=tD, in0=pacc[a], in1=tD, op=ALU.mult),
                      reads=[pacc_k[a], 'tD'], writes=['tD'])
                tk.op('act', lambda e: e.activation(out=tC, in_=tD, func=AF.Square), reads=['tD'], writes=['tC'])
                tk.op('dve', lambda e: e.tensor_reduce(out=st8s, in_=tC.rearrange("p (g d) -> p g d", d=64), axis=AX.X,
                                                       op=ALU.add), reads=['tC'], writes=['st8s'])
                rstd_from_ss(st8s, 64.0, 8, 'st8s', 'rs8s', rs8s)
                tk.op('dve', lambda e: e.tensor_tensor(out=tD.rearrange("p (g d) -> p g d", d=64),
                                                       in0=tD.rearrange("p (g d) -> p g d", d=64),
                                                       in1=rs8s.unsqueeze(2).to_broadcast([128, 8, 64]), op=ALU.mult),
                      reads=['tD', 'rs8s'], writes=['tD'])
                transposes_to_yT(tD, 'tD', 16 + j * 4, scg)
                yield

        def tile_C(T, hb, h3, xbc, jj):
            scg_ = sc_gen(hb, h3)
            XBK = [('xbc', jj, i) for i in range(4)]
            xs3 = xbc[:, 0:2048].rearrange("p (h d) -> p h d", d=64)
            dt_for_tile(hb, h3)
            for _i in range(3):
                next(scg_, None)
            make_Bb()
            tk.dma('sp', lambda e, T=T: e.dma_start(out=HFb, in_=HFS[T]), 'hfl', reads=[('HFS', T)], writes=['HFb'])
            for (srcoff, dstb, dk) in [(2048, BT_b, 'BT_b'), (2560, CT_b, 'CT_b')]:
                for g in range(4):
                    tk.op('pe', lambda e, g=g, srcoff=srcoff: e.transpose(out=PT[:, g * 128:(g + 1) * 128],
                                                                         in_=xbc[:, srcoff + g * 128:srcoff + (g + 1) * 128],
                                                                         identity=ident),
                          reads=[('xbc', jj, 4), ('xbc', jj, 5), 'ident'], writes=['pt'], inc=(g == 3))
                tk.op('act', lambda e, dstb=dstb: e.activation(out=dstb, in_=PT, func=AF.Copy),
                      reads=['pt' for g in range(4)], writes=[dk])
            tk.op('pool', lambda e: e.tensor_tensor(out=ysum.rearrange("p (h d) -> p h d", d=64), in0=xs3,
                                                    in1=dsk.unsqueeze(2).to_broadcast([128, 32, 64]), op=ALU.mult),
                  reads=XBK + ['dsk'], writes=['ysum'] + HFk)
            for dr in (1, 0):
                sl0 = dr * 32
                chunk_scalars(dr, True)
                for _i in range(2):
                    next(scg_, None)
                Hb, Hbk = (HRb, 'HRb') if dr == 1 else (HFb, 'HFb')
                if dr == 1:
                    tk.op('act', lambda e: e.activation(out=HRb, in_=HR, func=AF.Copy), reads=Hkeys, writes=['HRb'])
                tri, trik = (triF, 'triF') if dr == 0 else (triR, 'triR')
                for g in range(4):
                    hs = slice(sl0 + g * 8, sl0 + g * 8 + 8)
                    tk.op('pe', lambda e, g=g: e.matmul(PS_[:, 64:192], lhsT=BT_b[:, g * 128:(g + 1) * 128],
                                                        rhs=CT_b[:, g * 128:(g + 1) * 128], start=True, stop=True),
                          reads=['BT_b', 'CT_b'], writes=['ps_'])
                    X3v = X3.rearrange("p (h l) -> p h l", l=128)
                    tk.op('pool', lambda e, hs=hs, tri=tri: e.tensor_tensor(
                        out=X3v, in0=dtA[:, hs].unsqueeze(2).to_broadcast([128, 8, 128]),
                        in1=tri.unsqueeze(1).to_broadcast([128, 8, 128]), op=ALU.mult),
                        reads=['dtA', trik], writes=['X3'])
                    for hf in range(2):
                        tk.op('pe', lambda e, hf=hf: e.matmul(PC[:, hf * 512:(hf + 1) * 512], lhsT=ones_f,
                                                              rhs=X3[:, hf * 512:(hf + 1) * 512], start=True, stop=True),
                              reads=['ones_f', 'X3'], writes=['pc'], inc=(hf == 1))
                    D3v = D3.rearrange("p (h l) -> p h l", l=128)
                    tk.op('dve', lambda e, g=g: e.tensor_tensor(
                        out=D3v, in0=PC.rearrange("p (h l) -> p h l", l=128),
                        in1=cs32[:, g * 8:(g + 1) * 8].unsqueeze(2).to_broadcast([128, 8, 128]), op=ALU.subtract),
                        reads=['pc', 'cs32'], writes=['D3'])
                    if dr == 0:
                        patt, cm = [[0, 8], [1, 128]], -1
                    else:
                        patt, cm = [[0, 8], [-1, 128]], 1
                    tk.op('pool', lambda e, patt=patt, cm=cm: e.affine_select(out=D3v, in_=D3v, pattern=patt, compare_op=ALU.is_ge,
                                                                             fill=tk.getreg(e, -200.0), base=0, channel_multiplier=cm),
                          reads=['D3'], writes=['D3'])
                    tk.op('act', lambda e: e.activation(out=E3, in_=D3, func=AF.Exp), reads=['D3'], writes=['E3'])
                    tk.op('dve', lambda e: e.tensor_tensor(
                        out=MT_b.rearrange("p (h l) -> p h l", l=128), in0=E3.rearrange("p (h l) -> p h l", l=128),
                        in1=PS_[:, 64:192].unsqueeze(1).to_broadcast([128, 8, 128]), op=ALU.mult),
                        reads=['E3', 'ps_'], writes=['MT_b'])
                    for _i in range(3):
                        next(scg_, None)
                    for hh in range(8):
                        hd = g * 8 + hh
                        tk.op('pe', lambda e, hh=hh, hd=hd: e.matmul(PY[:, hh * 64:(hh + 1) * 64], lhsT=MT_b[:, hh * 128:(hh + 1) * 128],
                                                                     rhs=xd_b[:, hd * 64:(hd + 1) * 64], start=True, stop=True),
                              reads=['MT_b', 'xd_b'], writes=['py'], inc=(hh == 7))
                    tk.op('pe', lambda e, g=g, Hb=Hb: e.matmul(PO, lhsT=CT_b[:, g * 128:(g + 1) * 128],
                                                               rhs=Hb[:, g * 512:(g + 1) * 512], start=True, stop=True),
                          reads=['CT_b', Hbk], writes=['po'])
                    tk.op('dve', lambda e, g=g: e.tensor_tensor(
                        out=tA.rearrange("p (h d) -> p h d", d=64), in0=PO.rearrange("p (h d) -> p h d", d=64),
                        in1=ecs32[:, g * 8:(g + 1) * 8].unsqueeze(2).to_broadcast([128, 8, 64]), op=ALU.mult),
                        reads=['po', 'ecs32'], writes=['tA'])
                    tk.op('dve', lambda e: e.tensor_tensor(out=tA, in0=tA, in1=PY, op=ALU.add), reads=['tA', 'py'], writes=['tA'])
                    tk.op('pool', lambda e, g=g: e.tensor_tensor(out=ysum[:, g * 512:(g + 1) * 512],
                                                                 in0=ysum[:, g * 512:(g + 1) * 512], in1=tA, op=ALU.add),
                          reads=['ysum', 'tA'], writes=['ysum'])
                if dr == 1:
                    state_update(HR, 'HR')
            for _ in scg_:
                pass
            zb = [tB, tA]
            zk = ['tB', 'tA']
            zacc = []

            def z_proj():
                a = state['acc'] % 2
                state['acc'] += 1
                wv, wkey = ws.next()
                proj(pacc[a], pacc_k[a], hb, h3, [(1, wv, wkey)])
                zacc.append(a)

            def z_chain(g):
                a = zacc[g]
                tz, kz = zb[g % 2], zk[g % 2]
                ss, rs = st8[:, g % 2:g % 2 + 1], rs8[:, g % 2:g % 2 + 1]
                kss, krs = ('st8z', g % 2), ('rs8z', g % 2)
                tk.op('act', lambda e: e.activation(out=tz, in_=pacc[a], func=AF.Silu), reads=[pacc_k[a]], writes=[kz])
                tk.op('dve', lambda e: e.tensor_tensor(out=tz, in0=tz, in1=ysum[:, g * 512:(g + 1) * 512], op=ALU.mult),
                      reads=[kz, 'ysum'], writes=[kz])
                tk.op('act', lambda e: e.activation(out=vt[0], in_=tz, func=AF.Square, accum_out=ss),
                      reads=[kz], writes=[('vt', 0), kss])
                rstd_from_ss(ss, 512.0, 1, kss, krs, rs)
                tk.op('dve', lambda e: e.tensor_scalar(out=tz, in0=tz, scalar1=rs, scalar2=None, op0=ALU.mult),
                      reads=[kz, krs], writes=[kz])
                transposes_to_yT(tz, kz, g * 4, ssdg)

            z_proj()
            for g in range(4):
                if g + 1 < 4:
                    z_proj()
                z_chain(g)
            for _ in scg_:
                pass
            ypk = [('yT', k) for k in range(32)]
            for nb in range(4):
                a = state['acc'] % 2
                state['acc'] += 1
                for hf in range(2):
                    wv, wkey = ws.next()
                    for kc in range(16):
                        tk.op('pe', lambda e, a=a, hf=hf, kc=kc, wv=wv: e.matmul(
                            pacc[a], lhsT=yT3[:, hf * 16 + kc, :], rhs=wv[:, kc, :], start=(hf == 0 and kc == 0),
                            stop=(hf == 1 and kc == 15)), reads=ypk + [wkey], writes=[pacc_k[a]], inc=(kc == 15))
                tk.op('dve', lambda e, a=a, nb=nb: e.tensor_tensor(out=tA, in0=pacc[a], in1=g1b[:, nb * 512:(nb + 1) * 512], op=ALU.mult),
                      reads=[pacc_k[a], 'g1b'], writes=['tA'])
                if DBG == 1:
                    tk.op('dve', lambda e, a=a, nb=nb: e.tensor_copy(out=u_t[:, nb * 512:(nb + 1) * 512], in_=pacc[a]),
                          reads=[pacc_k[a], 'tA'], writes=['u_t'])
                else:
                    tk.op('dve', lambda e, nb=nb, hb=hb: e.scalar_tensor_tensor(out=u_t[:, nb * 512:(nb + 1) * 512],
                                                                                in0=XT[hb][:, nb * 512:(nb + 1) * 512], scalar=ALPHA, in1=tA,
                                                                                op0=ALU.mult, op1=ALU.add),
                          reads=[('XT', hb), 'tA'], writes=['u_t'])
            if DBG == 3:
                tk.op('dve', lambda e: e.tensor_copy(out=u_t[:, 0:1024], in_=xbc[:, 2048:3072]), reads=[('xbc', jj, 4), ('xbc', jj, 5), 'u_t'], writes=['u_t'])
                tk.op('dve', lambda e: e.tensor_copy(out=u_t[:, 1024:1088], in_=dt64), reads=['dt64', 'u_t'], writes=['u_t'])
                tk.op('dve', lambda e: e.tensor_copy(out=u_t[:, 1088:2048], in_=xbc[:, 0:960]), reads=XBK + ['u_t'], writes=['u_t'])
            if DBG == 2:
                tk.op('dve', lambda e: e.tensor_copy(out=u_t, in_=ysum), reads=['ysum', 'u_t'], writes=['u_t'])
            if DBG == 0:
                layer_norm(tk, u_t, 'u_t', sm, l1g, 'l1g', l1b, 'l1b', xbc[:, 0:2048], XBK)
            tk.dma('pool', lambda e, T=T: e.dma_start(out=X1S[T * 128:(T + 1) * 128, :], in_=u_t), 'x1s',
                   reads=['u_t'], writes=[('X1S', T)])

        for pi in range(8):
            if stage < 3:
                break
            pair = [15 - 2 * pi, 14 - 2 * pi]
            infos = [load_tile(T * 128, T == 0, False, xt=j, ht=j) for j, T in enumerate(pair)]
            xbc_blocks_group(ws, infos, range(6))
            for j, T in enumerate(pair):
                cur['j'] = j
                cur['xb'] = xbcG[j]
                tile_C(T, infos[j][0], infos[j][1], xbcG[j], j)
        tk.seal('x1s')

        tk.barrier()
        ar.off = PERSIST
        if stage >= 4:
            peer_phase(nc, tk, ar, bank, PSM, WS, X1S, modd, kT, EUb, EVb, ln2g_b, ln2b_b, out, ident, iota16)
        else:
            tb = ar.f32(2048)
            for T in range(16):
                tk.dma('sp', lambda e, T=T: e.dma_start(out=tb, in_=X1S[T * 128:(T + 1) * 128, :]), 'dbl',
                       reads=[('X1S', T)], writes=['tb'])
                tk.dma('sp', lambda e, T=T: e.dma_start(out=out[T * 128:(T + 1) * 128, :], in_=tb), 'outst',
                       reads=['tb'], writes=[('out', T)])
        tk.barrier()
        tk.emit()
    return nc


def layer_norm(tk, u, uk, sm, g, gk, b, bk, scratch, scratch_keys):
    mean = sm[:, 224:225]
    ssq = sm[:, 225:226]
    rstd = sm[:, 226:227]
    tk.op('act', lambda e: e.activation(out=scratch, in_=u, func=AF.Identity, accum_out=mean),
          reads=[uk], writes=list(scratch_keys) + ['ln_mean'])
    tk.op('dve', lambda e: e.tensor_scalar(out=mean, in0=mean, scalar1=1.0 / 2048.0, scalar2=None, op0=ALU.mult),
          reads=['ln_mean'], writes=['ln_mean'])
    tk.op('dve', lambda e: e.tensor_scalar(out=u, in0=u, scalar1=mean, scalar2=None, op0=ALU.subtract),
          reads=[uk, 'ln_mean'], writes=[uk])
    tk.op('act', lambda e: e.activation(out=scratch, in_=u, func=AF.Square, accum_out=ssq),
          reads=[uk], writes=list(scratch_keys) + ['ln_ssq'])
    tk.op('dve', lambda e: e.tensor_scalar(out=rstd, in0=ssq, scalar1=1.0 / 2048.0, scalar2=EPS, op0=ALU.mult, op1=ALU.add),
          reads=['ln_ssq'], writes=['ln_rstd'])
    tk.op('act', lambda e: e.activation(out=rstd, in_=rstd, func=AF.Sqrt), reads=['ln_rstd'], writes=['ln_rstd'])
    tk.op('dve', lambda e: e.reciprocal(out=rstd, in_=rstd), reads=['ln_rstd'], writes=['ln_rstd'])
    tk.op('dve', lambda e: e.tensor_scalar(out=u, in0=u, scalar1=rstd, scalar2=None, op0=ALU.mult),
          reads=[uk, 'ln_rstd'], writes=[uk])
    tk.op('dve', lambda e: e.tensor_tensor(out=u, in0=u, in1=g, op=ALU.mult), reads=[uk, gk], writes=[uk])
    tk.op('dve', lambda e: e.tensor_tensor(out=u, in0=u, in1=b, op=ALU.add), reads=[uk, bk], writes=[uk])


def peer_phase(nc, tk, ar, bank, PSM, WS, X1S, modd, kT, eu, ev, ln2g_b, ln2b_b, out, ident, iota16):
    NB = 12
    sc2p = ar.f32(2048)
    sh2 = ar.f32(2048)
    g2b = ar.f32(2048)
    l2g = ar.f32(2048)
    l2b = ar.f32(2048)
    KT_b = ar.bf16(4096)
    acc = ar.f32(2048)
    x1 = [acc, ar.f32(2048)]
    h2 = [ar.f32(2048), ar.f32(2048)]
    h2T = ar.bf16(16 * 128)
    qT = ar.bf16(32 * 128)
    WQ = [ar.bf16(8192)]
    S = ar.f32(2048)
    S2 = ar.f32(2048)
    V16 = ar.f32(256)
    I16 = ar.u32(256)
    I16f = ar.f32(256)
    CAND = ar.f32(2048)
    CAND2 = S2
    SCV = ar.f32(128)
    FL = ar.u32(128)
    ABf = ar.f32(256)
    OH = CAND
    E12 = ar.f32(256)
    IDX = [ar.i32(128), ar.i32(128), ar.i32(128)]
    IDXf = ar.f32(128)
    GATE = [ar.f32(128), ar.f32(128), ar.f32(128)]
    ACT_ = [ar.f32(128), ar.f32(128)]
    COEF = [ar.f32(128), ar.f32(128)]
    sm = ar.f32(512)
    thr16 = sm[:, 16:32]
    UBR = ar.f32(NB * 1024)
    UB = [UBR[:, b * 1024:(b + 1) * 1024].bitcast(BF16) for b in range(NB)]
    DG = [ar.bf16(128) for _ in range(4)]
    junk = OH

    tk.dma('sp', lambda e: e.dma_start(out=sh2, in_=modd[0:1, 6144:8192].partition_broadcast(128)), 'pd', reads=['modd'], writes=['sh2'])
    tk.dma('sp', lambda e: e.dma_start(out=sc2p, in_=modd[0:1, 8192:10240].partition_broadcast(128)), 'pd', reads=['modd'], writes=['sc2p'])
    tk.dma('sp', lambda e: e.dma_start(out=g2b, in_=modd[0:1, 10240:12288].partition_broadcast(128)), 'pd', reads=['modd'], writes=['g2b'])
    tk.dma('sp', lambda e: e.dma_start(out=l2g, in_=ln2g_b[:, :]), 'pd', writes=['l2g'])
    tk.dma('sp', lambda e: e.dma_start(out=l2b, in_=ln2b_b[:, :]), 'pd', writes=['l2b'])
    tk.seal('pd')
    tk.op('dve', lambda e: e.tensor_scalar(out=thr16, in0=iota16, scalar1=16.0, scalar2=16.0, op0=ALU.mult, op1=ALU.add),
          reads=['iota16'], writes=['thr16'])
    tk.op('dve', lambda e: e.tensor_scalar(out=sc2p, in0=sc2p, scalar1=1.0, scalar2=None, op0=ALU.add), reads=['sc2p'], writes=['sc2p'])
    for i in range(2):
        k32 = UBR[:, i * 2048:(i + 1) * 2048]
        tk.dma('sp', lambda e, i=i, k32=k32: e.dma_start(out=k32, in_=kT[:, i * 2048:(i + 1) * 2048]), 'pd2_%d' % i,
               writes=[('UB', 2 * i), ('UB', 2 * i + 1)])
        tk.op('act', lambda e, i=i, k32=k32: e.activation(out=KT_b[:, i * 2048:(i + 1) * 2048], in_=k32, func=AF.Copy),
              reads=[('UB', 2 * i), ('UB', 2 * i + 1)], writes=['KT_b'])
    h2T3 = h2T.rearrange("p (k t) -> p k t", t=128)
    qT3 = qT.rearrange("p (k t) -> p k t", t=128)
    KT3 = KT_b.rearrange("p (k n) -> p k n", n=128)
    PT = bank(2)
    PQ = [bank(0), bank(1)]
    PS3 = bank(3)
    PSC = PSM[:, 4 * 512:8 * 512]
    st = {'qi': 0, 'gi': 0}
    V3 = V16.rearrange("p (h k) -> p h k", k=16)
    I3 = I16.rearrange("p (h k) -> p h k", k=16)
    S3 = S.rearrange("p (h k) -> p h k", k=128)
    S23 = S2.rearrange("p (h k) -> p h k", k=128)
    V4 = V16.rearrange("p (h s k) -> p h s k", s=2, k=16)
    C4 = CAND.rearrange("p (h a b) -> p h a b", a=16, b=16)
    C3 = CAND.rearrange("p (h c) -> p h c", c=256)
    C23 = CAND2.rearrange("p (h c) -> p h c", c=256)
    SC3 = SCV.rearrange("p (h k) -> p h k", k=16)
    FL3 = FL.rearrange("p (h k) -> p h k", k=16)
    I4f = I16f.rearrange("p (h s k) -> p h s k", s=2, k=16)
    OH4 = OH.rearrange("p (h k a) -> p h k a", k=16, a=16)

    def top16(vals, idxs, src, src2, hh, keys):
        kv, ki, ks, ks2 = keys
        tk.op('dve', lambda e: e.max(out=vals[:, hh, 0:8], in_=src[:, hh, :]), reads=[ks], writes=[kv])
        tk.op('dve', lambda e: e.max_index(out=idxs[:, hh, 0:8], in_max=vals[:, hh, 0:8], in_values=src[:, hh, :]),
              reads=[ks, kv], writes=[ki])
        tk.op('dve', lambda e: e.match_replace(out=src2[:, hh, :], in_to_replace=vals[:, hh, 0:8], in_values=src[:, hh, :],
                                               imm_value=NEG), reads=[ks, kv], writes=[ks2])
        tk.op('dve', lambda e: e.max(out=vals[:, hh, 8:16], in_=src2[:, hh, :]), reads=[ks2], writes=[kv])
        tk.op('dve', lambda e: e.max_index(out=idxs[:, hh, 8:16], in_max=vals[:, hh, 8:16], in_values=src2[:, hh, :]),
              reads=[ks2, kv], writes=[ki])

    def stage_A(T):
        p = T % 2
        p3 = T % 3
        x1p, h2p, IDXp, GATEp = x1[0], h2[p], IDX[p3], GATE[p3]
        kx, kh, ki_, kg = 'acc', ('h2', p), ('IDX', p3), ('GATE', p3)
        tk.dma('sp', lambda e: e.dma_start(out=x1p, in_=X1S[T * 128:(T + 1) * 128, :]), 'x1l0', reads=[('X1S', T)], writes=[kx])
        tk.op('dve', lambda e: e.tensor_tensor(out=h2p, in0=x1p, in1=sc2p, op=ALU.mult), reads=[kx, 'sc2p'], writes=[kh])
        tk.op('dve', lambda e: e.tensor_tensor(out=h2p, in0=h2p, in1=sh2, op=ALU.add), reads=[kh, 'sh2'], writes=[kh])
        yield
        for k4 in range(4):
            for q in range(4):
                kc = k4 * 4 + q
                tk.op('pe', lambda e, kc=kc, q=q: e.transpose(out=PT[:, q * 128:(q + 1) * 128], in_=h2p[:, kc * 128:(kc + 1) * 128],
                                                             identity=ident), reads=[kh, 'ident'], writes=['pt'], inc=(q == 3))
            for q in range(4):
                kc = k4 * 4 + q
                tk.op('act', lambda e, kc=kc, q=q: e.activation(out=h2T3[:, kc, :], in_=PT[:, q * 128:(q + 1) * 128], func=AF.Copy),
                      reads=['pt'], writes=[('h2T', kc)])
            yield
        h2Tk = [('h2T', kc) for kc in range(16)]
        for hb in range(8):
            tk.dma('sp', lambda e, hb=hb: e.dma_start(out=WQ[0], in_=WS[blk_q(hb)]), 'wq0',
                   reads=[('WS', blk_q(hb))], writes=[('WQ', 0)])
            wv = WQ[0].rearrange("p (k n) -> p k n", n=512)
            for cc in range(4):
                a = st['qi'] % 2
                st['qi'] += 1
                for kc in range(16):
                    tk.op('pe', lambda e, a=a, kc=kc, cc=cc, wv=wv: e.matmul(PQ[a][:, 0:128], lhsT=wv[:, kc, cc * 128:(cc + 1) * 128],
                                                                           rhs=h2T3[:, kc, :], start=(kc == 0), stop=(kc == 15)),
                          reads=h2Tk + [('WQ', 0)], writes=[('ps', a)], inc=(kc == 15))
                tk.op('act', lambda e, a=a, hb=hb, cc=cc: e.activation(out=qT3[:, hb * 4 + cc, :], in_=PQ[a][:, 0:128], func=AF.Copy),
                      reads=[('ps', a)], writes=[('qT', hb * 4 + cc)])
                yield
        qTk = [('qT', i) for i in range(32)]
        for b4 in range(4):
            for h4 in range(4):
                hh = b4 * 4 + h4
                for jc in range(2):
                    tk.op('pe', lambda e, hh=hh, h4=h4, jc=jc: e.matmul(PS3[:, h4 * 128:(h4 + 1) * 128], lhsT=qT3[:, hh * 2 + jc, :],
                                                                      rhs=KT3[:, hh * 2 + jc, :], start=(jc == 0), stop=(jc == 1)),
                          reads=qTk + ['KT_b'], writes=['ps3'], inc=(h4 == 3 and jc == 1))
            tk.op('act', lambda e, b4=b4: e.activation(out=S[:, b4 * 512:(b4 + 1) * 512], in_=PS3, func=AF.Copy),
                  reads=['ps3'], writes=['S'])
            yield
        for hh in range(16):
            top16(V3, I3, S3, S23, hh, ('V16', 'I16', 'S', 'S2'))
            yield
        tk.op('dve', lambda e: e.tensor_tensor(out=C4, in0=V4[:, :, 0, :].unsqueeze(3).to_broadcast([128, 8, 16, 16]),
                                               in1=V4[:, :, 1, :].unsqueeze(2).to_broadcast([128, 8, 16, 16]), op=ALU.add),
              reads=['V16'], writes=['CAND'])
        yield
        for h in range(8):
            top16(SC3, FL3, C3, C23, h, ('SCV', 'FL', 'CAND', 'S2'))
            yield
        tk.op('dve', lambda e: e.tensor_copy(out=ABf[:, 128:256], in_=FL), reads=['FL'], writes=['ABf'])
        tk.op('dve', lambda e: e.tensor_tensor(out=OH.rearrange("p (hk a) -> p hk a", a=16),
                                               in0=ABf[:, 128:256].unsqueeze(2).to_broadcast([128, 128, 16]),
                                               in1=thr16.unsqueeze(1).to_broadcast([128, 128, 16]), op=ALU.is_ge),
              reads=['ABf', 'thr16'], writes=['CAND'])
        tk.op('dve', lambda e: e.tensor_reduce(out=ABf[:, 0:128], in_=OH.rearrange("p (hk a) -> p hk a", a=16), axis=AX.X,
                                               op=ALU.add), reads=['CAND'], writes=['ABf'])
        tk.op('dve', lambda e: e.scalar_tensor_tensor(out=ABf[:, 128:256], in0=ABf[:, 0:128], scalar=-16.0, in1=ABf[:, 128:256],
                                                      op0=ALU.mult, op1=ALU.add), reads=['ABf'], writes=['ABf'])
        tk.op('dve', lambda e: e.tensor_copy(out=I16f, in_=I16), reads=['I16'], writes=['I16f'])
        yield
        for s_ in range(2):
            ab = ABf[:, s_ * 128:(s_ + 1) * 128].rearrange("p (h k) -> p h k", k=16)
            tk.op('dve', lambda e, ab=ab: e.tensor_tensor(out=OH4, in0=ab.unsqueeze(3).to_broadcast([128, 8, 16, 16]),
                                                          in1=iota16.unsqueeze(1).unsqueeze(1).to_broadcast([128, 8, 16, 16]),
                                                          op=ALU.is_equal), reads=['ABf', 'iota16'], writes=['CAND'])
            tk.op('dve', lambda e, s_=s_: e.tensor_tensor(out=OH4, in0=OH4,
                                                          in1=I4f[:, :, s_, :].unsqueeze(2).to_broadcast([128, 8, 16, 16]),
                                                          op=ALU.mult), reads=['CAND', 'I16f'], writes=['CAND'])
            tk.op('dve', lambda e, s_=s_: e.tensor_reduce(out=E12[:, s_ * 128:(s_ + 1) * 128],
                                                          in_=OH.rearrange("p (hk a) -> p hk a", a=16), axis=AX.X, op=ALU.add),
                  reads=['CAND'], writes=['E12'])
            yield
        tk.op('dve', lambda e: e.scalar_tensor_tensor(out=IDXf, in0=E12[:, 0:128], scalar=128.0, in1=E12[:, 128:256],
                                                      op0=ALU.mult, op1=ALU.add), reads=['E12'], writes=['IDXf'])
        tk.op('dve', lambda e: e.tensor_copy(out=IDXp, in_=IDXf), reads=['IDXf'], writes=[ki_])
        G3 = GATEp.rearrange("p (h k) -> p h k", k=16)
        tk.op('dve', lambda e: e.tensor_tensor(out=G3, in0=SC3, in1=SC3[:, :, 0:1].to_broadcast([128, 8, 16]), op=ALU.subtract),
              reads=['SCV'], writes=[kg])
        tk.op('act', lambda e: e.activation(out=GATEp, in_=GATEp, func=AF.Exp), reads=[kg], writes=[kg])
        tk.op('dve', lambda e: e.tensor_reduce(out=sm[:, 0:8], in_=G3, axis=AX.X, op=ALU.add), reads=[kg], writes=['gsum'])
        tk.op('dve', lambda e: e.reciprocal(out=sm[:, 0:8], in_=sm[:, 0:8]), reads=['gsum'], writes=['gsum'])
        tk.op('dve', lambda e: e.tensor_tensor(out=G3, in0=G3, in1=sm[:, 0:8].unsqueeze(2).to_broadcast([128, 8, 16]), op=ALU.mult),
              reads=[kg, 'gsum'], writes=[kg])
        yield

    def u_slot(T, sl):
        p, p3 = T % 2, T % 3
        b = st['gi'] % NB
        st['gi'] += 1
        IDXp, h2p, ACTp = IDX[p3], h2[p], ACT_[p]
        tk.dma('pool', lambda e: e.indirect_dma_start(
            out=UB[b], out_offset=None, in_=eu[:, :], in_offset=bass.IndirectOffsetOnAxis(ap=IDXp[:, sl:sl + 1], axis=0)),
            'ub%d' % b, reads=[('IDX', p3)], writes=[('UB', b)])
        tk.op('dve', lambda e: e.scalar_tensor_tensor(out=UB[b], in0=UB[b], scalar=1.0, in1=h2p, op0=ALU.mult,
                                                      op1=ALU.mult, accum_out=ACTp[:, sl:sl + 1]),
              reads=[('UB', b), ('h2', p)], writes=[('UB', b), ('ACT', p, sl)])

    def u_finish(T):
        p, p3 = T % 2, T % 3
        ACTp, COEFp, GATEp = ACT_[p], COEF[p], GATE[p3]
        tk.op('act', lambda e: e.activation(out=COEFp, in_=ACTp, func=AF.Gelu), reads=[('ACT', p, sl) for sl in range(128)],
              writes=[('COEF', p)])
        tk.op('dve', lambda e: e.tensor_tensor(out=COEFp, in0=COEFp, in1=GATEp, op=ALU.mult),
              reads=[('COEF', p), ('GATE', p3)], writes=[('COEF', p)])

    def v_slot(T, sl):
        p, p3 = T % 2, T % 3
        b = st['gi'] % NB
        st['gi'] += 1
        IDXp, COEFp = IDX[p3], COEF[p]
        tk.dma('pool', lambda e: e.indirect_dma_start(
            out=UB[b], out_offset=None, in_=ev[:, :], in_offset=bass.IndirectOffsetOnAxis(ap=IDXp[:, sl:sl + 1], axis=0)),
            'ub%d' % b, reads=[('IDX', p3)], writes=[('UB', b)])
        if sl % 4 == 3:
            accd = x1[1]
            if sl == 3:
                tk.op('dve', lambda e: e.tensor_scalar(out=accd, in0=UB[b], scalar1=COEFp[:, sl:sl + 1], scalar2=None, op0=ALU.mult),
                      reads=[('UB', b), ('COEF', p)], writes=[('x1', 1)])
            else:
                tk.op('dve', lambda e: e.scalar_tensor_tensor(out=accd, in0=UB[b], scalar=COEFp[:, sl:sl + 1], in1=accd,
                                                              op0=ALU.mult, op1=ALU.add),
                      reads=[('UB', b), ('COEF', p), ('x1', 1)], writes=[('x1', 1)])
            return
        dj = sl % 4
        tk.op('act', lambda e: e.activation(out=DG[dj], in_=ident, func=AF.Copy, scale=COEFp[:, sl:sl + 1]),
              reads=['ident', ('COEF', p)], writes=[('DG', dj)])
        for nb in range(4):
            tk.op('pe', lambda e, nb=nb: e.matmul(PSC[:, nb * 512:(nb + 1) * 512], lhsT=DG[dj],
                                                  rhs=UB[b][:, nb * 512:(nb + 1) * 512], start=(sl == 0), stop=(sl == 126)),
                  reads=[('DG', dj), ('UB', b)], writes=['psc'], inc=(nb == 3))

    def v_finish(T):
        x1f = x1[1]
        tk.op('dve', lambda e: e.tensor_tensor(out=acc, in0=PSC, in1=x1f, op=ALU.add), reads=['psc', ('x1', 1)], writes=['acc'])
        tk.dma('sp', lambda e: e.dma_start(out=x1f, in_=X1S[T * 128:(T + 1) * 128, :]), 'x1l1', reads=[('X1S', T)], writes=[('x1', 1)])
        tk.op('dve', lambda e: e.tensor_tensor(out=acc, in0=acc, in1=g2b, op=ALU.mult), reads=['acc', 'g2b'], writes=['acc'])
        tk.op('dve', lambda e: e.scalar_tensor_tensor(out=acc, in0=x1f, scalar=ALPHA, in1=acc, op0=ALU.mult, op1=ALU.add),
              reads=[('x1', 1), 'acc'], writes=['acc'])
        layer_norm(tk, acc, 'acc', sm, l2g, 'l2g', l2b, 'l2b', PSC, ['psc'])
        tk.dma('sp', lambda e: e.dma_start(out=out[T * 128:(T + 1) * 128, :], in_=acc), 'outst',
               reads=['acc'], writes=[('out', T)])

    for i in range(NT + 2):
        genA = stage_A(i) if i < NT else iter(())
        tu = i - 1 if 0 <= i - 1 < NT else None
        tv = i - 2 if 0 <= i - 2 < NT else None
        if tu is None and tv is None:
            for _ in genA:
                pass
            continue
        for sl in range(128):
            if tv is not None:
                v_slot(tv, sl)
            if tu is not None:
                u_slot(tu, sl)
            if sl % 2 == 1 or (sl % 10 == 0 and sl > 0):
                next(genA, None)
        for _ in genA:
            pass
        if tu is not None:
            u_finish(tu)
        if tv is not None:
            v_finish(tv)


_CACHE = {}


def make_inputs(inputs, core):
    b, half = core // 2, core % 2
    flip = half == 1
    f = np.float32
    x = np.asarray(inputs['x'])[b]
    if flip:
        x = x[::-1]
    xp = np.zeros((4098, 2048), f)
    xp[1:4097] = x
    w_in = np.ascontiguousarray(np.asarray(inputs['w_in'])[0], dtype=f)
    wd = w_in[:, ODT:ODT + 64]
    cw = np.asarray(inputs['conv_ssd_w'])[0]
    scw = np.asarray(inputs['short_conv_w'])[0]
    dbf, dbb = np.asarray(inputs['dt_bias_f'])[0], np.asarray(inputs['dt_bias_b'])[0]
    alf, alb = np.asarray(inputs['a_log_f'])[0], np.asarray(inputs['a_log_b'])[0]
    if flip:
        wd = np.concatenate([wd[:, 32:64], wd[:, 0:32]], axis=1)
        cw = cw[::-1]
        scw = scw[::-1]
        dtb = np.concatenate([dbb, dbf])
        alog = np.concatenate([alb, alf])
    else:
        dtb = np.concatenate([dbf, dbb])
        alog = np.concatenate([alf, alb])

    def bc(v):
        v = np.asarray(v, dtype=f).reshape(1, -1)
        return np.ascontiguousarray(np.broadcast_to(v, (128, v.shape[1])))

    def fm(v):
        return np.ascontiguousarray(np.asarray(v, dtype=f).reshape(16, 128).T)

    sk = np.asarray(inputs['sub_keys'])[0]
    kT = sk.reshape(8, 2, 128, 2, 128).transpose(4, 0, 1, 3, 2).reshape(128, 4096)
    return {
        'xp': xp,
        'cT': fm(np.asarray(inputs['c'])[b]),
        'w_ada': np.ascontiguousarray(np.asarray(inputs['w_ada'])[0], dtype=f),
        'b_ada': np.ascontiguousarray(np.asarray(inputs['b_ada'])[0:1], dtype=f),
        'w_in': w_in,
        'w_dt': np.ascontiguousarray(wd, dtype=f),
        'cwb': bc(np.ascontiguousarray(cw).reshape(-1)),
        'scwb': bc(np.ascontiguousarray(scw).reshape(-1)),
        'cbias': np.ascontiguousarray(np.asarray(inputs['conv_ssd_b'])[0:1], dtype=f),
        'dtb_b': bc(dtb),
        'alog_b': bc(alog),
        'dsk_b': bc(np.asarray(inputs['d_skip'])[0]),
        'ssdgT': fm(np.asarray(inputs['ssd_norm_g'])[0]),
        'scgT': fm(np.asarray(inputs['sc_norm_g'])[0]),
        'w_out': np.ascontiguousarray(np.asarray(inputs['w_out'])[0], dtype=f),
        'ln1g_b': bc(np.asarray(inputs['ln1_g'])[0]),
        'ln1b_b': bc(np.asarray(inputs['ln1_b'])[0]),
        'w_query': np.ascontiguousarray(np.asarray(inputs['w_query'])[0], dtype=f),
        'kT': np.ascontiguousarray(kT, dtype=f),
        'eu': np.ascontiguousarray(np.asarray(inputs['expert_u'])[0], dtype=f),
        'ev': np.ascontiguousarray(np.asarray(inputs['expert_v'])[0], dtype=f),
        'ln2g_b': bc(np.asarray(inputs['ln2_g'])[0]),
        'ln2b_b': bc(np.asarray(inputs['ln2_b'])[0]),
    }


def kernel(_stage=99, _cores=8, **inputs):
    if _stage not in _CACHE:
        _CACHE[_stage] = build(_stage)
    nc = _CACHE[_stage]
    in_maps = [make_inputs(inputs, c) for c in range(_cores)]
    res = run_bass_kernel_spmd(nc, in_maps, core_ids=list(range(_cores)))
    outp = np.zeros((4, 4096, 2048), np.float32)
    for c in range(_cores):
        b, half = c // 2, c % 2
        o = np.asarray(res.results[c]['out'])
        if half == 0:
            outp[b, 0:2048] = o
        else:
            outp[b, 2048:4096] = o[::-1]
    return outp
```

```python
import numpy as np
import concourse.bass as bass
import concourse.mybir as mybir
from concourse.bass_utils import run_bass_kernel_spmd
from contextlib import ExitStack

F32 = mybir.dt.float32
BF16 = mybir.dt.bfloat16
I32 = mybir.dt.int32
U32 = mybir.dt.uint32
AF = mybir.ActivationFunctionType
ALU = mybir.AluOpType
AX = mybir.AxisListType

ENGS = ['pe', 'act', 'dve', 'pool', 'sp']
ALPHA = 2.0 ** 0.25
EPS = 1e-5
NT = 16
NEG = -1.0e30


class Tracker:
    def __init__(self, nc, es):
        self.nc = nc
        self.es = es
        self.stream = {e: [] for e in ENGS}
        self.sems = {}
        self.cnt = {}
        self.seen = {e: {} for e in ENGS}
        self.lastw = {}
        self.readers = {}
        self.chan_keys = {}
        for e in ENGS:
            self._sem('E_' + e)

    def _sem(self, name):
        if name not in self.sems:
            self.sems[name] = self.es.enter_context(self.nc.semaphore(name))
            self.cnt[name] = 0
        return name

    def _deps(self, reads, writes):
        deps = []
        for k in reads:
            if k in self.lastw:
                deps.append(self.lastw[k])
        for k in writes:
            if k in self.lastw:
                deps.append(self.lastw[k])
            deps.extend(self.readers.get(k, {}).items())
        return deps

    def _emit_waits(self, eng, deps, skip_sem=None):
        best = {}
        for (s, v) in deps:
            if s == skip_sem:
                continue
            if v > best.get(s, 0):
                best[s] = v
        for s, v in best.items():
            if self.seen[eng].get(s, 0) < v:
                self.stream[eng].append(('wait', s, v))
                self.seen[eng][s] = v

    def _commit(self, token, reads, writes):
        for k in writes:
            self.lastw[k] = token
            self.readers[k] = {}
        for k in reads:
            d = self.readers.setdefault(k, {})
            if d.get(token[0], 0) < token[1]:
                d[token[0]] = token[1]

    def op(self, eng, fn, reads=(), writes=(), inc=True):
        s = 'E_' + eng
        deps = self._deps(reads, writes)
        self._emit_waits(eng, deps, skip_sem=(s if eng == 'pe' else None))
        if inc:
            self.cnt[s] += 1
            token = (s, self.cnt[s])
        else:
            token = (s, self.cnt[s] + 1)
        self.stream[eng].append(('op', fn, (s, 1) if inc else None))
        self._commit(token, reads, writes)
        return token

    def dma(self, q, fn, chan, reads=(), writes=()):
        s = self._sem('D_' + chan)
        deps = self._deps(reads, writes)
        self._emit_waits(q, deps)
        self.cnt[s] += 16
        token = (s, self.cnt[s])
        self.stream[q].append(('op', fn, (s, 16)))
        self._commit(token, reads, writes)
        self.chan_keys.setdefault(chan, set()).update(writes)
        return token

    def seal(self, chan):
        s = 'D_' + chan
        if s not in self.cnt:
            return
        tok = (s, self.cnt[s])
        for k in self.chan_keys.get(chan, ()):
            if k in self.lastw and self.lastw[k][0] == s:
                self.lastw[k] = tok

    def barrier(self):
        for e in ENGS:
            for s, c in self.cnt.items():
                if c > 0 and self.seen[e].get(s, 0) < c:
                    self.stream[e].append(('wait', s, c))
                    self.seen[e][s] = c

    def getreg(self, eng, val):
        if not hasattr(self, '_regs'):
            self._regs = {}
        if val not in self._regs:
            self._regs[val] = eng.to_reg(val)
        return self._regs[val]

    def emit(self):
        nc = self.nc
        tk = self

        def run(engname):
            def body(eng):
                for it in tk.stream[engname]:
                    if it[0] == 'wait':
                        eng.wait_ge(tk.sems[it[1]], it[2])
                    else:
                        ins = it[1](eng)
                        if it[2] is not None:
                            ins.then_inc(tk.sems[it[2][0]], it[2][1])
            return body

        with nc.Block() as block:
            block.tensor(run('pe'))
            block.scalar(run('act'))
            block.vector(run('dve'))
            block.gpsimd(run('pool'))
            block.sync(run('sp'))


class Arena:
    def __init__(self, ap):
        self.ap = ap
        self.off = 0
        self.N = ap.shape[1]

    def f32(self, n):
        a = self.ap[:, self.off:self.off + n]
        self.off += n
        assert self.off <= self.N, ("arena overflow", self.off, self.N)
        return a

    def bf16(self, n):
        m = (n + 1) // 2
        return self.f32(m).bitcast(BF16)

    def i32(self, n):
        return self.f32(n).bitcast(I32)

    def u32(self, n):
        return self.f32(n).bitcast(U32)


OZ, OXBC, ODT, OGB, OGC, OV = 0, 2048, 5120, 5184, 7232, 9280
NBLK = 58


def blk_xbc(cb, tap):
    return cb * 3 + tap


def blk_z(g):
    return 18 + g


def blk_sc(j, k):
    return 22 + j * 5 + k


def blk_out(nb, hf):
    return 42 + nb * 2 + hf


def blk_q(hb):
    return 50 + hb


def build(stage=99):
    import os
    SUB = int(os.environ.get('K_SUB', '9'))
    DBG = int(os.environ.get('K_DBG', '0'))
    nc = bass.Bass("TRN2", target_bir_lowering=False)

    def din(name, shape, dt=F32):
        return nc.dram_tensor(name, shape, dt, kind="ExternalInput").ap()

    xp = din("xp", [4098, 2048])
    cT = din("cT", [128, 16])
    w_ada = din("w_ada", [2048, 12288])
    b_ada = din("b_ada", [1, 12288])
    w_in = din("w_in", [2048, 11328])
    w_dt = din("w_dt", [2048, 64])
    cwb = din("cwb", [128, 9216])
    scwb = din("scwb", [128, 6144])
    cbias = din("cbias", [1, 3072])
    dtb_b = din("dtb_b", [128, 64])
    alog_b = din("alog_b", [128, 64])
    dsk_b = din("dsk_b", [128, 32])
    ssdgT = din("ssdgT", [128, 16])
    scgT = din("scgT", [128, 16])
    w_out = din("w_out", [4096, 2048])
    ln1g_b = din("ln1g_b", [128, 2048])
    ln1b_b = din("ln1b_b", [128, 2048])
    w_query = din("w_query", [2048, 4096])
    kT = din("kT", [128, 4096])
    eu = din("eu", [16384, 2048])
    ev = din("ev", [16384, 2048])
    ln2g_b = din("ln2g_b", [128, 2048])
    ln2b_b = din("ln2b_b", [128, 2048])
    out = nc.dram_tensor("out", [2048, 2048], F32, kind="ExternalOutput").ap()
    WS = nc.dram_tensor("WS", [NBLK, 128, 16 * 512], BF16, kind="Internal").ap()
    modd = nc.dram_tensor("modd", [1, 12288], F32, kind="Internal").ap()
    HFS = nc.dram_tensor("HFS", [NT, 128, 2048], BF16, kind="Internal").ap()
    X1S = nc.dram_tensor("X1S", [2048, 2048], F32, kind="Internal").ap()
    EUb = nc.dram_tensor("EUb", [16384, 2048], BF16, kind="Internal").ap()
    EVb = nc.dram_tensor("EVb", [16384, 2048], BF16, kind="Internal").ap()

    es = ExitStack()
    with es:
        tk = Tracker(nc, es)
        ARN = es.enter_context(nc.sbuf_tensor("arena", [128, 52900], F32))
        PSM = es.enter_context(nc.psum_tensor("psm", [128, 4096], F32))
        ar = Arena(ARN[:, :])

        def bank(i, n=512):
            return PSM[:, i * 512:i * 512 + n]

        ident = ar.f32(128)
        ones_f = ar.f32(128)
        triF = ar.f32(128)
        triR = ar.f32(128)
        ones_b = ar.bf16(128)
        sc1pT = ar.f32(16)
        sh1T = ar.f32(16)
        dtb = ar.f32(64)
        a_neg = ar.f32(64)
        dsk = ar.f32(32)
        ssdg = ar.f32(16)
        scg = ar.f32(16)
        iota16 = ar.f32(16)
        bias2 = ar.bf16(3072)
        wdt_sb = ar.bf16(16 * 64)
        PERSIST = ar.off

        tk.op('pool', lambda e: e.memset(ident, 0.0), writes=['ident'])
        tk.op('pool', lambda e: e.affine_select(out=ident, in_=ident, pattern=[[-1, 128]], compare_op=ALU.not_equal,
                                                fill=tk.getreg(e, 1.0), base=0, channel_multiplier=1), reads=['ident'], writes=['ident'])
        tk.op('pool', lambda e: e.memset(ones_f, 1.0), writes=['ones_f'])
        tk.op('pool', lambda e: e.memset(ones_b, 1.0), writes=['ones_b'])
        tk.op('pool', lambda e: e.memset(triF, 1.0), writes=['triF'])
        tk.op('pool', lambda e: e.affine_select(out=triF, in_=triF, pattern=[[1, 128]], compare_op=ALU.is_ge,
                                                fill=tk.getreg(e, 0.0), base=0, channel_multiplier=-1), reads=['triF'], writes=['triF'])
        tk.op('pool', lambda e: e.memset(triR, 1.0), writes=['triR'])
        tk.op('pool', lambda e: e.affine_select(out=triR, in_=triR, pattern=[[-1, 128]], compare_op=ALU.is_ge,
                                                fill=tk.getreg(e, 0.0), base=0, channel_multiplier=1), reads=['triR'], writes=['triR'])
        tk.op('pool', lambda e: e.iota(iota16, pattern=[[1, 16]], base=0, channel_multiplier=0,
                                       allow_small_or_imprecise_dtypes=True), writes=['iota16'])
        for (dst, src, nm) in [(dtb, dtb_b, 'dtb'), (dsk, dsk_b, 'dsk'), (ssdg, ssdgT, 'ssdg'), (scg, scgT, 'scg')]:
            tk.dma('sp', lambda e, dst=dst, src=src: e.dma_start(out=dst, in_=src[:, :]), 'par', writes=[nm])
        tk.dma('sp', lambda e: e.dma_start(out=a_neg, in_=alog_b[:, :]), 'par', writes=['a_neg'])
        tk.seal('par')
        tk.op('act', lambda e: e.activation(out=a_neg, in_=a_neg, func=AF.Exp), reads=['a_neg'], writes=['a_neg'])
        tk.op('dve', lambda e: e.tensor_scalar(out=a_neg, in0=a_neg, scalar1=-1.0, scalar2=None, op0=ALU.mult),
              reads=['a_neg'], writes=['a_neg'])

        P0 = ar.off
        cs_ = ar.f32(16)
        bada = ar.f32(12288)
        wa = [ar.f32(4096), ar.f32(4096)]
        wab = [ar.bf16(4096), ar.bf16(4096)]
        cs_b = ar.bf16(16)
        stg = ar.f32(4096)
        tk.dma('sp', lambda e: e.dma_start(out=cs_, in_=cT[:, :]), 'p0', writes=['cs_'])
        tk.dma('sp', lambda e: e.dma_start(out=bada[0:1, :], in_=b_ada[:, :]), 'p0', writes=['bada'])
        tk.seal('p0')
        tk.op('act', lambda e: e.activation(out=cs_, in_=cs_, func=AF.Silu), reads=['cs_'], writes=['cs_'])
        tk.op('dve', lambda e: e.tensor_copy(out=cs_b, in_=cs_), reads=['cs_'], writes=['cs_b'])
        it = 0
        for g in range(3):
            for kc in range(16):
                b = it % 2
                it += 1
                tk.dma('sp', lambda e, b=b, kc=kc, g=g: e.dma_start(
                    out=wa[b], in_=w_ada[kc * 128:(kc + 1) * 128, g * 4096:(g + 1) * 4096]), 'wa%d' % b, writes=[('wa', b)])
                ceng = ['dve', 'pool', 'act'][it % 3]
                if ceng == 'act':
                    tk.op('act', lambda e, b=b: e.activation(out=wab[b], in_=wa[b], func=AF.Copy),
                          reads=[('wa', b)], writes=[('wab', b)])
                else:
                    tk.op(ceng, lambda e, b=b: e.tensor_copy(out=wab[b], in_=wa[b]), reads=[('wa', b)], writes=[('wab', b)])
                for j in range(8):
                    tk.op('pe', lambda e, b=b, kc=kc, j=j: e.matmul(bank(j)[0:1, :], lhsT=cs_b[:, kc:kc + 1],
                                                                   rhs=wab[b][:, j * 512:(j + 1) * 512],
                                                                   start=(kc == 0), stop=(kc == 15)),
                          reads=['cs_b', ('wab', b)], writes=[('ps', j)], inc=(j == 7))
            for j in range(8):
                tk.op('dve', lambda e, j=j, g=g: e.tensor_tensor(out=stg[0:1, j * 512:(j + 1) * 512], in0=bank(j)[0:1, :],
                                                                 in1=bada[0:1, g * 4096 + j * 512:g * 4096 + (j + 1) * 512],
                                                                 op=ALU.add),
                      reads=[('ps', j), 'bada'], writes=['stg'])
            tk.dma('sp', lambda e, g=g: e.dma_start(out=modd[0:1, g * 4096:(g + 1) * 4096], in_=stg[0:1, :]), 'modst',
                   reads=['stg'], writes=['modd'])
        tk.seal('modst')
        t16 = ar.f32(128)
        for (dst, off, nm, addone) in [(sh1T, 0, 'sh1T', False), (sc1pT, 2048, 'sc1pT', True)]:
            tk.dma('sp', lambda e, off=off: e.dma_start(out=t16[0:16, :],
                                                        in_=modd[0, off:off + 2048].rearrange("(k p) -> k p", p=128)),
                   'm16', reads=['modd'], writes=['t16'])
            tk.op('pe', lambda e: e.transpose(out=bank(0)[:, 0:16], in_=t16[0:16, :], identity=ident[0:16, 0:16]),
                  reads=['t16', 'ident'], writes=[('ps', 0)])
            if addone:
                tk.op('dve', lambda e, dst=dst: e.tensor_scalar(out=dst, in0=bank(0)[:, 0:16], scalar1=1.0, scalar2=None,
                                                                op0=ALU.add), reads=[('ps', 0)], writes=[nm])
            else:
                tk.op('dve', lambda e, dst=dst: e.tensor_copy(out=dst, in_=bank(0)[:, 0:16]), reads=[('ps', 0)], writes=[nm])
        cb32 = ar.f32(3072)
        cbt = ar.f32(3072)
        tk.dma('sp', lambda e: e.dma_start(out=cb32[0:1, :], in_=cbias[:, :]), 'p0b', writes=['cb32'])
        tk.op('dve', lambda e: e.tensor_copy(out=bias2[0:1, :], in_=cb32[0:1, :]), reads=['cb32'], writes=['bias2'])
        tk.op('dve', lambda e: e.tensor_tensor(out=cbt[0:1, :], in0=cb32[0:1, :], in1=bias2[0:1, :], op=ALU.subtract),
              reads=['cb32', 'bias2'], writes=['cbt'])
        lo_d = nc.dram_tensor("lo_d", [1, 3072], BF16, kind="Internal").ap()
        lo16 = ar.bf16(3072)
        tk.op('dve', lambda e: e.tensor_copy(out=lo16[0:1, :], in_=cbt[0:1, :]), reads=['cbt'], writes=['lo16'])
        tk.dma('sp', lambda e: e.dma_start(out=lo_d[:, :], in_=lo16[0:1, :]), 'p0c', reads=['lo16'], writes=['lo_d'])
        tk.dma('sp', lambda e: e.dma_start(out=bias2[1:2, :], in_=lo_d[:, :]), 'p0d', reads=['lo_d', 'bias2'], writes=['bias2'])
        wdt32 = ar.f32(16 * 64)
        tk.dma('sp', lambda e: e.dma_start(out=wdt32.rearrange("p (k n) -> p k n", n=64),
                                           in_=w_dt.rearrange("(k p) n -> p k n", p=128)), 'p0e', writes=['wdt32'])
        tk.op('dve', lambda e: e.tensor_copy(out=wdt_sb, in_=wdt32), reads=['wdt32'], writes=['wdt_sb'])

        tk.barrier()
        ar.off = PERSIST
        cw_sb = ar.f32(9216)
        scw_sb = ar.f32(6144)
        wst = [ar.f32(8192), ar.f32(8192)]
        wcs = [ar.bf16(8192), ar.bf16(8192)]
        tk.dma('sp', lambda e: e.dma_start(out=cw_sb, in_=cwb[:, :]), 'pa', writes=['cw_sb'])
        tk.dma('sp', lambda e: e.dma_start(out=scw_sb, in_=scwb[:, :]), 'pa', writes=['scw_sb'])
        tk.seal('pa')
        bases = []
        for cb in range(6):
            bases.append((w_in[:, OXBC + cb * 512:OXBC + (cb + 1) * 512],
                          [(blk_xbc(cb, t), cw_sb[:, t * 3072 + cb * 512:t * 3072 + (cb + 1) * 512]) for t in range(3)]))
        for g in range(4):
            bases.append((w_in[:, OZ + g * 512:OZ + (g + 1) * 512], [(blk_z(g), None)]))
        for j in range(4):
            bases.append((w_in[:, OGB + j * 512:OGB + (j + 1) * 512], [(blk_sc(j, 0), None)]))
            bases.append((w_in[:, OGC + j * 512:OGC + (j + 1) * 512],
                          [(blk_sc(j, 1 + t), scw_sb[:, t * 2048 + j * 512:t * 2048 + (j + 1) * 512]) for t in range(3)]))
            bases.append((w_in[:, OV + j * 512:OV + (j + 1) * 512], [(blk_sc(j, 4), None)]))
        for nb in range(4):
            for hf in range(2):
                bases.append((w_out[hf * 2048:(hf + 1) * 2048, nb * 512:(nb + 1) * 512], [(blk_out(nb, hf), None)]))
        for hb in range(8):
            bases.append((w_query[:, hb * 512:(hb + 1) * 512], [(blk_q(hb), None)]))
        ci = 0
        for bi, (src, ders) in enumerate(bases if stage >= 1 else []):
            sb_ = bi % 2
            n_ = src.shape[1]
            tk.dma('sp', lambda e, sb_=sb_, src=src, n_=n_: e.dma_start(out=wst[sb_].rearrange("p (k n) -> p k n", n=n_),
                                                                 in_=src.rearrange("(k p) n -> p k n", p=128)),
                   'wst%d' % sb_, writes=[('wst', sb_)])
            for (bid, scl) in ders:
                cbuf = ci % 2
                ci += 1
                if scl is None:
                    eng = ['act', 'dve', 'pool'][ci % 3] if isinstance(bid, tuple) else 'act'
                    if eng == 'act':
                        tk.op('act', lambda e, sb_=sb_, cbuf=cbuf: e.activation(out=wcs[cbuf], in_=wst[sb_], func=AF.Copy),
                              reads=[('wst', sb_)], writes=[('wcs', cbuf)])
                    else:
                        tk.op(eng, lambda e, sb_=sb_, cbuf=cbuf: e.tensor_copy(out=wcs[cbuf], in_=wst[sb_]),
                              reads=[('wst', sb_)], writes=[('wcs', cbuf)])
                else:
                    eng = 'dve' if (ci % 2 == 0) else 'pool'
                    tk.op(eng, lambda e, sb_=sb_, cbuf=cbuf, scl=scl: e.tensor_tensor(
                        out=wcs[cbuf].rearrange("p (k n) -> p k n", n=512),
                        in0=wst[sb_].rearrange("p (k n) -> p k n", n=512),
                        in1=scl.unsqueeze(1).to_broadcast([128, 16, 512]), op=ALU.mult),
                        reads=[('wst', sb_), 'cw_sb', 'scw_sb'], writes=[('wcs', cbuf)])
                if isinstance(bid, tuple):
                    dst, nm = bid
                    tk.dma('act', lambda e, dst=dst, cbuf=cbuf, n_=n_: e.dma_start(
                        out=dst.rearrange("(k p) n -> p k n", p=128), in_=wcs[cbuf].rearrange("p (k n) -> p k n", n=n_)), 'wsst%d' % cbuf,
                        reads=[('wcs', cbuf)], writes=[nm])
                else:
                    tk.dma('act', lambda e, bid=bid, cbuf=cbuf: e.dma_start(out=WS[bid], in_=wcs[cbuf]), 'wsst%d' % cbuf,
                           reads=[('wcs', cbuf)], writes=[('WS', bid)])
        tk.seal('wsst0')
        tk.seal('wsst1')

        tk.barrier()
        ar.off = PERSIST
        NWB = 2
        WB = [ar.bf16(8192) for _ in range(NWB)]
        XT = [ar.f32(2048), ar.f32(2048)]
        XH = [ar.f32(256), ar.f32(256)]
        hT = [ar.bf16(16 * 130) for _ in range(4)]
        xbc0 = ar.f32(3072)
        xbc1 = ar.f32(3072)
        cur = {'j': 0, 'xb': xbc0}
        ysum = ar.f32(2048)
        xd_b = ar.bf16(2048)
        xdd_b = ar.bf16(2048)
        B_b = ar.bf16(512)
        BT_b = ar.bf16(512)
        CT_b = ar.bf16(512)
        HR = ar.f32(2048)
        HRb = ar.bf16(2048)
        HFb = ar.bf16(2048)
        off_x3 = ar.off
        X3 = ar.f32(1024)
        D3 = ar.f32(1024)
        E3 = ar.f32(1024)
        MT_b = ar.bf16(1024)
        yT = ar.bf16(32 * 128)
        R6 = ar.f32(6144)
        g1b, l1g, l1b = R6[:, 0:2048], R6[:, 2048:4096], R6[:, 4096:6144]
        xbcG = [xbc0, xbc1, R6[:, 0:3072], R6[:, 3072:6144]]
        off_ta = ar.off
        tA = ar.f32(512)
        tB = ar.f32(512)
        vt = [ar.f32(512) for _ in range(3)]
        u_t = ar.f32(2048)
        dt64 = ar.f32(64)
        dtA = ar.f32(64)
        sm = ar.f32(512)
        cs32, dst32, ecs32, cdec32, w232 = sm[:, 0:32], sm[:, 32:64], sm[:, 64:96], sm[:, 96:128], sm[:, 128:160]
        t32 = sm[:, 160:192]
        st8 = sm[:, 192:208]
        rs8 = sm[:, 208:224]
        e64 = sm[:, 256:320]

        PA, PB, PT, PS_, PY, PO = bank(0), bank(1), bank(2), bank(3), bank(4), bank(5)
        PC = PSM[:, 6 * 512:8 * 512]
        pacc = [PA, PB]
        pacc_k = [('ps', 0), ('ps', 1)]
        gacc = [PA, PB, PY, PO]
        gacc_k = [('ps', 0), ('ps', 1), 'py', 'po']
        state = {'wi': 0, 'acc': 0, 'hb': 0}

        class WStream:
            def __init__(self, seq):
                self.seq = seq
                self.issued = 0
                self.pos = 0

            def _issue(self):
                if self.issued < len(self.seq):
                    bid = self.seq[self.issued]
                    slot = state['wi'] % NWB
                    state['wi'] += 1
                    tk.dma('sp', lambda e, bid=bid, slot=slot: e.dma_start(out=WB[slot], in_=WS[bid]), 'wb%d' % slot,
                           reads=[('WS', bid)], writes=[('WB', slot)])
                    self.slots = getattr(self, 'slots', []) + [slot]
                    self.issued += 1

            def next(self):
                while self.issued < min(self.pos + NWB, len(self.seq)):
                    self._issue()
                slot = self.slots[self.pos]
                self.pos += 1
                return WB[slot].rearrange("p (k n) -> p k n", n=512), ('WB', slot)

        def load_tile(pos0, zero_lo, zero_hi, xt=None, ht=None):
            xb_ = xt
            hb = ht
            tk.dma('sp', lambda e: e.dma_start(out=XT[xb_], in_=xp[pos0 + 1:pos0 + 129, :]), 'xt%d' % xb_, writes=[('XT', xb_)])
            tk.dma('sp', lambda e: e.dma_start(out=XH[xb_][0:16, 0:128], in_=xp[pos0, :].rearrange("(k p) -> k p", p=128)),
                   'xh%d' % xb_, writes=[('XH', xb_)])
            tk.dma('sp', lambda e: e.dma_start(out=XH[xb_][0:16, 128:256], in_=xp[pos0 + 129, :].rearrange("(k p) -> k p", p=128)),
                   'xh%d' % xb_, writes=[('XH', xb_)])
            h3 = hT[hb].rearrange("p (k t) -> p k t", t=130)
            for k4 in range(4):
                for q in range(4):
                    kc = k4 * 4 + q
                    tk.op('pe', lambda e, kc=kc, q=q: e.transpose(out=PT[:, q * 128:(q + 1) * 128],
                                                                 in_=XT[xb_][:, kc * 128:(kc + 1) * 128], identity=ident),
                          reads=[('XT', xb_), 'ident'], writes=['pt'], inc=(q == 3))
                for q in range(4):
                    kc = k4 * 4 + q
                    tk.op('act', lambda e, kc=kc, q=q: e.activation(out=h3[:, kc, 1:129], in_=PT[:, q * 128:(q + 1) * 128],
                                                                   func=AF.Identity, scale=sc1pT[:, kc:kc + 1],
                                                                   bias=sh1T[:, kc:kc + 1]),
                          reads=['pt', 'sc1pT', 'sh1T'], writes=[('hT', hb, kc)])
            for hi in range(2):
                tk.op('pe', lambda e, hi=hi: e.transpose(out=PS_[:, 256 + hi * 16:256 + hi * 16 + 16],
                                                        in_=XH[xb_][0:16, hi * 128:(hi + 1) * 128], identity=ident[0:16, 0:16]),
                      reads=[('XH', xb_), 'ident'], writes=['ps_'], inc=(hi == 1))
            tk.op('dve', lambda e: e.tensor_tensor(out=t32.rearrange("p (t k) -> p t k", k=16),
                                                   in0=PS_[:, 256:288].rearrange("p (t k) -> p t k", k=16),
                                                   in1=sc1pT.unsqueeze(1).to_broadcast([128, 2, 16]), op=ALU.mult),
                  reads=['ps_', 'sc1pT'], writes=['t32'])
            tk.op('dve', lambda e: e.tensor_tensor(out=h3[:, :, 0:130:129].rearrange("p k t -> p t k"),
                                                   in0=t32.rearrange("p (t k) -> p t k", k=16),
                                                   in1=sh1T.unsqueeze(1).to_broadcast([128, 2, 16]), op=ALU.add),
                  reads=['t32', 'sh1T'], writes=[('hTh', hb)])
            if zero_lo:
                tk.op('dve', lambda e: e.memset(h3[:, :, 0:1], 0.0), reads=[('hTh', hb)], writes=[('hTh', hb)])
            if zero_hi:
                tk.op('dve', lambda e: e.memset(h3[:, :, 129:130], 0.0), reads=[('hTh', hb)], writes=[('hTh', hb)])
            return hb, h3

        def hkeys(hb):
            return [('hT', hb, kc) for kc in range(16)] + [('hTh', hb)]

        def proj(ps, pskey, hb, h3, taps_blocks, ncols=512, bias_cols=None, first=True, last=True):
            n = len(taps_blocks)
            for ti, (tap, wv, wkey) in enumerate(taps_blocks):
                for kc in range(16):
                    st = first and ti == 0 and kc == 0
                    sp_ = last and (bias_cols is None) and ti == n - 1 and kc == 15
                    tk.op('pe', lambda e, tap=tap, wv=wv, kc=kc, st=st, sp_=sp_: e.matmul(
                        ps[:, 0:ncols], lhsT=h3[:, kc, tap:tap + 128], rhs=wv[:, kc, 0:ncols], start=st, stop=sp_),
                        reads=hkeys(hb) + [wkey], writes=[pskey], inc=(kc == 15))
            if bias_cols is not None:
                c0 = bias_cols
                tk.op('pe', lambda e: e.matmul(ps[:, 0:ncols], lhsT=ones_b[0:2, :], rhs=bias2[0:2, c0:c0 + ncols],
                                               start=False, stop=True),
                      reads=['ones_b', 'bias2'], writes=[pskey])

        wdt3 = wdt_sb.rearrange("p (k n) -> p k n", n=64)

        def xbc_blocks_group(ws, infos, cbs, hook=None):
            for cb in cbs:
                for tap in range(3):
                    wv, wkey = ws.next()
                    for j, (hb, h3) in enumerate(infos):
                        proj(gacc[j], gacc_k[j], hb, h3, [(tap, wv, wkey)], bias_cols=(cb * 512 if tap == 2 else None),
                             first=(tap == 0), last=(tap == 2))
                    if hook is not None:
                        hook()
                for j, (hb, h3) in enumerate(infos):
                    tk.op('act', lambda e, j=j, cb=cb: e.activation(out=xbcG[j][:, cb * 512:(cb + 1) * 512], in_=gacc[j], func=AF.Silu),
                          reads=[gacc_k[j]], writes=[('xbc', j, cb)])

        def dt_for_tile(hb, h3):
            a = state['acc'] % 2
            state['acc'] += 1
            proj(pacc[a], pacc_k[a], hb, h3, [(1, wdt3, 'wdt_sb')], ncols=64)
            tk.op('dve', lambda e, a=a: e.tensor_tensor(out=dt64, in0=pacc[a][:, 0:64], in1=dtb, op=ALU.add),
                  reads=[pacc_k[a], 'dtb'], writes=['dt64'])
            tk.op('act', lambda e: e.activation(out=e64, in_=dt64, func=AF.Exp), reads=['dt64'], writes=['e64'])
            tk.op('act', lambda e: e.activation(out=dt64, in_=e64, func=AF.Ln, bias=1.0), reads=['e64'], writes=['dt64'])
            tk.op('dve', lambda e: e.tensor_tensor(out=dtA, in0=dt64, in1=a_neg, op=ALU.mult),
                  reads=['dt64', 'a_neg'], writes=['dtA'])

        def chunk_scalars(dr, full):
            xbc = cur['xb']
            jj = cur['j']
            sl = slice(dr * 32, dr * 32 + 32)
            tri, trik = (triF, 'triF') if dr == 0 else (triR, 'triR')
            tk.op('pe', lambda e: e.matmul(PS_[:, 0:32], lhsT=tri, rhs=dtA[:, sl], start=True, stop=True),
                  reads=[trik, 'dtA'], writes=['ps_'], inc=False)
            tk.op('pe', lambda e: e.matmul(PS_[:, 32:64], lhsT=ones_f, rhs=dtA[:, sl], start=True, stop=True),
                  reads=['ones_f', 'dtA'], writes=['ps_'])
            tk.op('dve', lambda e: e.tensor_copy(out=cs32, in_=PS_[:, 0:32]), reads=['ps_'], writes=['cs32'])
            tk.op('dve', lambda e: e.tensor_tensor(out=dst32, in0=PS_[:, 32:64], in1=cs32, op=ALU.subtract),
                  reads=['ps_', 'cs32'], writes=['dst32'])
            tk.op('act', lambda e: e.activation(out=dst32, in_=dst32, func=AF.Exp), reads=['dst32'], writes=['dst32'])
            tk.op('act', lambda e: e.activation(out=cdec32, in_=PS_[:, 32:64], func=AF.Exp), reads=['ps_'], writes=['cdec32'])
            if full:
                tk.op('act', lambda e: e.activation(out=ecs32, in_=cs32, func=AF.Exp), reads=['cs32'], writes=['ecs32'])
            tk.op('dve', lambda e: e.tensor_tensor(out=w232, in0=dt64[:, sl], in1=dst32, op=ALU.mult),
                  reads=['dt64', 'dst32'], writes=['w232'])
            xs3 = xbc[:, 0:2048].rearrange("p (h d) -> p h d", d=64)
            tk.op('pool', lambda e: e.tensor_tensor(out=xdd_b.rearrange("p (h d) -> p h d", d=64), in0=xs3,
                                                    in1=w232.unsqueeze(2).to_broadcast([128, 32, 64]), op=ALU.mult),
                  reads=[('xbc', jj, i) for i in range(4)] + ['w232'], writes=['xdd_b'])
            if full:
                tk.op('pool', lambda e: e.tensor_tensor(out=xd_b.rearrange("p (h d) -> p h d", d=64), in0=xs3,
                                                        in1=dt64[:, sl].unsqueeze(2).to_broadcast([128, 32, 64]), op=ALU.mult),
                      reads=[('xbc', jj, i) for i in range(4)] + ['dt64'], writes=['xd_b'])

        def state_update(H, Hk):
            for g in range(4):
                tk.op('pe', lambda e, g=g: e.matmul(PO, lhsT=B_b[:, g * 128:(g + 1) * 128], rhs=xdd_b[:, g * 512:(g + 1) * 512],
                                                    start=True, stop=True),
                      reads=['B_b', 'xdd_b'], writes=['po'])
                Hg = H[:, g * 512:(g + 1) * 512].rearrange("p (h d) -> p h d", d=64)
                tk.op('dve', lambda e, g=g, Hg=Hg: e.tensor_tensor(out=Hg, in0=Hg,
                                                                   in1=cdec32[:, g * 8:(g + 1) * 8].unsqueeze(2).to_broadcast([128, 8, 64]),
                                                                   op=ALU.mult),
                      reads=[(Hk, g), 'cdec32'], writes=[(Hk, g)])
                tk.op('dve', lambda e, g=g: e.tensor_tensor(out=H[:, g * 512:(g + 1) * 512], in0=H[:, g * 512:(g + 1) * 512],
                                                            in1=PO, op=ALU.add),
                      reads=[(Hk, g), 'po'], writes=[(Hk, g)])

        def make_Bb():
            xbc = cur['xb']
            tk.op('act', lambda e: e.activation(out=B_b, in_=xbc[:, 2048:2560], func=AF.Copy), reads=[('xbc', cur['j'], 4)], writes=['B_b'])

        Hkeys = [('HR', g) for g in range(4)]

        cin = [ARN[:, off_x3:off_x3 + 2048], ARN[:, off_x3 + 2048:off_x3 + 4096]]
        cout = [ARN[:, off_ta:off_ta + 1024].bitcast(BF16), ARN[:, off_ta + 1024:off_ta + 2048].bitcast(BF16)]

        def conv_gen():
            def src(k):
                tab = eu if k < 128 else ev
                r0 = (k % 128) * 128
                return tab[r0:r0 + 128, :]

            def dst(k):
                tab = EUb if k < 128 else EVb
                r0 = (k % 128) * 128
                return tab[r0:r0 + 128, :], ('EUb' if k < 128 else 'EVb')

            def load(k):
                i = k % 2
                sk = src(k)
                tk.dma('pool', lambda e: e.dma_start(out=cin[i], in_=sk), 'cvl%d' % i, writes=[('cin', i)])
            load(0)
            for k in range(256):
                if k + 1 < 256:
                    load(k + 1)
                i = k % 2
                tk.op('dve', lambda e, i=i: e.tensor_copy(out=cout[i], in_=cin[i]), reads=[('cin', i)], writes=[('cout', i)])
                dk, nm = dst(k)
                tk.dma('pool', lambda e, i=i, dk=dk: e.dma_start(out=dk, in_=cout[i]), 'cvs%d' % i, reads=[('cout', i)], writes=[nm])
                yield

        cvg = conv_gen()

        def state_sweep(tiles, dr, H, Hk, spill):
            groups = [tiles[i:i + 4] for i in range(0, len(tiles), 4)]
            seq = []
            for _ in groups:
                for cb in range(5):
                    for tap in range(3):
                        seq.append(blk_xbc(cb, tap))
            ws = WStream(seq)
            for grp in groups:
                infos = []
                for j, T in enumerate(grp):
                    infos.append(load_tile(T * 128, T == 0, T == 31, xt=j % 2, ht=j))
                xbc_blocks_group(ws, infos, range(5), hook=lambda: [next(cvg, None) for _c in range(3)])
                for j, T in enumerate(grp):
                    if spill:
                        tk.op('act', lambda e: e.activation(out=HFb, in_=H, func=AF.Copy), reads=[(Hk, g) for g in range(4)], writes=['HFb'])
                        tk.dma('sp', lambda e, T=T: e.dma_start(out=HFS[T], in_=HFb), 'hfs', reads=['HFb'], writes=[('HFS', T)])
                    cur['j'] = j
                    cur['xb'] = xbcG[j]
                    dt_for_tile(*infos[j])
                    make_Bb()
                    chunk_scalars(dr, False)
                    state_update(H, Hk)

        tk.op('dve', lambda e: e.memset(HR, 0.0), writes=Hkeys)
        if stage >= 2:
            state_sweep(list(range(31, 15, -1)), 1, HR, 'HR', False)
        HF = ysum
        HFk = [('HF', g) for g in range(4)]
        tk.op('dve', lambda e: e.memset(HF, 0.0), writes=HFk)
        if stage >= 2:
            state_sweep(list(range(15)), 0, HF, 'HF', True)
            tk.op('act', lambda e: e.activation(out=HFb, in_=HF, func=AF.Copy), reads=HFk, writes=['HFb'])
            tk.dma('sp', lambda e: e.dma_start(out=HFS[15], in_=HFb), 'hfs', reads=['HFb'], writes=[('HFS', 15)])
        tk.seal('hfs')
        for _ in cvg:
            pass
        tk.barrier()
        tk.dma('sp', lambda e: e.dma_start(out=g1b, in_=modd[0:1, 4096:6144].partition_broadcast(128)), 'pm',
               reads=['modd'], writes=['g1b'])
        tk.dma('sp', lambda e: e.dma_start(out=l1g, in_=ln1g_b[:, :]), 'pm', writes=['l1g'])
        tk.dma('sp', lambda e: e.dma_start(out=l1b, in_=ln1b_b[:, :]), 'pm', writes=['l1b'])
        tk.seal('pm')

        seqC = []
        for pi in range(8):
            for cb in range(6):
                for tap in range(3):
                    seqC.append(blk_xbc(cb, tap))
            for _t in range(2):
                for j in range(4):
                    seqC += [blk_sc(j, 4), blk_sc(j, 1), blk_sc(j, 2), blk_sc(j, 3), blk_sc(j, 0)]
                for g in range(4):
                    seqC.append(blk_z(g))
                for nb in range(4):
                    seqC += [blk_out(nb, 0), blk_out(nb, 1)]
        ws = WStream(seqC)
        yT3 = yT.rearrange("p (k t) -> p k t", t=128)

        def transposes_to_yT(src, srckey, kbase, gT):
            for cc in range(4):
                q = cc
                tk.op('pe', lambda e, cc=cc, q=q: e.transpose(out=PT[:, q * 128:(q + 1) * 128],
                                                             in_=src[:, cc * 128:(cc + 1) * 128], identity=ident),
                      reads=[srckey, 'ident'], writes=['pt'], inc=(q == 3))
            for cc in range(4):
                q = cc
                col = kbase + cc - (kbase // 16) * 16
                tk.op('act', lambda e, cc=cc, q=q, col=col: e.activation(out=yT3[:, kbase + cc, :], in_=PT[:, q * 128:(q + 1) * 128],
                                                                        func=AF.Copy, scale=gT[:, col:col + 1]),
                      reads=['pt', 'ssdg', 'scg'], writes=[('yT', kbase + cc)])

        def rstd_from_ss(ss_ap, n_elems, width, key_in, key_out, out_ap):
            tk.op('dve', lambda e: e.tensor_scalar(out=out_ap, in0=ss_ap, scalar1=1.0 / n_elems, scalar2=EPS, op0=ALU.mult,
                                                   op1=ALU.add), reads=[key_in], writes=[key_out])
            tk.op('act', lambda e: e.activation(out=out_ap, in_=out_ap, func=AF.Sqrt), reads=[key_out], writes=[key_out])
            tk.op('dve', lambda e: e.reciprocal(out=out_ap, in_=out_ap), reads=[key_out], writes=[key_out])

        def sc_gen(hb, h3):
            tC = u_t[:, 0:512]
            tD = u_t[:, 512:1024]
            st8s = sm[:, 320:328]
            rs8s = sm[:, 328:336]
            for j in range(4):
                wv, wkey = ws.next()
                for tap in range(3):
                    a = state['acc'] % 2
                    state['acc'] += 1
                    proj(pacc[a], pacc_k[a], hb, h3, [(tap, wv, wkey)])
                    tk.op('act', lambda e, a=a, tap=tap: e.activation(out=vt[tap], in_=pacc[a], func=AF.Copy),
                          reads=[pacc_k[a]], writes=[('vt', tap)])
                    yield
                for tap in range(3):
                    a = state['acc'] % 2
                    state['acc'] += 1
                    wv, wkey = ws.next()
                    proj(pacc[a], pacc_k[a], hb, h3, [(tap, wv, wkey)])
                    if tap == 0:
                        tk.op('dve', lambda e, a=a: e.tensor_tensor(out=tD, in0=pacc[a], in1=vt[0], op=ALU.mult),
                              reads=[pacc_k[a], ('vt', 0)], writes=['tD', 'u_t'])
                    else:
                        tk.op('dve', lambda e, a=a, tap=tap: e.tensor_tensor(out=tC, in0=pacc[a], in1=vt[tap], op=ALU.mult),
                              reads=[pacc_k[a], ('vt', tap)], writes=['tC', 'u_t'])
                        tk.op('pool', lambda e: e.tensor_tensor(out=tD, in0=tD, in1=tC, op=ALU.add),
                              reads=['tC', 'tD'], writes=['tD'])
                    yield
                a = state['acc'] % 2
                state['acc'] += 1
                wv, wkey = ws.next()
                proj(pacc[a], pacc_k[a], hb, h3, [(1, wv, wkey)])
                tk.op('dve', lambda e, a=a: e.tensor_tensor(out=tD, in0=pacc[a], in1=tD, op=ALU.mult),
                      reads=[pacc_k[a], 'tD'], writes=['tD'])
                tk.op('act', lambda e: e.activation(out=tC, in_=tD, func=AF.Square), reads=['tD'], writes=['tC'])
                tk.op('dve', lambda e: e.tensor_reduce(out=st8s, in_=tC.rearrange("p (g d) -> p g d", d=64), axis=AX.X,
                                                       op=ALU.add), reads=['tC'], writes=['st8s'])
                rstd_from_ss(st8s, 64.0, 8, 'st8s', 'rs8s', rs8s)
                tk.op('dve', lambda e: e.tensor_tensor(out=tD.rearrange("p (g d) -> p g d", d=64),
                                                       in0=tD.rearrange("p (g d) -> p g d", d=64),
                                                       in1=rs8s.unsqueeze(2).to_broadcast([128, 8, 64]), op=ALU.mult),
                      reads=['tD', 'rs8s'], writes=['tD'])
                transposes_to_yT(tD, 'tD', 16 + j * 4, scg)
                yield

        def tile_C(T, hb, h3, xbc, jj):
            scg_ = sc_gen(hb, h3)
            XBK = [('xbc', jj, i) for i in range(4)]
            xs3 = xbc[:, 0:2048].rearrange("p (h d) -> p h d", d=64)
            dt_for_tile(hb, h3)
            for _i in range(3):
                next(scg_, None)
            make_Bb()
            tk.dma('sp', lambda e, T=T: e.dma_start(out=HFb, in_=HFS[T]), 'hfl', reads=[('HFS', T)], writes=['HFb'])
            for (srcoff, dstb, dk) in [(2048, BT_b, 'BT_b'), (2560, CT_b, 'CT_b')]:
                for g in range(4):
                    tk.op('pe', lambda e, g=g, srcoff=srcoff: e.transpose(out=PT[:, g * 128:(g + 1) * 128],
                                                                         in_=xbc[:, srcoff + g * 128:srcoff + (g + 1) * 128],
                                                                         identity=ident),
                          reads=[('xbc', jj, 4), ('xbc', jj, 5), 'ident'], writes=['pt'], inc=(g == 3))
                tk.op('act', lambda e, dstb=dstb: e.activation(out=dstb, in_=PT, func=AF.Copy),
                      reads=['pt' for g in range(4)], writes=[dk])
            tk.op('pool', lambda e: e.tensor_tensor(out=ysum.rearrange("p (h d) -> p h d", d=64), in0=xs3,
                                                    in1=dsk.unsqueeze(2).to_broadcast([128, 32, 64]), op=ALU.mult),
                  reads=XBK + ['dsk'], writes=['ysum'] + HFk)
            for dr in (1, 0):
                sl0 = dr * 32
                chunk_scalars(dr, True)
                for _i in range(2):
                    next(scg_, None)
                Hb, Hbk = (HRb, 'HRb') if dr == 1 else (HFb, 'HFb')
                if dr == 1:
                    tk.op('act', lambda e: e.activation(out=HRb, in_=HR, func=AF.Copy), reads=Hkeys, writes=['HRb'])
                tri, trik = (triF, 'triF') if dr == 0 else (triR, 'triR')
                for g in range(4):
                    hs = slice(sl0 + g * 8, sl0 + g * 8 + 8)
                    tk.op('pe', lambda e, g=g: e.matmul(PS_[:, 64:192], lhsT=BT_b[:, g * 128:(g + 1) * 128],
                                                        rhs=CT_b[:, g * 128:(g + 1) * 128], start=True, stop=True),
                          reads=['BT_b', 'CT_b'], writes=['ps_'])
                    X3v = X3.rearrange("p (h l) -> p h l", l=128)
                    tk.op('pool', lambda e, hs=hs, tri=tri: e.tensor_tensor(
                        out=X3v, in0=dtA[:, hs].unsqueeze(2).to_broadcast([128, 8, 128]),
                        in1=tri.unsqueeze(1).to_broadcast([128, 8, 128]), op=ALU.mult),
                        reads=['dtA', trik], writes=['X3'])
                    for hf in range(2):
                        tk.op('pe', lambda e, hf=hf: e.matmul(PC[:, hf * 512:(hf + 1) * 512], lhsT=ones_f,
                                                              rhs=X3[:, hf * 512:(hf + 1) * 512], start=True, stop=True),
                              reads=['ones_f', 'X3'], writes=['pc'], inc=(hf == 1))
                    D3v = D3.rearrange("p (h l) -> p h l", l=128)
                    tk.op('dve', lambda e, g=g: e.tensor_tensor(
                        out=D3v, in0=PC.rearrange("p (h l) -> p h l", l=128),
                        in1=cs32[:, g * 8:(g + 1) * 8].unsqueeze(2).to_broadcast([128, 8, 128]), op=ALU.subtract),
                        reads=['pc', 'cs32'], writes=['D3'])
                    if dr == 0:
                        patt, cm = [[0, 8], [1, 128]], -1
                    else:
                        patt, cm = [[0, 8], [-1, 128]], 1
                    tk.op('pool', lambda e, patt=patt, cm=cm: e.affine_select(out=D3v, in_=D3v, pattern=patt, compare_op=ALU.is_ge,
                                                                             fill=tk.getreg(e, -200.0), base=0, channel_multiplier=cm),
                          reads=['D3'], writes=['D3'])
                    tk.op('act', lambda e: e.activation(out=E3, in_=D3, func=AF.Exp), reads=['D3'], writes=['E3'])
                    tk.op('dve', lambda e: e.tensor_tensor(
                        out=MT_b.rearrange("p (h l) -> p h l", l=128), in0=E3.rearrange("p (h l) -> p h l", l=128),
                        in1=PS_[:, 64:192].unsqueeze(1).to_broadcast([128, 8, 128]), op=ALU.mult),
                        reads=['E3', 'ps_'], writes=['MT_b'])
                    for _i in range(3):
                        next(scg_, None)
                    for hh in range(8):
                        hd = g * 8 + hh
                        tk.op('pe', lambda e, hh=hh, hd=hd: e.matmul(PY[:, hh * 64:(hh + 1) * 64], lhsT=MT_b[:, hh * 128:(hh + 1) * 128],
                                                                     rhs=xd_b[:, hd * 64:(hd + 1) * 64], start=True, stop=True),
                              reads=['MT_b', 'xd_b'], writes=['py'], inc=(hh == 7))
                    tk.op('pe', lambda e, g=g, Hb=Hb: e.matmul(PO, lhsT=CT_b[:, g * 128:(g + 1) * 128],
                                                               rhs=Hb[:, g * 512:(g + 1) * 512], start=True, stop=True),
                          reads=['CT_b', Hbk], writes=['po'])
                    tk.op('dve', lambda e, g=g: e.tensor_tensor(
                        out=tA.rearrange("p (h d) -> p h d", d=64), in0=PO.rearrange("p (h d) -> p h d", d=64),
                        in1=ecs32[:, g * 8:(g + 1) * 8].unsqueeze(2).to_broadcast([128, 8, 64]), op=ALU.mult),
                        reads=['po', 'ecs32'], writes=['tA'])
                    tk.op('dve', lambda e: e.tensor_tensor(out=tA, in0=tA, in1=PY, op=ALU.add), reads=['tA', 'py'], writes=['tA'])
                    tk.op('pool', lambda e, g=g: e.tensor_tensor(out=ysum[:, g * 512:(g + 1) * 512],
                                                                 in0=ysum[:, g * 512:(g + 1) * 512], in1=tA, op=ALU.add),
                          reads=['ysum', 'tA'], writes=['ysum'])
                if dr == 1:
                    state_update(HR, 'HR')
            for _ in scg_:
                pass
            zb = [tB, tA]
            zk = ['tB', 'tA']
            zacc = []

            def z_proj():
                a = state['acc'] % 2
                state['acc'] += 1
                wv, wkey = ws.next()
                proj(pacc[a], pacc_k[a], hb, h3, [(1, wv, wkey)])
                zacc.append(a)

            def z_chain(g):
                a = zacc[g]
                tz, kz = zb[g % 2], zk[g % 2]
                ss, rs = st8[:, g % 2:g % 2 + 1], rs8[:, g % 2:g % 2 + 1]
                kss, krs = ('st8z', g % 2), ('rs8z', g % 2)
                tk.op('act', lambda e: e.activation(out=tz, in_=pacc[a], func=AF.Silu), reads=[pacc_k[a]], writes=[kz])
                tk.op('dve', lambda e: e.tensor_tensor(out=tz, in0=tz, in1=ysum[:, g * 512:(g + 1) * 512], op=ALU.mult),
                      reads=[kz, 'ysum'], writes=[kz])
                tk.op('act', lambda e: e.activation(out=vt[0], in_=tz, func=AF.Square, accum_out=ss),
                      reads=[kz], writes=[('vt', 0), kss])
                rstd_from_ss(ss, 512.0, 1, kss, krs, rs)
                tk.op('dve', lambda e: e.tensor_scalar(out=tz, in0=tz, scalar1=rs, scalar2=None, op0=ALU.mult),
                      reads=[kz, krs], writes=[kz])
                transposes_to_yT(tz, kz, g * 4, ssdg)

            z_proj()
            for g in range(4):
                if g + 1 < 4:
                    z_proj()
                z_chain(g)
            for _ in scg_:
                pass
            ypk = [('yT', k) for k in range(32)]
            for nb in range(4):
                a = state['acc'] % 2
                state['acc'] += 1
                for hf in range(2):
                    wv, wkey = ws.next()
                    for kc in range(16):
                        tk.op('pe', lambda e, a=a, hf=hf, kc=kc, wv=wv: e.matmul(
                            pacc[a], lhsT=yT3[:, hf * 16 + kc, :], rhs=wv[:, kc, :], start=(hf == 0 and kc == 0),
                            stop=(hf == 1 and kc == 15)), reads=ypk + [wkey], writes=[pacc_k[a]], inc=(kc == 15))
                tk.op('dve', lambda e, a=a, nb=nb: e.tensor_tensor(out=tA, in0=pacc[a], in1=g1b[:, nb * 512:(nb + 1) * 512], op=ALU.mult),
                      reads=[pacc_k[a], 'g1b'], writes=['tA'])
                if DBG == 1:
                    tk.op('dve', lambda e, a=a, nb=nb: e.tensor_copy(out=u_t[:, nb * 512:(nb + 1) * 512], in_=pacc[a]),
                          reads=[pacc_k[a], 'tA'], writes=['u_t'])
                else:
                    tk.op('dve', lambda e, nb=nb, hb=hb: e.scalar_tensor_tensor(out=u_t[:, nb * 512:(nb + 1) * 512],
                                                                                in0=XT[hb][:, nb * 512:(nb + 1) * 512], scalar=ALPHA, in1=tA,
                                                                                op0=ALU.mult, op1=ALU.add),
                          reads=[('XT', hb), 'tA'], writes=['u_t'])
            if DBG == 3:
                tk.op('dve', lambda e: e.tensor_copy(out=u_t[:, 0:1024], in_=xbc[:, 2048:3072]), reads=[('xbc', jj, 4), ('xbc', jj, 5), 'u_t'], writes=['u_t'])
                tk.op('dve', lambda e: e.tensor_copy(out=u_t[:, 1024:1088], in_=dt64), reads=['dt64', 'u_t'], writes=['u_t'])
                tk.op('dve', lambda e: e.tensor_copy(out=u_t[:, 1088:2048], in_=xbc[:, 0:960]), reads=XBK + ['u_t'], writes=['u_t'])
            if DBG == 2:
                tk.op('dve', lambda e: e.tensor_copy(out=u_t, in_=ysum), reads=['ysum', 'u_t'], writes=['u_t'])
            if DBG == 0:
                layer_norm(tk, u_t, 'u_t', sm, l1g, 'l1g', l1b, 'l1b', xbc[:, 0:2048], XBK)
            tk.dma('pool', lambda e, T=T: e.dma_start(out=X1S[T * 128:(T + 1) * 128, :], in_=u_t), 'x1s',
                   reads=['u_t'], writes=[('X1S', T)])

        for pi in range(8):
            if stage < 3:
                break
            pair = [15 - 2 * pi, 14 - 2 * pi]
            infos = [load_tile(T * 128, T == 0, False, xt=j, ht=j) for j, T in enumerate(pair)]
            xbc_blocks_group(ws, infos, range(6))
            for j, T in enumerate(pair):
                cur['j'] = j
                cur['xb'] = xbcG[j]
                tile_C(T, infos[j][0], infos[j][1], xbcG[j], j)
        tk.seal('x1s')

        tk.barrier()
        ar.off = PERSIST
        if stage >= 4:
            peer_phase(nc, tk, ar, bank, PSM, WS, X1S, modd, kT, EUb, EVb, ln2g_b, ln2b_b, out, ident, iota16)
        else:
            tb = ar.f32(2048)
            for T in range(16):
                tk.dma('sp', lambda e, T=T: e.dma_start(out=tb, in_=X1S[T * 128:(T + 1) * 128, :]), 'dbl',
                       reads=[('X1S', T)], writes=['tb'])
                tk.dma('sp', lambda e, T=T: e.dma_start(out=out[T * 128:(T + 1) * 128, :], in_=tb), 'outst',
                       reads=['tb'], writes=[('out', T)])
        tk.barrier()
        tk.emit()
    return nc


def layer_norm(tk, u, uk, sm, g, gk, b, bk, scratch, scratch_keys):
    mean = sm[:, 224:225]
    ssq = sm[:, 225:226]
    rstd = sm[:, 226:227]
    tk.op('act', lambda e: e.activation(out=scratch, in_=u, func=AF.Identity, accum_out=mean),
          reads=[uk], writes=list(scratch_keys) + ['ln_mean'])
    tk.op('dve', lambda e: e.tensor_scalar(out=mean, in0=mean, scalar1=1.0 / 2048.0, scalar2=None, op0=ALU.mult),
          reads=['ln_mean'], writes=['ln_mean'])
    tk.op('dve', lambda e: e.tensor_scalar(out=u, in0=u, scalar1=mean, scalar2=None, op0=ALU.subtract),
          reads=[uk, 'ln_mean'], writes=[uk])
    tk.op('act', lambda e: e.activation(out=scratch, in_=u, func=AF.Square, accum_out=ssq),
          reads=[uk], writes=list(scratch_keys) + ['ln_ssq'])
    tk.op('dve', lambda e: e.tensor_scalar(out=rstd, in0=ssq, scalar1=1.0 / 2048.0, scalar2=EPS, op0=ALU.mult, op1=ALU.add),
          reads=['ln_ssq'], writes=['ln_rstd'])
    tk.op('act', lambda e: e.activation(out=rstd, in_=rstd, func=AF.Sqrt), reads=['ln_rstd'], writes=['ln_rstd'])
    tk.op('dve', lambda e: e.reciprocal(out=rstd, in_=rstd), reads=['ln_rstd'], writes=['ln_rstd'])
    tk.op('dve', lambda e: e.tensor_scalar(out=u, in0=u, scalar1=rstd, scalar2=None, op0=ALU.mult),
          reads=[uk, 'ln_rstd'], writes=[uk])
    tk.op('dve', lambda e: e.tensor_tensor(out=u, in0=u, in1=g, op=ALU.mult), reads=[uk, gk], writes=[uk])
    tk.op('dve', lambda e: e.tensor_tensor(out=u, in0=u, in1=b, op=ALU.add), reads=[uk, bk], writes=[uk])


def peer_phase(nc, tk, ar, bank, PSM, WS, X1S, modd, kT, eu, ev, ln2g_b, ln2b_b, out, ident, iota16):
    NB = 12
    sc2p = ar.f32(2048)
    sh2 = ar.f32(2048)
    g2b = ar.f32(2048)
    l2g = ar.f32(2048)
    l2b = ar.f32(2048)
    KT_b = ar.bf16(4096)
    acc = ar.f32(2048)
    x1 = [acc, ar.f32(2048)]
    h2 = [ar.f32(2048), ar.f32(2048)]
    h2T = ar.bf16(16 * 128)
    qT = ar.bf16(32 * 128)
    WQ = [ar.bf16(8192)]
    S = ar.f32(2048)
    S2 = ar.f32(2048)
    V16 = ar.f32(256)
    I16 = ar.u32(256)
    I16f = ar.f32(256)
    CAND = ar.f32(2048)
    CAND2 = S2
    SCV = ar.f32(128)
    FL = ar.u32(128)
    ABf = ar.f32(256)
    OH = CAND
    E12 = ar.f32(256)
    IDX = [ar.i32(128), ar.i32(128), ar.i32(128)]
    IDXf = ar.f32(128)
    GATE = [ar.f32(128), ar.f32(128), ar.f32(128)]
    ACT_ = [ar.f32(128), ar.f32(128)]
    COEF = [ar.f32(128), ar.f32(128)]
    sm = ar.f32(512)
    thr16 = sm[:, 16:32]
    UBR = ar.f32(NB * 1024)
    UB = [UBR[:, b * 1024:(b + 1) * 1024].bitcast(BF16) for b in range(NB)]
    DG = [ar.bf16(128) for _ in range(4)]
    junk = OH

    tk.dma('sp', lambda e: e.dma_start(out=sh2, in_=modd[0:1, 6144:8192].partition_broadcast(128)), 'pd', reads=['modd'], writes=['sh2'])
    tk.dma('sp', lambda e: e.dma_start(out=sc2p, in_=modd[0:1, 8192:10240].partition_broadcast(128)), 'pd', reads=['modd'], writes=['sc2p'])
    tk.dma('sp', lambda e: e.dma_start(out=g2b, in_=modd[0:1, 10240:12288].partition_broadcast(128)), 'pd', reads=['modd'], writes=['g2b'])
    tk.dma('sp', lambda e: e.dma_start(out=l2g, in_=ln2g_b[:, :]), 'pd', writes=['l2g'])
    tk.dma('sp', lambda e: e.dma_start(out=l2b, in_=ln2b_b[:, :]), 'pd', writes=['l2b'])
    tk.seal('pd')
    tk.op('dve', lambda e: e.tensor_scalar(out=thr16, in0=iota16, scalar1=16.0, scalar2=16.0, op0=ALU.mult, op1=ALU.add),
          reads=['iota16'], writes=['thr16'])
    tk.op('dve', lambda e: e.tensor_scalar(out=sc2p, in0=sc2p, scalar1=1.0, scalar2=None, op0=ALU.add), reads=['sc2p'], writes=['sc2p'])
    for i in range(2):
        k32 = UBR[:, i * 2048:(i + 1) * 2048]
        tk.dma('sp', lambda e, i=i, k32=k32: e.dma_start(out=k32, in_=kT[:, i * 2048:(i + 1) * 2048]), 'pd2_%d' % i,
               writes=[('UB', 2 * i), ('UB', 2 * i + 1)])
        tk.op('act', lambda e, i=i, k32=k32: e.activation(out=KT_b[:, i * 2048:(i + 1) * 2048], in_=k32, func=AF.Copy),
              reads=[('UB', 2 * i), ('UB', 2 * i + 1)], writes=['KT_b'])
    h2T3 = h2T.rearrange("p (k t) -> p k t", t=128)
    qT3 = qT.rearrange("p (k t) -> p k t", t=128)
    KT3 = KT_b.rearrange("p (k n) -> p k n", n=128)
    PT = bank(2)
    PQ = [bank(0), bank(1)]
    PS3 = bank(3)
    PSC = PSM[:, 4 * 512:8 * 512]
    st = {'qi': 0, 'gi': 0}
    V3 = V16.rearrange("p (h k) -> p h k", k=16)
    I3 = I16.rearrange("p (h k) -> p h k", k=16)
    S3 = S.rearrange("p (h k) -> p h k", k=128)
    S23 = S2.rearrange("p (h k) -> p h k", k=128)
    V4 = V16.rearrange("p (h s k) -> p h s k", s=2, k=16)
    C4 = CAND.rearrange("p (h a b) -> p h a b", a=16, b=16)
    C3 = CAND.rearrange("p (h c) -> p h c", c=256)
    C23 = CAND2.rearrange("p (h c) -> p h c", c=256)
    SC3 = SCV.rearrange("p (h k) -> p h k", k=16)
    FL3 = FL.rearrange("p (h k) -> p h k", k=16)
    I4f = I16f.rearrange("p (h s k) -> p h s k", s=2, k=16)
    OH4 = OH.rearrange("p (h k a) -> p h k a", k=16, a=16)

    def top16(vals, idxs, src, src2, hh, keys):
        kv, ki, ks, ks2 = keys
        tk.op('dve', lambda e: e.max(out=vals[:, hh, 0:8], in_=src[:, hh, :]), reads=[ks], writes=[kv])
        tk.op('dve', lambda e: e.max_index(out=idxs[:, hh, 0:8], in_max=vals[:, hh, 0:8], in_values=src[:, hh, :]),
              reads=[ks, kv], writes=[ki])
        tk.op('dve', lambda e: e.match_replace(out=src2[:, hh, :], in_to_replace=vals[:, hh, 0:8], in_values=src[:, hh, :],
                                               imm_value=NEG), reads=[ks, kv], writes=[ks2])
        tk.op('dve', lambda e: e.max(out=vals[:, hh, 8:16], in_=src2[:, hh, :]), reads=[ks2], writes=[kv])
        tk.op('dve', lambda e: e.max_index(out=idxs[:, hh, 8:16], in_max=vals[:, hh, 8:16], in_values=src2[:, hh, :]),
              reads=[ks2, kv], writes=[ki])

    def stage_A(T):
        p = T % 2
        p3 = T % 3
        x1p, h2p, IDXp, GATEp = x1[0], h2[p], IDX[p3], GATE[p3]
        kx, kh, ki_, kg = 'acc', ('h2', p), ('IDX', p3), ('GATE', p3)
        tk.dma('sp', lambda e: e.dma_start(out=x1p, in_=X1S[T * 128:(T + 1) * 128, :]), 'x1l0', reads=[('X1S', T)], writes=[kx])
        tk.op('dve', lambda e: e.tensor_tensor(out=h2p, in0=x1p, in1=sc2p, op=ALU.mult), reads=[kx, 'sc2p'], writes=[kh])
        tk.op('dve', lambda e: e.tensor_tensor(out=h2p, in0=h2p, in1=sh2, op=ALU.add), reads=[kh, 'sh2'], writes=[kh])
        yield
        for k4 in range(4):
            for q in range(4):
                kc = k4 * 4 + q
                tk.op('pe', lambda e, kc=kc, q=q: e.transpose(out=PT[:, q * 128:(q + 1) * 128], in_=h2p[:, kc * 128:(kc + 1) * 128],
                                                             identity=ident), reads=[kh, 'ident'], writes=['pt'], inc=(q == 3))
            for q in range(4):
                kc = k4 * 4 + q
                tk.op('act', lambda e, kc=kc, q=q: e.activation(out=h2T3[:, kc, :], in_=PT[:, q * 128:(q + 1) * 128], func=AF.Copy),
                      reads=['pt'], writes=[('h2T', kc)])
            yield
        h2Tk = [('h2T', kc) for kc in range(16)]
        for hb in range(8):
            tk.dma('sp', lambda e, hb=hb: e.dma_start(out=WQ[0], in_=WS[blk_q(hb)]), 'wq0',
                   reads=[('WS', blk_q(hb))], writes=[('WQ', 0)])
            wv = WQ[0].rearrange("p (k n) -> p k n", n=512)
            for cc in range(4):
                a = st['qi'] % 2
                st['qi'] += 1
                for kc in range(16):
                    tk.op('pe', lambda e, a=a, kc=kc, cc=cc, wv=wv: e.matmul(PQ[a][:, 0:128], lhsT=wv[:, kc, cc * 128:(cc + 1) * 128],
                                                                           rhs=h2T3[:, kc, :], start=(kc == 0), stop=(kc == 15)),
                          reads=h2Tk + [('WQ', 0)], writes=[('ps', a)], inc=(kc == 15))
                tk.op('act', lambda e, a=a, hb=hb, cc=cc: e.activation(out=qT3[:, hb * 4 + cc, :], in_=PQ[a][:, 0:128], func=AF.Copy),
                      reads=[('ps', a)], writes=[('qT', hb * 4 + cc)])
                yield
        qTk = [('qT', i) for i in range(32)]
        for b4 in range(4):
            for h4 in range(4):
                hh = b4 * 4 + h4
                for jc in range(2):
                    tk.op('pe', lambda e, hh=hh, h4=h4, jc=jc: e.matmul(PS3[:, h4 * 128:(h4 + 1) * 128], lhsT=qT3[:, hh * 2 + jc, :],
                                                                      rhs=KT3[:, hh * 2 + jc, :], start=(jc == 0), stop=(jc == 1)),
                          reads=qTk + ['KT_b'], writes=['ps3'], inc=(h4 == 3 and jc == 1))
            tk.op('act', lambda e, b4=b4: e.activation(out=S[:, b4 * 512:(b4 + 1) * 512], in_=PS3, func=AF.Copy),
                  reads=['ps3'], writes=['S'])
            yield
        for hh in range(16):
            top16(V3, I3, S3, S23, hh, ('V16', 'I16', 'S', 'S2'))
            yield
        tk.op('dve', lambda e: e.tensor_tensor(out=C4, in0=V4[:, :, 0, :].unsqueeze(3).to_broadcast([128, 8, 16, 16]),
                                               in1=V4[:, :, 1, :].unsqueeze(2).to_broadcast([128, 8, 16, 16]), op=ALU.add),
              reads=['V16'], writes=['CAND'])
        yield
        for h in range(8):
            top16(SC3, FL3, C3, C23, h, ('SCV', 'FL', 'CAND', 'S2'))
            yield
        tk.op('dve', lambda e: e.tensor_copy(out=ABf[:, 128:256], in_=FL), reads=['FL'], writes=['ABf'])
        tk.op('dve', lambda e: e.tensor_tensor(out=OH.rearrange("p (hk a) -> p hk a", a=16),
                                               in0=ABf[:, 128:256].unsqueeze(2).to_broadcast([128, 128, 16]),
                                               in1=thr16.unsqueeze(1).to_broadcast([128, 128, 16]), op=ALU.is_ge),
              reads=['ABf', 'thr16'], writes=['CAND'])
        tk.op('dve', lambda e: e.tensor_reduce(out=ABf[:, 0:128], in_=OH.rearrange("p (hk a) -> p hk a", a=16), axis=AX.X,
                                               op=ALU.add), reads=['CAND'], writes=['ABf'])
        tk.op('dve', lambda e: e.scalar_tensor_tensor(out=ABf[:, 128:256], in0=ABf[:, 0:128], scalar=-16.0, in1=ABf[:, 128:256],
                                                      op0=ALU.mult, op1=ALU.add), reads=['ABf'], writes=['ABf'])
        tk.op('dve', lambda e: e.tensor_copy(out=I16f, in_=I16), reads=['I16'], writes=['I16f'])
        yield
        for s_ in range(2):
            ab = ABf[:, s_ * 128:(s_ + 1) * 128].rearrange("p (h k) -> p h k", k=16)
            tk.op('dve', lambda e, ab=ab: e.tensor_tensor(out=OH4, in0=ab.unsqueeze(3).to_broadcast([128, 8, 16, 16]),
                                                          in1=iota16.unsqueeze(1).unsqueeze(1).to_broadcast([128, 8, 16, 16]),
                                                          op=ALU.is_equal), reads=['ABf', 'iota16'], writes=['CAND'])
            tk.op('dve', lambda e, s_=s_: e.tensor_tensor(out=OH4, in0=OH4,
                                                          in1=I4f[:, :, s_, :].unsqueeze(2).to_broadcast([128, 8, 16, 16]),
                                                          op=ALU.mult), reads=['CAND', 'I16f'], writes=['CAND'])
            tk.op('dve', lambda e, s_=s_: e.tensor_reduce(out=E12[:, s_ * 128:(s_ + 1) * 128],
                                                          in_=OH.rearrange("p (hk a) -> p hk a", a=16), axis=AX.X, op=ALU.add),
                  reads=['CAND'], writes=['E12'])
            yield
        tk.op('dve', lambda e: e.scalar_tensor_tensor(out=IDXf, in0=E12[:, 0:128], scalar=128.0, in1=E12[:, 128:256],
                                                      op0=ALU.mult, op1=ALU.add), reads=['E12'], writes=['IDXf'])
        tk.op('dve', lambda e: e.tensor_copy(out=IDXp, in_=IDXf), reads=['IDXf'], writes=[ki_])
        G3 = GATEp.rearrange("p (h k) -> p h k", k=16)
        tk.op('dve', lambda e: e.tensor_tensor(out=G3, in0=SC3, in1=SC3[:, :, 0:1].to_broadcast([128, 8, 16]), op=ALU.subtract),
              reads=['SCV'], writes=[kg])
        tk.op('act', lambda e: e.activation(out=GATEp, in_=GATEp, func=AF.Exp), reads=[kg], writes=[kg])
        tk.op('dve', lambda e: e.tensor_reduce(out=sm[:, 0:8], in_=G3, axis=AX.X, op=ALU.add), reads=[kg], writes=['gsum'])
        tk.op('dve', lambda e: e.reciprocal(out=sm[:, 0:8], in_=sm[:, 0:8]), reads=['gsum'], writes=['gsum'])
        tk.op('dve', lambda e: e.tensor_tensor(out=G3, in0=G3, in1=sm[:, 0:8].unsqueeze(2).to_broadcast([128, 8, 16]), op=ALU.mult),
              reads=[kg, 'gsum'], writes=[kg])
        yield

    def u_slot(T, sl):
        p, p3 = T % 2, T % 3
        b = st['gi'] % NB
        st['gi'] += 1
        IDXp, h2p, ACTp = IDX[p3], h2[p], ACT_[p]
        tk.dma('pool', lambda e: e.indirect_dma_start(
            out=UB[b], out_offset=None, in_=eu[:, :], in_offset=bass.IndirectOffsetOnAxis(ap=IDXp[:, sl:sl + 1], axis=0)),
            'ub%d' % b, reads=[('IDX', p3)], writes=[('UB', b)])
        tk.op('dve', lambda e: e.scalar_tensor_tensor(out=UB[b], in0=UB[b], scalar=1.0, in1=h2p, op0=ALU.mult,
                                                      op1=ALU.mult, accum_out=ACTp[:, sl:sl + 1]),
              reads=[('UB', b), ('h2', p)], writes=[('UB', b), ('ACT', p, sl)])

    def u_finish(T):
        p, p3 = T % 2, T % 3
        ACTp, COEFp, GATEp = ACT_[p], COEF[p], GATE[p3]
        tk.op('act', lambda e: e.activation(out=COEFp, in_=ACTp, func=AF.Gelu), reads=[('ACT', p, sl) for sl in range(128)],
              writes=[('COEF', p)])
        tk.op('dve', lambda e: e.tensor_tensor(out=COEFp, in0=COEFp, in1=GATEp, op=ALU.mult),
              reads=[('COEF', p), ('GATE', p3)], writes=[('COEF', p)])

    def v_slot(T, sl):
        p, p3 = T % 2, T % 3
        b = st['gi'] % NB
        st['gi'] += 1
        IDXp, COEFp = IDX[p3], COEF[p]
        tk.dma('pool', lambda e: e.indirect_dma_start(
            out=UB[b], out_offset=None, in_=ev[:, :], in_offset=bass.IndirectOffsetOnAxis(ap=IDXp[:, sl:sl + 1], axis=0)),
            'ub%d' % b, reads=[('IDX', p3)], writes=[('UB', b)])
        if sl % 4 == 3:
            accd = x1[1]
            if sl == 3:
                tk.op('dve', lambda e: e.tensor_scalar(out=accd, in0=UB[b], scalar1=COEFp[:, sl:sl + 1], scalar2=None, op0=ALU.mult),
                      reads=[('UB', b), ('COEF', p)], writes=[('x1', 1)])
            else:
                tk.op('dve', lambda e: e.scalar_tensor_tensor(out=accd, in0=UB[b], scalar=COEFp[:, sl:sl + 1], in1=accd,
                                                              op0=ALU.mult, op1=ALU.add),
                      reads=[('UB', b), ('COEF', p), ('x1', 1)], writes=[('x1', 1)])
            return
        dj = sl % 4
        tk.op('act', lambda e: e.activation(out=DG[dj], in_=ident, func=AF.Copy, scale=COEFp[:, sl:sl + 1]),
              reads=['ident', ('COEF', p)], writes=[('DG', dj)])
        for nb in range(4):
            tk.op('pe', lambda e, nb=nb: e.matmul(PSC[:, nb * 512:(nb + 1) * 512], lhsT=DG[dj],
                                                  rhs=UB[b][:, nb * 512:(nb + 1) * 512], start=(sl == 0), stop=(sl == 126)),
                  reads=[('DG', dj), ('UB', b)], writes=['psc'], inc=(nb == 3))

    def v_finish(T):
        x1f = x1[1]
        tk.op('dve', lambda e: e.tensor_tensor(out=acc, in0=PSC, in1=x1f, op=ALU.add), reads=['psc', ('x1', 1)], writes=['acc'])
        tk.dma('sp', lambda e: e.dma_start(out=x1f, in_=X1S[T * 128:(T + 1) * 128, :]), 'x1l1', reads=[('X1S', T)], writes=[('x1', 1)])
        tk.op('dve', lambda e: e.tensor_tensor(out=acc, in0=acc, in1=g2b, op=ALU.mult), reads=['acc', 'g2b'], writes=['acc'])
        tk.op('dve', lambda e: e.scalar_tensor_tensor(out=acc, in0=x1f, scalar=ALPHA, in1=acc, op0=ALU.mult, op1=ALU.add),
              reads=[('x1', 1), 'acc'], writes=['acc'])
        layer_norm(tk, acc, 'acc', sm, l2g, 'l2g', l2b, 'l2b', PSC, ['psc'])
        tk.dma('sp', lambda e: e.dma_start(out=out[T * 128:(T + 1) * 128, :], in_=acc), 'outst',
               reads=['acc'], writes=[('out', T)])

    for i in range(NT + 2):
        genA = stage_A(i) if i < NT else iter(())
        tu = i - 1 if 0 <= i - 1 < NT else None
        tv = i - 2 if 0 <= i - 2 < NT else None
        if tu is None and tv is None:
            for _ in genA:
                pass
            continue
        for sl in range(128):
            if tv is not None:
                v_slot(tv, sl)
            if tu is not None:
                u_slot(tu, sl)
            if sl % 2 == 1 or (sl % 10 == 0 and sl > 0):
                next(genA, None)
        for _ in genA:
            pass
        if tu is not None:
            u_finish(tu)
        if tv is not None:
            v_finish(tv)


_CACHE = {}


def make_inputs(inputs, core):
    b, half = core // 2, core % 2
    flip = half == 1
    f = np.float32
    x = np.asarray(inputs['x'])[b]
    if flip:
        x = x[::-1]
    xp = np.zeros((4098, 2048), f)
    xp[1:4097] = x
    w_in = np.ascontiguousarray(np.asarray(inputs['w_in'])[0], dtype=f)
    wd = w_in[:, ODT:ODT + 64]
    cw = np.asarray(inputs['conv_ssd_w'])[0]
    scw = np.asarray(inputs['short_conv_w'])[0]
    dbf, dbb = np.asarray(inputs['dt_bias_f'])[0], np.asarray(inputs['dt_bias_b'])[0]
    alf, alb = np.asarray(inputs['a_log_f'])[0], np.asarray(inputs['a_log_b'])[0]
    if flip:
        wd = np.concatenate([wd[:, 32:64], wd[:, 0:32]], axis=1)
        cw = cw[::-1]
        scw = scw[::-1]
        dtb = np.concatenate([dbb, dbf])
        alog = np.concatenate([alb, alf])
    else:
        dtb = np.concatenate([dbf, dbb])
        alog = np.concatenate([alf, alb])

    def bc(v):
        v = np.asarray(v, dtype=f).reshape(1, -1)
        return np.ascontiguousarray(np.broadcast_to(v, (128, v.shape[1])))

    def fm(v):
        return np.ascontiguousarray(np.asarray(v, dtype=f).reshape(16, 128).T)

    sk = np.asarray(inputs['sub_keys'])[0]
    kT = sk.reshape(8, 2, 128, 2, 128).transpose(4, 0, 1, 3, 2).reshape(128, 4096)
    return {
        'xp': xp,
        'cT': fm(np.asarray(inputs['c'])[b]),
        'w_ada': np.ascontiguousarray(np.asarray(inputs['w_ada'])[0], dtype=f),
        'b_ada': np.ascontiguousarray(np.asarray(inputs['b_ada'])[0:1], dtype=f),
        'w_in': w_in,
        'w_dt': np.ascontiguousarray(wd, dtype=f),
        'cwb': bc(np.ascontiguousarray(cw).reshape(-1)),
        'scwb': bc(np.ascontiguousarray(scw).reshape(-1)),
        'cbias': np.ascontiguousarray(np.asarray(inputs['conv_ssd_b'])[0:1], dtype=f),
        'dtb_b': bc(dtb),
        'alog_b': bc(alog),
        'dsk_b': bc(np.asarray(inputs['d_skip'])[0]),
        'ssdgT': fm(np.asarray(inputs['ssd_norm_g'])[0]),
        'scgT': fm(np.asarray(inputs['sc_norm_g'])[0]),
        'w_out': np.ascontiguousarray(np.asarray(inputs['w_out'])[0], dtype=f),
        'ln1g_b': bc(np.asarray(inputs['ln1_g'])[0]),
        'ln1b_b': bc(np.asarray(inputs['ln1_b'])[0]),
        'w_query': np.ascontiguousarray(np.asarray(inputs['w_query'])[0], dtype=f),
        'kT': np.ascontiguousarray(kT, dtype=f),
        'eu': np.ascontiguousarray(np.asarray(inputs['expert_u'])[0], dtype=f),
        'ev': np.ascontiguousarray(np.asarray(inputs['expert_v'])[0], dtype=f),
        'ln2g_b': bc(np.asarray(inputs['ln2_g'])[0]),
        'ln2b_b': bc(np.asarray(inputs['ln2_b'])[0]),
    }


def kernel(_stage=99, _cores=8, **inputs):
    if _stage not in _CACHE:
        _CACHE[_stage] = build(_stage)
    nc = _CACHE[_stage]
    in_maps = [make_inputs(inputs, c) for c in range(_cores)]
    res = run_bass_kernel_spmd(nc, in_maps, core_ids=list(range(_cores)))
    outp = np.zeros((4, 4096, 2048), np.float32)
    for c in range(_cores):
        b, half = c // 2, c % 2
        o = np.asarray(res.results[c]['out'])
        if half == 0:
            outp[b, 0:2048] = o
        else:
            outp[b, 2048:4096] = o[::-1]
    return outp
```

```python
import numpy as np
import concourse.bass as bass
import concourse.mybir as mybir
from concourse.bass_utils import run_bass_kernel_spmd
from contextlib import ExitStack

F32 = mybir.dt.float32
BF16 = mybir.dt.bfloat16
I32 = mybir.dt.int32
U32 = mybir.dt.uint32
AF = mybir.ActivationFunctionType
ALU = mybir.AluOpType
AX = mybir.AxisListType

ENGS = ['pe', 'act', 'dve', 'pool', 'sp']
ALPHA = 2.0 ** 0.25
EPS = 1e-5
NT = 16
NEG = -1.0e30


class Tracker:
    def __init__(self, nc, es):
        self.nc = nc
        self.es = es
        self.stream = {e: [] for e in ENGS}
        self.sems = {}
        self.cnt = {}
        self.seen = {e: {} for e in ENGS}
        self.lastw = {}
        self.readers = {}
        self.chan_keys = {}
        for e in ENGS:
            self._sem('E_' + e)

    def _sem(self, name):
        if name not in self.sems:
            self.sems[name] = self.es.enter_context(self.nc.semaphore(name))
            self.cnt[name] = 0
        return name

    def _deps(self, reads, writes):
        deps = []
        for k in reads:
            if k in self.lastw:
                deps.append(self.lastw[k])
        for k in writes:
            if k in self.lastw:
                deps.append(self.lastw[k])
            deps.extend(self.readers.get(k, {}).items())
        return deps

    def _emit_waits(self, eng, deps, skip_sem=None):
        best = {}
        for (s, v) in deps:
            if s == skip_sem:
                continue
            if v > best.get(s, 0):
                best[s] = v
        for s, v in best.items():
            if self.seen[eng].get(s, 0) < v:
                self.stream[eng].append(('wait', s, v))
                self.seen[eng][s] = v

    def _commit(self, token, reads, writes):
        for k in writes:
            self.lastw[k] = token
            self.readers[k] = {}
        for k in reads:
            d = self.readers.setdefault(k, {})
            if d.get(token[0], 0) < token[1]:
                d[token[0]] = token[1]

    def op(self, eng, fn, reads=(), writes=(), inc=True):
        s = 'E_' + eng
        deps = self._deps(reads, writes)
        self._emit_waits(eng, deps, skip_sem=(s if eng == 'pe' else None))
        if inc:
            self.cnt[s] += 1
            token = (s, self.cnt[s])
        else:
            token = (s, self.cnt[s] + 1)
        self.stream[eng].append(('op', fn, (s, 1) if inc else None))
        self._commit(token, reads, writes)
        return token

    def dma(self, q, fn, chan, reads=(), writes=()):
        s = self._sem('D_' + chan)
        deps = self._deps(reads, writes)
        self._emit_waits(q, deps)
        self.cnt[s] += 16
        token = (s, self.cnt[s])
        self.stream[q].append(('op', fn, (s, 16)))
        self._commit(token, reads, writes)
        self.chan_keys.setdefault(chan, set()).update(writes)
        return token

    def seal(self, chan):
        s = 'D_' + chan
        if s not in self.cnt:
            return
        tok = (s, self.cnt[s])
        for k in self.chan_keys.get(chan, ()):
            if k in self.lastw and self.lastw[k][0] == s:
                self.lastw[k] = tok

    def barrier(self):
        for e in ENGS:
            for s, c in self.cnt.items():
                if c > 0 and self.seen[e].get(s, 0) < c:
                    self.stream[e].append(('wait', s, c))
                    self.seen[e][s] = c

    def getreg(self, eng, val):
        if not hasattr(self, '_regs'):
            self._regs = {}
        if val not in self._regs:
            self._regs[val] = eng.to_reg(val)
        return self._regs[val]

    def emit(self):
        nc = self.nc
        tk = self

        def run(engname):
            def body(eng):
                for it in tk.stream[engname]:
                    if it[0] == 'wait':
                        eng.wait_ge(tk.sems[it[1]], it[2])
                    else:
                        ins = it[1](eng)
                        if it[2] is not None:
                            ins.then_inc(tk.sems[it[2][0]], it[2][1])
            return body

        with nc.Block() as block:
            block.tensor(run('pe'))
            block.scalar(run('act'))
            block.vector(run('dve'))
            block.gpsimd(run('pool'))
            block.sync(run('sp'))


class Arena:
    def __init__(self, ap):
        self.ap = ap
        self.off = 0
        self.N = ap.shape[1]

    def f32(self, n):
        a = self.ap[:, self.off:self.off + n]
        self.off += n
        assert self.off <= self.N, ("arena overflow", self.off, self.N)
        return a

    def bf16(self, n):
        m = (n + 1) // 2
        return self.f32(m).bitcast(BF16)

    def i32(self, n):
        return self.f32(n).bitcast(I32)

    def u32(self, n):
        return self.f32(n).bitcast(U32)


OZ, OXBC, ODT, OGB, OGC, OV = 0, 2048, 5120, 5184, 7232, 9280
NBLK = 58


def blk_xbc(cb, tap):
    return cb * 3 + tap


def blk_z(g):
    return 18 + g


def blk_sc(j, k):
    return 22 + j * 5 + k


def blk_out(nb, hf):
    return 42 + nb * 2 + hf


def blk_q(hb):
    return 50 + hb


def build(stage=99):
    import os
    SUB = int(os.environ.get('K_SUB', '9'))
    DBG = int(os.environ.get('K_DBG', '0'))
    nc = bass.Bass("TRN2", target_bir_lowering=False)

    def din(name, shape, dt=F32):
        return nc.dram_tensor(name, shape, dt, kind="ExternalInput").ap()

    xp = din("xp", [4098, 2048])
    cT = din("cT", [128, 16])
    w_ada = din("w_ada", [2048, 12288])
    b_ada = din("b_ada", [1, 12288])
    w_in = din("w_in", [2048, 11328])
    w_dt = din("w_dt", [2048, 64])
    cwb = din("cwb", [128, 9216])
    scwb = din("scwb", [128, 6144])
    cbias = din("cbias", [1, 3072])
    dtb_b = din("dtb_b", [128, 64])
    alog_b = din("alog_b", [128, 64])
    dsk_b = din("dsk_b", [128, 32])
    ssdgT = din("ssdgT", [128, 16])
    scgT = din("scgT", [128, 16])
    w_out = din("w_out", [4096, 2048])
    ln1g_b = din("ln1g_b", [128, 2048])
    ln1b_b = din("ln1b_b", [128, 2048])
    w_query = din("w_query", [2048, 4096])
    kT = din("kT", [128, 4096])
    eu = din("eu", [16384, 2048])
    ev = din("ev", [16384, 2048])
    ln2g_b = din("ln2g_b", [128, 2048])
    ln2b_b = din("ln2b_b", [128, 2048])
    out = nc.dram_tensor("out", [2048, 2048], F32, kind="ExternalOutput").ap()
    WS = nc.dram_tensor("WS", [NBLK, 128, 16 * 512], BF16, kind="Internal").ap()
    modd = nc.dram_tensor("modd", [1, 12288], F32, kind="Internal").ap()
    HFS = nc.dram_tensor("HFS", [NT, 128, 2048], BF16, kind="Internal").ap()
    X1S = nc.dram_tensor("X1S", [2048, 2048], F32, kind="Internal").ap()
    EUb = nc.dram_tensor("EUb", [16384, 2048], BF16, kind="Internal").ap()
    EVb = nc.dram_tensor("EVb", [16384, 2048], BF16, kind="Internal").ap()

    es = ExitStack()
    with es:
        tk = Tracker(nc, es)
        ARN = es.enter_context(nc.sbuf_tensor("arena", [128, 52900], F32))
        PSM = es.enter_context(nc.psum_tensor("psm", [128, 4096], F32))
        ar = Arena(ARN[:, :])

        def bank(i, n=512):
            return PSM[:, i * 512:i * 512 + n]

        ident = ar.f32(128)
        ones_f = ar.f32(128)
        triF = ar.f32(128)
        triR = ar.f32(128)
        ones_b = ar.bf16(128)
        sc1pT = ar.f32(16)
        sh1T = ar.f32(16)
        dtb = ar.f32(64)
        a_neg = ar.f32(64)
        dsk = ar.f32(32)
        ssdg = ar.f32(16)
        scg = ar.f32(16)
        iota16 = ar.f32(16)
        bias2 = ar.bf16(3072)
        wdt_sb = ar.bf16(16 * 64)
        PERSIST = ar.off

        tk.op('pool', lambda e: e.memset(ident, 0.0), writes=['ident'])
        tk.op('pool', lambda e: e.affine_select(out=ident, in_=ident, pattern=[[-1, 128]], compare_op=ALU.not_equal,
                                                fill=tk.getreg(e, 1.0), base=0, channel_multiplier=1), reads=['ident'], writes=['ident'])
        tk.op('pool', lambda e: e.memset(ones_f, 1.0), writes=['ones_f'])
        tk.op('pool', lambda e: e.memset(ones_b, 1.0), writes=['ones_b'])
        tk.op('pool', lambda e: e.memset(triF, 1.0), writes=['triF'])
        tk.op('pool', lambda e: e.affine_select(out=triF, in_=triF, pattern=[[1, 128]], compare_op=ALU.is_ge,
                                                fill=tk.getreg(e, 0.0), base=0, channel_multiplier=-1), reads=['triF'], writes=['triF'])
        tk.op('pool', lambda e: e.memset(triR, 1.0), writes=['triR'])
        tk.op('pool', lambda e: e.affine_select(out=triR, in_=triR, pattern=[[-1, 128]], compare_op=ALU.is_ge,
                                                fill=tk.getreg(e, 0.0), base=0, channel_multiplier=1), reads=['triR'], writes=['triR'])
        tk.op('pool', lambda e: e.iota(iota16, pattern=[[1, 16]], base=0, channel_multiplier=0,
                                       allow_small_or_imprecise_dtypes=True), writes=['iota16'])
        for (dst, src, nm) in [(dtb, dtb_b, 'dtb'), (dsk, dsk_b, 'dsk'), (ssdg, ssdgT, 'ssdg'), (scg, scgT, 'scg')]:
            tk.dma('sp', lambda e, dst=dst, src=src: e.dma_start(out=dst, in_=src[:, :]), 'par', writes=[nm])
        tk.dma('sp', lambda e: e.dma_start(out=a_neg, in_=alog_b[:, :]), 'par', writes=['a_neg'])
        tk.seal('par')
        tk.op('act', lambda e: e.activation(out=a_neg, in_=a_neg, func=AF.Exp), reads=['a_neg'], writes=['a_neg'])
        tk.op('dve', lambda e: e.tensor_scalar(out=a_neg, in0=a_neg, scalar1=-1.0, scalar2=None, op0=ALU.mult),
              reads=['a_neg'], writes=['a_neg'])

        P0 = ar.off
        cs_ = ar.f32(16)
        bada = ar.f32(12288)
        wa = [ar.f32(4096), ar.f32(4096)]
        stg = ar.f32(4096)
        tk.dma('sp', lambda e: e.dma_start(out=cs_, in_=cT[:, :]), 'p0', writes=['cs_'])
        tk.dma('sp', lambda e: e.dma_start(out=bada[0:1, :], in_=b_ada[:, :]), 'p0', writes=['bada'])
        tk.seal('p0')
        tk.op('act', lambda e: e.activation(out=cs_, in_=cs_, func=AF.Silu), reads=['cs_'], writes=['cs_'])
        it = 0
        for g in range(3):
            for kc in range(16):
                b = it % 2
                it += 1
                tk.dma('sp', lambda e, b=b, kc=kc, g=g: e.dma_start(
                    out=wa[b], in_=w_ada[kc * 128:(kc + 1) * 128, g * 4096:(g + 1) * 4096]), 'wa%d' % b, writes=[('wa', b)])
                for j in range(8):
                    tk.op('pe', lambda e, b=b, kc=kc, j=j: e.matmul(bank(j)[0:1, :], lhsT=cs_[:, kc:kc + 1],
                                                                   rhs=wa[b][:, j * 512:(j + 1) * 512],
                                                                   start=(kc == 0), stop=(kc == 15)),
                          reads=['cs_', ('wa', b)], writes=[('ps', j)], inc=(j == 7))
            for j in range(8):
                tk.op('dve', lambda e, j=j, g=g: e.tensor_tensor(out=stg[0:1, j * 512:(j + 1) * 512], in0=bank(j)[0:1, :],
                                                                 in1=bada[0:1, g * 4096 + j * 512:g * 4096 + (j + 1) * 512],
                                                                 op=ALU.add),
                      reads=[('ps', j), 'bada'], writes=['stg'])
            tk.dma('sp', lambda e, g=g: e.dma_start(out=modd[0:1, g * 4096:(g + 1) * 4096], in_=stg[0:1, :]), 'modst',
                   reads=['stg'], writes=['modd'])
        tk.seal('modst')
        t16 = ar.f32(128)
        for (dst, off, nm, addone) in [(sh1T, 0, 'sh1T', False), (sc1pT, 2048, 'sc1pT', True)]:
            tk.dma('sp', lambda e, off=off: e.dma_start(out=t16[0:16, :],
                                                        in_=modd[0, off:off + 2048].rearrange("(k p) -> k p", p=128)),
                   'm16', reads=['modd'], writes=['t16'])
            tk.op('pe', lambda e: e.transpose(out=bank(0)[:, 0:16], in_=t16[0:16, :], identity=ident[0:16, 0:16]),
                  reads=['t16', 'ident'], writes=[('ps', 0)])
            if addone:
                tk.op('dve', lambda e, dst=dst: e.tensor_scalar(out=dst, in0=bank(0)[:, 0:16], scalar1=1.0, scalar2=None,
                                                                op0=ALU.add), reads=[('ps', 0)], writes=[nm])
            else:
                tk.op('dve', lambda e, dst=dst: e.tensor_copy(out=dst, in_=bank(0)[:, 0:16]), reads=[('ps', 0)], writes=[nm])
        cb32 = ar.f32(3072)
        cbt = ar.f32(3072)
        tk.dma('sp', lambda e: e.dma_start(out=cb32[0:1, :], in_=cbias[:, :]), 'p0b', writes=['cb32'])
        tk.op('dve', lambda e: e.tensor_copy(out=bias2[0:1, :], in_=cb32[0:1, :]), reads=['cb32'], writes=['bias2'])
        tk.op('dve', lambda e: e.tensor_tensor(out=cbt[0:1, :], in0=cb32[0:1, :], in1=bias2[0:1, :], op=ALU.subtract),
              reads=['cb32', 'bias2'], writes=['cbt'])
        lo_d = nc.dram_tensor("lo_d", [1, 3072], BF16, kind="Internal").ap()
        lo16 = ar.bf16(3072)
        tk.op('dve', lambda e: e.tensor_copy(out=lo16[0:1, :], in_=cbt[0:1, :]), reads=['cbt'], writes=['lo16'])
        tk.dma('sp', lambda e: e.dma_start(out=lo_d[:, :], in_=lo16[0:1, :]), 'p0c', reads=['lo16'], writes=['lo_d'])
        tk.dma('sp', lambda e: e.dma_start(out=bias2[1:2, :], in_=lo_d[:, :]), 'p0d', reads=['lo_d', 'bias2'], writes=['bias2'])
        wdt32 = ar.f32(16 * 64)
        tk.dma('sp', lambda e: e.dma_start(out=wdt32.rearrange("p (k n) -> p k n", n=64),
                                           in_=w_dt.rearrange("(k p) n -> p k n", p=128)), 'p0e', writes=['wdt32'])
        tk.op('dve', lambda e: e.tensor_copy(out=wdt_sb, in_=wdt32), reads=['wdt32'], writes=['wdt_sb'])

        tk.barrier()
        ar.off = PERSIST
        cw_sb = ar.f32(9216)
        scw_sb = ar.f32(6144)
        wst = [ar.f32(8192), ar.f32(8192)]
        wcs = [ar.bf16(8192), ar.bf16(8192)]
        tk.dma('sp', lambda e: e.dma_start(out=cw_sb, in_=cwb[:, :]), 'pa', writes=['cw_sb'])
        tk.dma('sp', lambda e: e.dma_start(out=scw_sb, in_=scwb[:, :]), 'pa', writes=['scw_sb'])
        tk.seal('pa')
        bases = []
        for cb in range(6):
            bases.append((w_in[:, OXBC + cb * 512:OXBC + (cb + 1) * 512],
                          [(blk_xbc(cb, t), cw_sb[:, t * 3072 + cb * 512:t * 3072 + (cb + 1) * 512]) for t in range(3)]))
        for g in range(4):
            bases.append((w_in[:, OZ + g * 512:OZ + (g + 1) * 512], [(blk_z(g), None)]))
        for j in range(4):
            bases.append((w_in[:, OGB + j * 512:OGB + (j + 1) * 512], [(blk_sc(j, 0), None)]))
            bases.append((w_in[:, OGC + j * 512:OGC + (j + 1) * 512],
                          [(blk_sc(j, 1 + t), scw_sb[:, t * 2048 + j * 512:t * 2048 + (j + 1) * 512]) for t in range(3)]))
            bases.append((w_in[:, OV + j * 512:OV + (j + 1) * 512], [(blk_sc(j, 4), None)]))
        for nb in range(4):
            for hf in range(2):
                bases.append((w_out[hf * 2048:(hf + 1) * 2048, nb * 512:(nb + 1) * 512], [(blk_out(nb, hf), None)]))
        for hb in range(8):
            bases.append((w_query[:, hb * 512:(hb + 1) * 512], [(blk_q(hb), None)]))
        ci = 0
        for bi, (src, ders) in enumerate(bases if stage >= 1 else []):
            sb_ = bi % 2
            n_ = src.shape[1]
            tk.dma('sp', lambda e, sb_=sb_, src=src, n_=n_: e.dma_start(out=wst[sb_].rearrange("p (k n) -> p k n", n=n_),
                                                                 in_=src.rearrange("(k p) n -> p k n", p=128)),
                   'wst%d' % sb_, writes=[('wst', sb_)])
            for (bid, scl) in ders:
                cbuf = ci % 2
                ci += 1
                if scl is None:
                    eng = ['act', 'dve', 'pool'][ci % 3] if isinstance(bid, tuple) else 'act'
                    if eng == 'act':
                        tk.op('act', lambda e, sb_=sb_, cbuf=cbuf: e.activation(out=wcs[cbuf], in_=wst[sb_], func=AF.Copy),
                              reads=[('wst', sb_)], writes=[('wcs', cbuf)])
                    else:
                        tk.op(eng, lambda e, sb_=sb_, cbuf=cbuf: e.tensor_copy(out=wcs[cbuf], in_=wst[sb_]),
                              reads=[('wst', sb_)], writes=[('wcs', cbuf)])
                else:
                    eng = 'dve' if (ci % 2 == 0) else 'pool'
                    tk.op(eng, lambda e, sb_=sb_, cbuf=cbuf, scl=scl: e.tensor_tensor(
                        out=wcs[cbuf].rearrange("p (k n) -> p k n", n=512),
                        in0=wst[sb_].rearrange("p (k n) -> p k n", n=512),
                        in1=scl.unsqueeze(1).to_broadcast([128, 16, 512]), op=ALU.mult),
                        reads=[('wst', sb_), 'cw_sb', 'scw_sb'], writes=[('wcs', cbuf)])
                if isinstance(bid, tuple):
                    dst, nm = bid
                    tk.dma('act', lambda e, dst=dst, cbuf=cbuf, n_=n_: e.dma_start(
                        out=dst.rearrange("(k p) n -> p k n", p=128), in_=wcs[cbuf].rearrange("p (k n) -> p k n", n=n_)), 'wsst%d' % cbuf,
                        reads=[('wcs', cbuf)], writes=[nm])
                else:
                    tk.dma('act', lambda e, bid=bid, cbuf=cbuf: e.dma_start(out=WS[bid], in_=wcs[cbuf]), 'wsst%d' % cbuf,
                           reads=[('wcs', cbuf)], writes=[('WS', bid)])
        tk.seal('wsst0')
        tk.seal('wsst1')

        tk.barrier()
        ar.off = PERSIST
        NWB = 2
        WB = [ar.bf16(8192) for _ in range(NWB)]
        XT = [ar.f32(2048), ar.f32(2048)]
        XH = [ar.f32(256), ar.f32(256)]
        hT = [ar.bf16(16 * 130) for _ in range(4)]
        xbc0 = ar.f32(3072)
        xbc1 = ar.f32(3072)
        cur = {'j': 0, 'xb': xbc0}
        ysum = ar.f32(2048)
        xd_b = ar.bf16(2048)
        xdd_b = ar.bf16(2048)
        B_b = ar.bf16(512)
        BT_b = ar.bf16(512)
        CT_b = ar.bf16(512)
        HR = ar.f32(2048)
        HRb = ar.bf16(2048)
        HFb = ar.bf16(2048)
        off_x3 = ar.off
        X3 = ar.f32(1024)
        D3 = ar.f32(1024)
        E3 = ar.f32(1024)
        MT_b = ar.bf16(1024)
        yT = ar.bf16(32 * 128)
        R6 = ar.f32(6144)
        g1b, l1g, l1b = R6[:, 0:2048], R6[:, 2048:4096], R6[:, 4096:6144]
        xbcG = [xbc0, xbc1, R6[:, 0:3072], R6[:, 3072:6144]]
        off_ta = ar.off
        tA = ar.f32(512)
        tB = ar.f32(512)
        vt = [ar.f32(512) for _ in range(3)]
        u_t = ar.f32(2048)
        dt64 = ar.f32(64)
        dtA = ar.f32(64)
        sm = ar.f32(512)
        cs32, dst32, ecs32, cdec32, w232 = sm[:, 0:32], sm[:, 32:64], sm[:, 64:96], sm[:, 96:128], sm[:, 128:160]
        t32 = sm[:, 160:192]
        st8 = sm[:, 192:208]
        rs8 = sm[:, 208:224]
        e64 = sm[:, 256:320]

        PA, PB, PT, PS_, PY, PO = bank(0), bank(1), bank(2), bank(3), bank(4), bank(5)
        PC = PSM[:, 6 * 512:8 * 512]
        pacc = [PA, PB]
        pacc_k = [('ps', 0), ('ps', 1)]
        gacc = [PA, PB, PY, PO]
        gacc_k = [('ps', 0), ('ps', 1), 'py', 'po']
        state = {'wi': 0, 'acc': 0, 'hb': 0}

        class WStream:
            def __init__(self, seq):
                self.seq = seq
                self.issued = 0
                self.pos = 0

            def _issue(self):
                if self.issued < len(self.seq):
                    bid = self.seq[self.issued]
                    slot = state['wi'] % NWB
                    state['wi'] += 1
                    tk.dma('sp', lambda e, bid=bid, slot=slot: e.dma_start(out=WB[slot], in_=WS[bid]), 'wb%d' % slot,
                           reads=[('WS', bid)], writes=[('WB', slot)])
                    self.slots = getattr(self, 'slots', []) + [slot]
                    self.issued += 1

            def next(self):
                while self.issued < min(self.pos + NWB, len(self.seq)):
                    self._issue()
                slot = self.slots[self.pos]
                self.pos += 1
                return WB[slot].rearrange("p (k n) -> p k n", n=512), ('WB', slot)

        def load_tile(pos0, zero_lo, zero_hi, xt=None, ht=None):
            xb_ = xt
            hb = ht
            tk.dma('sp', lambda e: e.dma_start(out=XT[xb_], in_=xp[pos0 + 1:pos0 + 129, :]), 'xt%d' % xb_, writes=[('XT', xb_)])
            tk.dma('sp', lambda e: e.dma_start(out=XH[xb_][0:16, 0:128], in_=xp[pos0, :].rearrange("(k p) -> k p", p=128)),
                   'xh%d' % xb_, writes=[('XH', xb_)])
            tk.dma('sp', lambda e: e.dma_start(out=XH[xb_][0:16, 128:256], in_=xp[pos0 + 129, :].rearrange("(k p) -> k p", p=128)),
                   'xh%d' % xb_, writes=[('XH', xb_)])
            h3 = hT[hb].rearrange("p (k t) -> p k t", t=130)
            for k4 in range(4):
                for q in range(4):
                    kc = k4 * 4 + q
                    tk.op('pe', lambda e, kc=kc, q=q: e.transpose(out=PT[:, q * 128:(q + 1) * 128],
                                                                 in_=XT[xb_][:, kc * 128:(kc + 1) * 128], identity=ident),
                          reads=[('XT', xb_), 'ident'], writes=['pt'], inc=(q == 3))
                for q in range(4):
                    kc = k4 * 4 + q
                    tk.op('act', lambda e, kc=kc, q=q: e.activation(out=h3[:, kc, 1:129], in_=PT[:, q * 128:(q + 1) * 128],
                                                                   func=AF.Identity, scale=sc1pT[:, kc:kc + 1],
                                                                   bias=sh1T[:, kc:kc + 1]),
                          reads=['pt', 'sc1pT', 'sh1T'], writes=[('hT', hb, kc)])
            for hi in range(2):
                tk.op('pe', lambda e, hi=hi: e.transpose(out=PS_[:, 256 + hi * 16:256 + hi * 16 + 16],
                                                        in_=XH[xb_][0:16, hi * 128:(hi + 1) * 128], identity=ident[0:16, 0:16]),
                      reads=[('XH', xb_), 'ident'], writes=['ps_'], inc=(hi == 1))
            tk.op('dve', lambda e: e.tensor_tensor(out=t32.rearrange("p (t k) -> p t k", k=16),
                                                   in0=PS_[:, 256:288].rearrange("p (t k) -> p t k", k=16),
                                                   in1=sc1pT.unsqueeze(1).to_broadcast([128, 2, 16]), op=ALU.mult),
                  reads=['ps_', 'sc1pT'], writes=['t32'])
            tk.op('dve', lambda e: e.tensor_tensor(out=h3[:, :, 0:130:129].rearrange("p k t -> p t k"),
                                                   in0=t32.rearrange("p (t k) -> p t k", k=16),
                                                   in1=sh1T.unsqueeze(1).to_broadcast([128, 2, 16]), op=ALU.add),
                  reads=['t32', 'sh1T'], writes=[('hTh', hb)])
            if zero_lo:
                tk.op('dve', lambda e: e.memset(h3[:, :, 0:1], 0.0), reads=[('hTh', hb)], writes=[('hTh', hb)])
            if zero_hi:
                tk.op('dve', lambda e: e.memset(h3[:, :, 129:130], 0.0), reads=[('hTh', hb)], writes=[('hTh', hb)])
            return hb, h3

        def hkeys(hb):
            return [('hT', hb, kc) for kc in range(16)] + [('hTh', hb)]

        def proj(ps, pskey, hb, h3, taps_blocks, ncols=512, bias_cols=None, first=True, last=True):
            n = len(taps_blocks)
            for ti, (tap, wv, wkey) in enumerate(taps_blocks):
                for kc in range(16):
                    st = first and ti == 0 and kc == 0
                    sp_ = last and (bias_cols is None) and ti == n - 1 and kc == 15
                    tk.op('pe', lambda e, tap=tap, wv=wv, kc=kc, st=st, sp_=sp_: e.matmul(
                        ps[:, 0:ncols], lhsT=h3[:, kc, tap:tap + 128], rhs=wv[:, kc, 0:ncols], start=st, stop=sp_),
                        reads=hkeys(hb) + [wkey], writes=[pskey], inc=(kc == 15))
            if bias_cols is not None:
                c0 = bias_cols
                tk.op('pe', lambda e: e.matmul(ps[:, 0:ncols], lhsT=ones_b[0:2, :], rhs=bias2[0:2, c0:c0 + ncols],
                                               start=False, stop=True),
                      reads=['ones_b', 'bias2'], writes=[pskey])

        wdt3 = wdt_sb.rearrange("p (k n) -> p k n", n=64)

        def xbc_blocks_group(ws, infos, cbs, hook=None):
            for cb in cbs:
                for tap in range(3):
                    wv, wkey = ws.next()
                    for j, (hb, h3) in enumerate(infos):
                        proj(gacc[j], gacc_k[j], hb, h3, [(tap, wv, wkey)], bias_cols=(cb * 512 if tap == 2 else None),
                             first=(tap == 0), last=(tap == 2))
                    if hook is not None:
                        hook()
                for j, (hb, h3) in enumerate(infos):
                    tk.op('act', lambda e, j=j, cb=cb: e.activation(out=xbcG[j][:, cb * 512:(cb + 1) * 512], in_=gacc[j], func=AF.Silu),
                          reads=[gacc_k[j]], writes=[('xbc', j, cb)])

        def dt_for_tile(hb, h3):
            a = state['acc'] % 2
            state['acc'] += 1
            proj(pacc[a], pacc_k[a], hb, h3, [(1, wdt3, 'wdt_sb')], ncols=64)
            tk.op('dve', lambda e, a=a: e.tensor_tensor(out=dt64, in0=pacc[a][:, 0:64], in1=dtb, op=ALU.add),
                  reads=[pacc_k[a], 'dtb'], writes=['dt64'])
            tk.op('act', lambda e: e.activation(out=e64, in_=dt64, func=AF.Exp), reads=['dt64'], writes=['e64'])
            tk.op('act', lambda e: e.activation(out=dt64, in_=e64, func=AF.Ln, bias=1.0), reads=['e64'], writes=['dt64'])
            tk.op('dve', lambda e: e.tensor_tensor(out=dtA, in0=dt64, in1=a_neg, op=ALU.mult),
                  reads=['dt64', 'a_neg'], writes=['dtA'])

        def chunk_scalars(dr, full):
            xbc = cur['xb']
            jj = cur['j']
            sl = slice(dr * 32, dr * 32 + 32)
            tri, trik = (triF, 'triF') if dr == 0 else (triR, 'triR')
            tk.op('pe', lambda e: e.matmul(PS_[:, 0:32], lhsT=tri, rhs=dtA[:, sl], start=True, stop=True),
                  reads=[trik, 'dtA'], writes=['ps_'], inc=False)
            tk.op('pe', lambda e: e.matmul(PS_[:, 32:64], lhsT=ones_f, rhs=dtA[:, sl], start=True, stop=True),
                  reads=['ones_f', 'dtA'], writes=['ps_'])
            tk.op('dve', lambda e: e.tensor_copy(out=cs32, in_=PS_[:, 0:32]), reads=['ps_'], writes=['cs32'])
            tk.op('dve', lambda e: e.tensor_tensor(out=dst32, in0=PS_[:, 32:64], in1=cs32, op=ALU.subtract),
                  reads=['ps_', 'cs32'], writes=['dst32'])
            tk.op('act', lambda e: e.activation(out=dst32, in_=dst32, func=AF.Exp), reads=['dst32'], writes=['dst32'])
            tk.op('act', lambda e: e.activation(out=cdec32, in_=PS_[:, 32:64], func=AF.Exp), reads=['ps_'], writes=['cdec32'])
            if full:
                tk.op('act', lambda e: e.activation(out=ecs32, in_=cs32, func=AF.Exp), reads=['cs32'], writes=['ecs32'])
            tk.op('dve', lambda e: e.tensor_tensor(out=w232, in0=dt64[:, sl], in1=dst32, op=ALU.mult),
                  reads=['dt64', 'dst32'], writes=['w232'])
            xs3 = xbc[:, 0:2048].rearrange("p (h d) -> p h d", d=64)
            tk.op('pool', lambda e: e.tensor_tensor(out=xdd_b.rearrange("p (h d) -> p h d", d=64), in0=xs3,
                                                    in1=w232.unsqueeze(2).to_broadcast([128, 32, 64]), op=ALU.mult),
                  reads=[('xbc', jj, i) for i in range(4)] + ['w232'], writes=['xdd_b'])
            if full:
                tk.op('pool', lambda e: e.tensor_tensor(out=xd_b.rearrange("p (h d) -> p h d", d=64), in0=xs3,
                                                        in1=dt64[:, sl].unsqueeze(2).to_broadcast([128, 32, 64]), op=ALU.mult),
                      reads=[('xbc', jj, i) for i in range(4)] + ['dt64'], writes=['xd_b'])

        def state_update(H, Hk, hook=None):
            for g in range(4):
                tk.op('pe', lambda e, g=g: e.matmul(PO, lhsT=B_b[:, g * 128:(g + 1) * 128], rhs=xdd_b[:, g * 512:(g + 1) * 512],
                                                    start=True, stop=True),
                      reads=['B_b', 'xdd_b'], writes=['po'])
                Hg = H[:, g * 512:(g + 1) * 512].rearrange("p (h d) -> p h d", d=64)
                tk.op('dve', lambda e, g=g, Hg=Hg: e.tensor_tensor(out=Hg, in0=Hg,
                                                                   in1=cdec32[:, g * 8:(g + 1) * 8].unsqueeze(2).to_broadcast([128, 8, 64]),
                                                                   op=ALU.mult),
                      reads=[(Hk, g), 'cdec32'], writes=[(Hk, g)])
                tk.op('dve', lambda e, g=g: e.tensor_tensor(out=H[:, g * 512:(g + 1) * 512], in0=H[:, g * 512:(g + 1) * 512],
                                                            in1=PO, op=ALU.add),
                      reads=[(Hk, g), 'po'], writes=[(Hk, g)])
                if hook is not None and g in (0, 2):
                    hook()

        def make_Bb():
            xbc = cur['xb']
            tk.op('act', lambda e: e.activation(out=B_b, in_=xbc[:, 2048:2560], func=AF.Copy), reads=[('xbc', cur['j'], 4)], writes=['B_b'])

        Hkeys = [('HR', g) for g in range(4)]

        cin = [ARN[:, off_x3:off_x3 + 2048], ARN[:, off_x3 + 2048:off_x3 + 4096]]
        cout = [ARN[:, off_ta:off_ta + 1024].bitcast(BF16), ARN[:, off_ta + 1024:off_ta + 2048].bitcast(BF16)]

        def conv_gen():
            def src(k):
                tab = eu if k < 128 else ev
                r0 = (k % 128) * 128
                return tab[r0:r0 + 128, :]

            def dst(k):
                tab = EUb if k < 128 else EVb
                r0 = (k % 128) * 128
                return tab[r0:r0 + 128, :], ('EUb' if k < 128 else 'EVb')

            def load(k):
                i = k % 2
                sk = src(k)
                tk.dma('pool', lambda e: e.dma_start(out=cin[i], in_=sk), 'cvl%d' % i, writes=[('cin', i)])
            load(0)
            for k in range(256):
                if k + 1 < 256:
                    load(k + 1)
                i = k % 2
                tk.op('dve', lambda e, i=i: e.tensor_copy(out=cout[i], in_=cin[i]), reads=[('cin', i)], writes=[('cout', i)])
                dk, nm = dst(k)
                tk.dma('pool', lambda e, i=i, dk=dk: e.dma_start(out=dk, in_=cout[i]), 'cvs%d' % i, reads=[('cout', i)], writes=[nm])
                yield

        cvg = conv_gen()

        def state_sweep(tiles, dr, H, Hk, spill):
            groups = [tiles[i:i + 4] for i in range(0, len(tiles), 4)]
            seq = []
            for _ in groups:
                for cb in range(5):
                    for tap in range(3):
                        seq.append(blk_xbc(cb, tap))
            ws = WStream(seq)
            for grp in groups:
                infos = []
                for j, T in enumerate(grp):
                    infos.append(load_tile(T * 128, T == 0, T == 31, xt=j % 2, ht=j))
                xbc_blocks_group(ws, infos, range(5), hook=lambda: [next(cvg, None) for _c in range(3)])
                for j, T in enumerate(grp):
                    if spill:
                        tk.op('act', lambda e: e.activation(out=HFb, in_=H, func=AF.Copy), reads=[(Hk, g) for g in range(4)], writes=['HFb'])
                        tk.dma('sp', lambda e, T=T: e.dma_start(out=HFS[T], in_=HFb), 'hfs', reads=['HFb'], writes=[('HFS', T)])
                    cur['j'] = j
                    cur['xb'] = xbcG[j]
                    dt_for_tile(*infos[j])
                    make_Bb()
                    chunk_scalars(dr, False)
                    state_update(H, Hk)

        tk.op('dve', lambda e: e.memset(HR, 0.0), writes=Hkeys)
        if stage >= 2:
            state_sweep(list(range(31, 15, -1)), 1, HR, 'HR', False)
        HF = ysum
        HFk = [('HF', g) for g in range(4)]
        tk.op('dve', lambda e: e.memset(HF, 0.0), writes=HFk)
        if stage >= 2:
            state_sweep(list(range(15)), 0, HF, 'HF', True)
            tk.op('act', lambda e: e.activation(out=HFb, in_=HF, func=AF.Copy), reads=HFk, writes=['HFb'])
            tk.dma('sp', lambda e: e.dma_start(out=HFS[15], in_=HFb), 'hfs', reads=['HFb'], writes=[('HFS', 15)])
        tk.seal('hfs')
        for _ in cvg:
            pass
        tk.barrier()
        tk.dma('sp', lambda e: e.dma_start(out=g1b, in_=modd[0:1, 4096:6144].partition_broadcast(128)), 'pm',
               reads=['modd'], writes=['g1b'])
        tk.dma('sp', lambda e: e.dma_start(out=l1g, in_=ln1g_b[:, :]), 'pm', writes=['l1g'])
        tk.dma('sp', lambda e: e.dma_start(out=l1b, in_=ln1b_b[:, :]), 'pm', writes=['l1b'])
        tk.seal('pm')

        seqC = []
        for pi in range(8):
            for cb in range(6):
                for tap in range(3):
                    seqC.append(blk_xbc(cb, tap))
            for _t in range(2):
                for j in range(4):
                    seqC += [blk_sc(j, 4), blk_sc(j, 1), blk_sc(j, 2), blk_sc(j, 3), blk_sc(j, 0)]
                for g in range(4):
                    seqC.append(blk_z(g))
                for nb in range(4):
                    seqC += [blk_out(nb, 0), blk_out(nb, 1)]
        ws = WStream(seqC)
        yT3 = yT.rearrange("p (k t) -> p k t", t=128)

        def transposes_to_yT(src, srckey, kbase, gT):
            for cc in range(4):
                q = cc
                tk.op('pe', lambda e, cc=cc, q=q: e.transpose(out=PT[:, q * 128:(q + 1) * 128],
                                                             in_=src[:, cc * 128:(cc + 1) * 128], identity=ident),
                      reads=[srckey, 'ident'], writes=['pt'], inc=(q == 3))
            for cc in range(4):
                q = cc
                col = kbase + cc - (kbase // 16) * 16
                tk.op('act', lambda e, cc=cc, q=q, col=col: e.activation(out=yT3[:, kbase + cc, :], in_=PT[:, q * 128:(q + 1) * 128],
                                                                        func=AF.Copy, scale=gT[:, col:col + 1]),
                      reads=['pt', 'ssdg', 'scg'], writes=[('yT', kbase + cc)])

        def rstd_from_ss(ss_ap, n_elems, width, key_in, key_out, out_ap):
            tk.op('dve', lambda e: e.tensor_scalar(out=out_ap, in0=ss_ap, scalar1=1.0 / n_elems, scalar2=EPS, op0=ALU.mult,
                                                   op1=ALU.add), reads=[key_in], writes=[key_out])
            tk.op('act', lambda e: e.activation(out=out_ap, in_=out_ap, func=AF.Sqrt), reads=[key_out], writes=[key_out])
            tk.op('dve', lambda e: e.reciprocal(out=out_ap, in_=out_ap), reads=[key_out], writes=[key_out])

        def sc_gen(hb, h3):
            tC = u_t[:, 0:512]
            tD = u_t[:, 512:1024]
            st8s = sm[:, 320:328]
            rs8s = sm[:, 328:336]
            for j in range(4):
                wv, wkey = ws.next()
                for tap in range(3):
                    a = state['acc'] % 2
                    state['acc'] += 1
                    proj(pacc[a], pacc_k[a], hb, h3, [(tap, wv, wkey)])
                    tk.op('act', lambda e, a=a, tap=tap: e.activation(out=vt[tap], in_=pacc[a], func=AF.Copy),
                          reads=[pacc_k[a]], writes=[('vt', tap)])
                    yield
                for tap in range(3):
                    a = state['acc'] % 2
                    state['acc'] += 1
                    wv, wkey = ws.next()
                    proj(pacc[a], pacc_k[a], hb, h3, [(tap, wv, wkey)])
                    if tap == 0:
                        tk.op('dve', lambda e, a=a: e.tensor_tensor(out=tD, in0=pacc[a], in1=vt[0], op=ALU.mult),
                              reads=[pacc_k[a], ('vt', 0)], writes=['tD', 'u_t'])
                    else:
                        tk.op('dve', lambda e, a=a, tap=tap: e.tensor_tensor(out=tC, in0=pacc[a], in1=vt[tap], op=ALU.mult),
                              reads=[pacc_k[a], ('vt', tap)], writes=['tC', 'u_t'])
                        tk.op('pool', lambda e: e.tensor_tensor(out=tD, in0=tD, in1=tC, op=ALU.add),
                              reads=['tC', 'tD'], writes=['tD'])
                    yield
                a = state['acc'] % 2
                state['acc'] += 1
                wv, wkey = ws.next()
                proj(pacc[a], pacc_k[a], hb, h3, [(1, wv, wkey)])
                tk.op('dve', lambda e, a=a: e.tensor_tensor(out=tD, in0=pacc[a], in1=tD, op=ALU.mult),
                      reads=[pacc_k[a], 'tD'], writes=['tD'])
                tk.op('act', lambda e: e.activation(out=tC, in_=tD, func=AF.Square), reads=['tD'], writes=['tC'])
                tk.op('dve', lambda e: e.tensor_reduce(out=st8s, in_=tC.rearrange("p (g d) -> p g d", d=64), axis=AX.X,
                                                       op=ALU.add), reads=['tC'], writes=['st8s'])
                rstd_from_ss(st8s, 64.0, 8, 'st8s', 'rs8s', rs8s)
                tk.op('dve', lambda e: e.tensor_tensor(out=tD.rearrange("p (g d) -> p g d", d=64),
                                                       in0=tD.rearrange("p (g d) -> p g d", d=64),
                                                       in1=rs8s.unsqueeze(2).to_broadcast([128, 8, 64]), op=ALU.mult),
                      reads=['tD', 'rs8s'], writes=['tD'])
                transposes_to_yT(tD, 'tD', 16 + j * 4, scg)
                yield

        def tile_C(T, hb, h3, xbc, jj):
            scg_ = sc_gen(hb, h3)
            XBK = [('xbc', jj, i) for i in range(4)]
            xs3 = xbc[:, 0:2048].rearrange("p (h d) -> p h d", d=64)
            dt_for_tile(hb, h3)
            for _i in range(3):
                next(scg_, None)
            make_Bb()
            tk.dma('sp', lambda e, T=T: e.dma_start(out=HFb, in_=HFS[T]), 'hfl', reads=[('HFS', T)], writes=['HFb'])
            for (srcoff, dstb, dk) in [(2048, BT_b, 'BT_b'), (2560, CT_b, 'CT_b')]:
                for g in range(4):
                    tk.op('pe', lambda e, g=g, srcoff=srcoff: e.transpose(out=PT[:, g * 128:(g + 1) * 128],
                                                                         in_=xbc[:, srcoff + g * 128:srcoff + (g + 1) * 128],
                                                                         identity=ident),
                          reads=[('xbc', jj, 4), ('xbc', jj, 5), 'ident'], writes=['pt'], inc=(g == 3))
                tk.op('act', lambda e, dstb=dstb: e.activation(out=dstb, in_=PT, func=AF.Copy),
                      reads=['pt' for g in range(4)], writes=[dk])
            tk.op('pool', lambda e: e.tensor_tensor(out=ysum.rearrange("p (h d) -> p h d", d=64), in0=xs3,
                                                    in1=dsk.unsqueeze(2).to_broadcast([128, 32, 64]), op=ALU.mult),
                  reads=XBK + ['dsk'], writes=['ysum'] + HFk)
            for dr in (1, 0):
                sl0 = dr * 32
                chunk_scalars(dr, True)
                for _i in range(2):
                    next(scg_, None)
                Hb, Hbk = (HRb, 'HRb') if dr == 1 else (HFb, 'HFb')
                if dr == 1:
                    tk.op('act', lambda e: e.activation(out=HRb, in_=HR, func=AF.Copy), reads=Hkeys, writes=['HRb'])
                tri, trik = (triF, 'triF') if dr == 0 else (triR, 'triR')
                for g in range(4):
                    hs = slice(sl0 + g * 8, sl0 + g * 8 + 8)
                    tk.op('pe', lambda e, g=g: e.matmul(PS_[:, 64:192], lhsT=BT_b[:, g * 128:(g + 1) * 128],
                                                        rhs=CT_b[:, g * 128:(g + 1) * 128], start=True, stop=True),
                          reads=['BT_b', 'CT_b'], writes=['ps_'])
                    X3v = X3.rearrange("p (h l) -> p h l", l=128)
                    tk.op('pool', lambda e, hs=hs, tri=tri: e.tensor_tensor(
                        out=X3v, in0=dtA[:, hs].unsqueeze(2).to_broadcast([128, 8, 128]),
                        in1=tri.unsqueeze(1).to_broadcast([128, 8, 128]), op=ALU.mult),
                        reads=['dtA', trik], writes=['X3'])
                    for hf in range(2):
                        tk.op('pe', lambda e, hf=hf: e.matmul(PC[:, hf * 512:(hf + 1) * 512], lhsT=ones_f,
                                                              rhs=X3[:, hf * 512:(hf + 1) * 512], start=True, stop=True),
                              reads=['ones_f', 'X3'], writes=['pc'], inc=(hf == 1))
                    D3v = D3.rearrange("p (h l) -> p h l", l=128)
                    tk.op('dve', lambda e, g=g: e.tensor_tensor(
                        out=D3v, in0=PC.rearrange("p (h l) -> p h l", l=128),
                        in1=cs32[:, g * 8:(g + 1) * 8].unsqueeze(2).to_broadcast([128, 8, 128]), op=ALU.subtract),
                        reads=['pc', 'cs32'], writes=['D3'])
                    if dr == 0:
                        patt, cm = [[0, 8], [1, 128]], -1
                    else:
                        patt, cm = [[0, 8], [-1, 128]], 1
                    tk.op('pool', lambda e, patt=patt, cm=cm: e.affine_select(out=D3v, in_=D3v, pattern=patt, compare_op=ALU.is_ge,
                                                                             fill=tk.getreg(e, -200.0), base=0, channel_multiplier=cm),
                          reads=['D3'], writes=['D3'])
                    tk.op('act', lambda e: e.activation(out=E3, in_=D3, func=AF.Exp), reads=['D3'], writes=['E3'])
                    tk.op('dve', lambda e: e.tensor_tensor(
                        out=MT_b.rearrange("p (h l) -> p h l", l=128), in0=E3.rearrange("p (h l) -> p h l", l=128),
                        in1=PS_[:, 64:192].unsqueeze(1).to_broadcast([128, 8, 128]), op=ALU.mult),
                        reads=['E3', 'ps_'], writes=['MT_b'])
                    for _i in range(3 if dr == 1 else 2):
                        next(scg_, None)
                    for hh in range(8):
                        hd = g * 8 + hh
                        tk.op('pe', lambda e, hh=hh, hd=hd: e.matmul(PY[:, hh * 64:(hh + 1) * 64], lhsT=MT_b[:, hh * 128:(hh + 1) * 128],
                                                                     rhs=xd_b[:, hd * 64:(hd + 1) * 64], start=True, stop=True),
                              reads=['MT_b', 'xd_b'], writes=['py'], inc=(hh == 7))
                    tk.op('pe', lambda e, g=g, Hb=Hb: e.matmul(PO, lhsT=CT_b[:, g * 128:(g + 1) * 128],
                                                               rhs=Hb[:, g * 512:(g + 1) * 512], start=True, stop=True),
                          reads=['CT_b', Hbk], writes=['po'])
                    tk.op('dve', lambda e, g=g: e.tensor_tensor(
                        out=tA.rearrange("p (h d) -> p h d", d=64), in0=PO.rearrange("p (h d) -> p h d", d=64),
                        in1=ecs32[:, g * 8:(g + 1) * 8].unsqueeze(2).to_broadcast([128, 8, 64]), op=ALU.mult),
                        reads=['po', 'ecs32'], writes=['tA'])
                    tk.op('dve', lambda e: e.tensor_tensor(out=tA, in0=tA, in1=PY, op=ALU.add), reads=['tA', 'py'], writes=['tA'])
                    tk.op('pool', lambda e, g=g: e.tensor_tensor(out=ysum[:, g * 512:(g + 1) * 512],
                                                                 in0=ysum[:, g * 512:(g + 1) * 512], in1=tA, op=ALU.add),
                          reads=['ysum', 'tA'], writes=['ysum'])
                if dr == 1:
                    state_update(HR, 'HR', hook=lambda: next(scg_, None))
            for _ in scg_:
                pass
            zb = [tB, tA]
            zk = ['tB', 'tA']
            zacc = []

            def z_proj():
                a = state['acc'] % 2
                state['acc'] += 1
                wv, wkey = ws.next()
                proj(pacc[a], pacc_k[a], hb, h3, [(1, wv, wkey)])
                zacc.append(a)

            def z_chain(g):
                a = zacc[g]
                tz, kz = zb[g % 2], zk[g % 2]
                ss, rs = st8[:, g % 2:g % 2 + 1], rs8[:, g % 2:g % 2 + 1]
                kss, krs = ('st8z', g % 2), ('rs8z', g % 2)
                tk.op('act', lambda e: e.activation(out=tz, in_=pacc[a], func=AF.Silu), reads=[pacc_k[a]], writes=[kz])
                tk.op('dve', lambda e: e.tensor_tensor(out=tz, in0=tz, in1=ysum[:, g * 512:(g + 1) * 512], op=ALU.mult),
                      reads=[kz, 'ysum'], writes=[kz])
                tk.op('act', lambda e: e.activation(out=vt[0], in_=tz, func=AF.Square, accum_out=ss),
                      reads=[kz], writes=[('vt', 0), kss])
                rstd_from_ss(ss, 512.0, 1, kss, krs, rs)
                tk.op('dve', lambda e: e.tensor_scalar(out=tz, in0=tz, scalar1=rs, scalar2=None, op0=ALU.mult),
                      reads=[kz, krs], writes=[kz])
                transposes_to_yT(tz, kz, g * 4, ssdg)

            z_proj()
            for g in range(4):
                if g + 1 < 4:
                    z_proj()
                z_chain(g)
            for _ in scg_:
                pass
            ypk = [('yT', k) for k in range(32)]
            for nb in range(4):
                a = state['acc'] % 2
                state['acc'] += 1
                for hf in range(2):
                    wv, wkey = ws.next()
                    for kc in range(16):
                        tk.op('pe', lambda e, a=a, hf=hf, kc=kc, wv=wv: e.matmul(
                            pacc[a], lhsT=yT3[:, hf * 16 + kc, :], rhs=wv[:, kc, :], start=(hf == 0 and kc == 0),
                            stop=(hf == 1 and kc == 15)), reads=ypk + [wkey], writes=[pacc_k[a]], inc=(kc == 15))
                tk.op('dve', lambda e, a=a, nb=nb: e.tensor_tensor(out=tA, in0=pacc[a], in1=g1b[:, nb * 512:(nb + 1) * 512], op=ALU.mult),
                      reads=[pacc_k[a], 'g1b'], writes=['tA'])
                if DBG == 1:
                    tk.op('dve', lambda e, a=a, nb=nb: e.tensor_copy(out=u_t[:, nb * 512:(nb + 1) * 512], in_=pacc[a]),
                          reads=[pacc_k[a], 'tA'], writes=['u_t'])
                else:
                    tk.op('dve', lambda e, nb=nb, hb=hb: e.scalar_tensor_tensor(out=u_t[:, nb * 512:(nb + 1) * 512],
                                                                                in0=XT[hb][:, nb * 512:(nb + 1) * 512], scalar=ALPHA, in1=tA,
                                                                                op0=ALU.mult, op1=ALU.add),
                          reads=[('XT', hb), 'tA'], writes=['u_t'])
            if DBG == 3:
                tk.op('dve', lambda e: e.tensor_copy(out=u_t[:, 0:1024], in_=xbc[:, 2048:3072]), reads=[('xbc', jj, 4), ('xbc', jj, 5), 'u_t'], writes=['u_t'])
                tk.op('dve', lambda e: e.tensor_copy(out=u_t[:, 1024:1088], in_=dt64), reads=['dt64', 'u_t'], writes=['u_t'])
                tk.op('dve', lambda e: e.tensor_copy(out=u_t[:, 1088:2048], in_=xbc[:, 0:960]), reads=XBK + ['u_t'], writes=['u_t'])
            if DBG == 2:
                tk.op('dve', lambda e: e.tensor_copy(out=u_t, in_=ysum), reads=['ysum', 'u_t'], writes=['u_t'])
            if DBG == 0:
                layer_norm(tk, u_t, 'u_t', sm, l1g, 'l1g', l1b, 'l1b', xbc[:, 0:2048], XBK)
            tk.dma('pool', lambda e, T=T: e.dma_start(out=X1S[T * 128:(T + 1) * 128, :], in_=u_t), 'x1s',
                   reads=['u_t'], writes=[('X1S', T)])

        for pi in range(8):
            if stage < 3:
                break
            pair = [15 - 2 * pi, 14 - 2 * pi]
            infos = [load_tile(T * 128, T == 0, False, xt=j, ht=j) for j, T in enumerate(pair)]
            xbc_blocks_group(ws, infos, range(6))
            for j, T in enumerate(pair):
                cur['j'] = j
                cur['xb'] = xbcG[j]
                tile_C(T, infos[j][0], infos[j][1], xbcG[j], j)
        tk.seal('x1s')

        tk.barrier()
        ar.off = PERSIST
        if stage >= 4:
            peer_phase(nc, tk, ar, bank, PSM, WS, X1S, modd, kT, EUb, EVb, ln2g_b, ln2b_b, out, ident, iota16)
        else:
            tb = ar.f32(2048)
            for T in range(16):
                tk.dma('sp', lambda e, T=T: e.dma_start(out=tb, in_=X1S[T * 128:(T + 1) * 128, :]), 'dbl',
                       reads=[('X1S', T)], writes=['tb'])
                tk.dma('sp', lambda e, T=T: e.dma_start(out=out[T * 128:(T + 1) * 128, :], in_=tb), 'outst',
                       reads=['tb'], writes=[('out', T)])
        tk.barrier()
        tk.emit()
    return nc


def layer_norm(tk, u, uk, sm, g, gk, b, bk, scratch, scratch_keys):
    mean = sm[:, 224:225]
    ssq = sm[:, 225:226]
    rstd = sm[:, 226:227]
    tk.op('act', lambda e: e.activation(out=scratch, in_=u, func=AF.Identity, accum_out=mean),
          reads=[uk], writes=list(scratch_keys) + ['ln_mean'])
    tk.op('dve', lambda e: e.tensor_scalar(out=mean, in0=mean, scalar1=1.0 / 2048.0, scalar2=None, op0=ALU.mult),
          reads=['ln_mean'], writes=['ln_mean'])
    tk.op('dve', lambda e: e.tensor_scalar(out=u, in0=u, scalar1=mean, scalar2=None, op0=ALU.subtract),
          reads=[uk, 'ln_mean'], writes=[uk])
    tk.op('act', lambda e: e.activation(out=scratch, in_=u, func=AF.Square, accum_out=ssq),
          reads=[uk], writes=list(scratch_keys) + ['ln_ssq'])
    tk.op('dve', lambda e: e.tensor_scalar(out=rstd, in0=ssq, scalar1=1.0 / 2048.0, scalar2=EPS, op0=ALU.mult, op1=ALU.add),
          reads=['ln_ssq'], writes=['ln_rstd'])
    tk.op('act', lambda e: e.activation(out=rstd, in_=rstd, func=AF.Sqrt), reads=['ln_rstd'], writes=['ln_rstd'])
    tk.op('dve', lambda e: e.reciprocal(out=rstd, in_=rstd), reads=['ln_rstd'], writes=['ln_rstd'])
    tk.op('dve', lambda e: e.tensor_scalar(out=u, in0=u, scalar1=rstd, scalar2=None, op0=ALU.mult),
          reads=[uk, 'ln_rstd'], writes=[uk])
    tk.op('dve', lambda e: e.tensor_tensor(out=u, in0=u, in1=g, op=ALU.mult), reads=[uk, gk], writes=[uk])
    tk.op('dve', lambda e: e.tensor_tensor(out=u, in0=u, in1=b, op=ALU.add), reads=[uk, bk], writes=[uk])


def peer_phase(nc, tk, ar, bank, PSM, WS, X1S, modd, kT, eu, ev, ln2g_b, ln2b_b, out, ident, iota16):
    NB = 12
    sc2p = ar.f32(2048)
    sh2 = ar.f32(2048)
    g2b = ar.f32(2048)
    l2g = ar.f32(2048)
    l2b = ar.f32(2048)
    KT_b = ar.bf16(4096)
    acc = ar.f32(2048)
    x1 = [acc, ar.f32(2048)]
    h2 = [ar.f32(2048), ar.f32(2048)]
    h2T = ar.bf16(16 * 128)
    qT = ar.bf16(32 * 128)
    WQ = [ar.bf16(8192)]
    S = ar.f32(2048)
    S2 = ar.f32(2048)
    V16 = ar.f32(256)
    I16 = ar.u32(256)
    I16f = ar.f32(256)
    CAND = ar.f32(2048)
    CAND2 = S2
    SCV = ar.f32(128)
    FL = ar.u32(128)
    ABf = ar.f32(256)
    OH = CAND
    E12 = ar.f32(256)
    IDX = [ar.i32(128), ar.i32(128), ar.i32(128)]
    IDXf = ar.f32(128)
    GATE = [ar.f32(128), ar.f32(128), ar.f32(128)]
    ACT_ = [ar.f32(128), ar.f32(128)]
    COEF = [ar.f32(128), ar.f32(128)]
    sm = ar.f32(512)
    thr16 = sm[:, 16:32]
    UBR = ar.f32(NB * 1024)
    UB = [UBR[:, b * 1024:(b + 1) * 1024].bitcast(BF16) for b in range(NB)]
    DG = [ar.bf16(128) for _ in range(4)]
    junk = OH

    tk.dma('sp', lambda e: e.dma_start(out=sh2, in_=modd[0:1, 6144:8192].partition_broadcast(128)), 'pd', reads=['modd'], writes=['sh2'])
    tk.dma('sp', lambda e: e.dma_start(out=sc2p, in_=modd[0:1, 8192:10240].partition_broadcast(128)), 'pd', reads=['modd'], writes=['sc2p'])
    tk.dma('sp', lambda e: e.dma_start(out=g2b, in_=modd[0:1, 10240:12288].partition_broadcast(128)), 'pd', reads=['modd'], writes=['g2b'])
    tk.dma('sp', lambda e: e.dma_start(out=l2g, in_=ln2g_b[:, :]), 'pd', writes=['l2g'])
    tk.dma('sp', lambda e: e.dma_start(out=l2b, in_=ln2b_b[:, :]), 'pd', writes=['l2b'])
    tk.seal('pd')
    tk.op('dve', lambda e: e.tensor_scalar(out=thr16, in0=iota16, scalar1=16.0, scalar2=16.0, op0=ALU.mult, op1=ALU.add),
          reads=['iota16'], writes=['thr16'])
    tk.op('dve', lambda e: e.tensor_scalar(out=sc2p, in0=sc2p, scalar1=1.0, scalar2=None, op0=ALU.add), reads=['sc2p'], writes=['sc2p'])
    for i in range(2):
        k32 = UBR[:, i * 2048:(i + 1) * 2048]
        tk.dma('sp', lambda e, i=i, k32=k32: e.dma_start(out=k32, in_=kT[:, i * 2048:(i + 1) * 2048]), 'pd2_%d' % i,
               writes=[('UB', 2 * i), ('UB', 2 * i + 1)])
        tk.op('act', lambda e, i=i, k32=k32: e.activation(out=KT_b[:, i * 2048:(i + 1) * 2048], in_=k32, func=AF.Copy),
              reads=[('UB', 2 * i), ('UB', 2 * i + 1)], writes=['KT_b'])
    h2T3 = h2T.rearrange("p (k t) -> p k t", t=128)
    qT3 = qT.rearrange("p (k t) -> p k t", t=128)
    KT3 = KT_b.rearrange("p (k n) -> p k n", n=128)
    PT = bank(2)
    PQ = [bank(0), bank(1)]
    PS3 = bank(3)
    PSC = PSM[:, 4 * 512:8 * 512]
    st = {'qi': 0, 'gi': 0}
    V3 = V16.rearrange("p (h k) -> p h k", k=16)
    I3 = I16.rearrange("p (h k) -> p h k", k=16)
    S3 = S.rearrange("p (h k) -> p h k", k=128)
    S23 = S2.rearrange("p (h k) -> p h k", k=128)
    V4 = V16.rearrange("p (h s k) -> p h s k", s=2, k=16)
    C4 = CAND.rearrange("p (h a b) -> p h a b", a=16, b=16)
    C3 = CAND.rearrange("p (h c) -> p h c", c=256)
    C23 = CAND2.rearrange("p (h c) -> p h c", c=256)
    SC3 = SCV.rearrange("p (h k) -> p h k", k=16)
    FL3 = FL.rearrange("p (h k) -> p h k", k=16)
    I4f = I16f.rearrange("p (h s k) -> p h s k", s=2, k=16)
    OH4 = OH.rearrange("p (h k a) -> p h k a", k=16, a=16)

    def top16(vals, idxs, src, src2, hh, keys):
        kv, ki, ks, ks2 = keys
        tk.op('dve', lambda e: e.max(out=vals[:, hh, 0:8], in_=src[:, hh, :]), reads=[ks], writes=[kv])
        tk.op('dve', lambda e: e.max_index(out=idxs[:, hh, 0:8], in_max=vals[:, hh, 0:8], in_values=src[:, hh, :]),
              reads=[ks, kv], writes=[ki])
        tk.op('dve', lambda e: e.match_replace(out=src2[:, hh, :], in_to_replace=vals[:, hh, 0:8], in_values=src[:, hh, :],
                                               imm_value=NEG), reads=[ks, kv], writes=[ks2])
        tk.op('dve', lambda e: e.max(out=vals[:, hh, 8:16], in_=src2[:, hh, :]), reads=[ks2], writes=[kv])
        tk.op('dve', lambda e: e.max_index(out=idxs[:, hh, 8:16], in_max=vals[:, hh, 8:16], in_values=src2[:, hh, :]),
              reads=[ks2, kv], writes=[ki])

    def stage_A(T):
        p = T % 2
        p3 = T % 3
        x1p, h2p, IDXp, GATEp = x1[0], h2[p], IDX[p3], GATE[p3]
        kx, kh, ki_, kg = 'acc', ('h2', p), ('IDX', p3), ('GATE', p3)
        tk.dma('sp', lambda e: e.dma_start(out=x1p, in_=X1S[T * 128:(T + 1) * 128, :]), 'x1l0', reads=[('X1S', T)], writes=[kx])
        tk.op('dve', lambda e: e.tensor_tensor(out=h2p, in0=x1p, in1=sc2p, op=ALU.mult), reads=[kx, 'sc2p'], writes=[kh])
        tk.op('dve', lambda e: e.tensor_tensor(out=h2p, in0=h2p, in1=sh2, op=ALU.add), reads=[kh, 'sh2'], writes=[kh])
        yield
        for k4 in range(4):
            for q in range(4):
                kc = k4 * 4 + q
                tk.op('pe', lambda e, kc=kc, q=q: e.transpose(out=PT[:, q * 128:(q + 1) * 128], in_=h2p[:, kc * 128:(kc + 1) * 128],
                                                             identity=ident), reads=[kh, 'ident'], writes=['pt'], inc=(q == 3))
            for q in range(4):
                kc = k4 * 4 + q
                tk.op('act', lambda e, kc=kc, q=q: e.activation(out=h2T3[:, kc, :], in_=PT[:, q * 128:(q + 1) * 128], func=AF.Copy),
                      reads=['pt'], writes=[('h2T', kc)])
            yield
        h2Tk = [('h2T', kc) for kc in range(16)]
        for hb in range(8):
            tk.dma('sp', lambda e, hb=hb: e.dma_start(out=WQ[0], in_=WS[blk_q(hb)]), 'wq0',
                   reads=[('WS', blk_q(hb))], writes=[('WQ', 0)])
            wv = WQ[0].rearrange("p (k n) -> p k n", n=512)
            for cc in range(4):
                a = st['qi'] % 2
                st['qi'] += 1
                for kc in range(16):
                    tk.op('pe', lambda e, a=a, kc=kc, cc=cc, wv=wv: e.matmul(PQ[a][:, 0:128], lhsT=wv[:, kc, cc * 128:(cc + 1) * 128],
                                                                           rhs=h2T3[:, kc, :], start=(kc == 0), stop=(kc == 15)),
                          reads=h2Tk + [('WQ', 0)], writes=[('ps', a)], inc=(kc == 15))
                tk.op('act', lambda e, a=a, hb=hb, cc=cc: e.activation(out=qT3[:, hb * 4 + cc, :], in_=PQ[a][:, 0:128], func=AF.Copy),
                      reads=[('ps', a)], writes=[('qT', hb * 4 + cc)])
                yield
        qTk = [('qT', i) for i in range(32)]
        for b4 in range(4):
            for h4 in range(4):
                hh = b4 * 4 + h4
                for jc in range(2):
                    tk.op('pe', lambda e, hh=hh, h4=h4, jc=jc: e.matmul(PS3[:, h4 * 128:(h4 + 1) * 128], lhsT=qT3[:, hh * 2 + jc, :],
                                                                      rhs=KT3[:, hh * 2 + jc, :], start=(jc == 0), stop=(jc == 1)),
                          reads=qTk + ['KT_b'], writes=['ps3'], inc=(h4 == 3 and jc == 1))
            tk.op('act', lambda e, b4=b4: e.activation(out=S[:, b4 * 512:(b4 + 1) * 512], in_=PS3, func=AF.Copy),
                  reads=['ps3'], writes=['S'])
            yield
        for hh in range(16):
            top16(V3, I3, S3, S23, hh, ('V16', 'I16', 'S', 'S2'))
            yield
        tk.op('dve', lambda e: e.tensor_tensor(out=C4, in0=V4[:, :, 0, :].unsqueeze(3).to_broadcast([128, 8, 16, 16]),
                                               in1=V4[:, :, 1, :].unsqueeze(2).to_broadcast([128, 8, 16, 16]), op=ALU.add),
              reads=['V16'], writes=['CAND'])
        yield
        for h in range(8):
            top16(SC3, FL3, C3, C23, h, ('SCV', 'FL', 'CAND', 'S2'))
            yield
        tk.op('dve', lambda e: e.tensor_copy(out=ABf[:, 128:256], in_=FL), reads=['FL'], writes=['ABf'])
        tk.op('dve', lambda e: e.tensor_tensor(out=OH.rearrange("p (hk a) -> p hk a", a=16),
                                               in0=ABf[:, 128:256].unsqueeze(2).to_broadcast([128, 128, 16]),
                                               in1=thr16.unsqueeze(1).to_broadcast([128, 128, 16]), op=ALU.is_ge),
              reads=['ABf', 'thr16'], writes=['CAND'])
        tk.op('dve', lambda e: e.tensor_reduce(out=ABf[:, 0:128], in_=OH.rearrange("p (hk a) -> p hk a", a=16), axis=AX.X,
                                               op=ALU.add), reads=['CAND'], writes=['ABf'])
        tk.op('dve', lambda e: e.scalar_tensor_tensor(out=ABf[:, 128:256], in0=ABf[:, 0:128], scalar=-16.0, in1=ABf[:, 128:256],
                                                      op0=ALU.mult, op1=ALU.add), reads=['ABf'], writes=['ABf'])
        tk.op('dve', lambda e: e.tensor_copy(out=I16f, in_=I16), reads=['I16'], writes=['I16f'])
        yield
        for s_ in range(2):
            ab = ABf[:, s_ * 128:(s_ + 1) * 128].rearrange("p (h k) -> p h k", k=16)
            tk.op('dve', lambda e, ab=ab: e.tensor_tensor(out=OH4, in0=ab.unsqueeze(3).to_broadcast([128, 8, 16, 16]),
                                                          in1=iota16.unsqueeze(1).unsqueeze(1).to_broadcast([128, 8, 16, 16]),
                                                          op=ALU.is_equal), reads=['ABf', 'iota16'], writes=['CAND'])
            tk.op('dve', lambda e, s_=s_: e.tensor_tensor(out=OH4, in0=OH4,
                                                          in1=I4f[:, :, s_, :].unsqueeze(2).to_broadcast([128, 8, 16, 16]),
                                                          op=ALU.mult), reads=['CAND', 'I16f'], writes=['CAND'])
            tk.op('dve', lambda e, s_=s_: e.tensor_reduce(out=E12[:, s_ * 128:(s_ + 1) * 128],
                                                          in_=OH.rearrange("p (hk a) -> p hk a", a=16), axis=AX.X, op=ALU.add),
                  reads=['CAND'], writes=['E12'])
            yield
        tk.op('dve', lambda e: e.scalar_tensor_tensor(out=IDXf, in0=E12[:, 0:128], scalar=128.0, in1=E12[:, 128:256],
                                                      op0=ALU.mult, op1=ALU.add), reads=['E12'], writes=['IDXf'])
        tk.op('dve', lambda e: e.tensor_copy(out=IDXp, in_=IDXf), reads=['IDXf'], writes=[ki_])
        G3 = GATEp.rearrange("p (h k) -> p h k", k=16)
        tk.op('dve', lambda e: e.tensor_tensor(out=G3, in0=SC3, in1=SC3[:, :, 0:1].to_broadcast([128, 8, 16]), op=ALU.subtract),
              reads=['SCV'], writes=[kg])
        tk.op('act', lambda e: e.activation(out=GATEp, in_=GATEp, func=AF.Exp), reads=[kg], writes=[kg])
        tk.op('dve', lambda e: e.tensor_reduce(out=sm[:, 0:8], in_=G3, axis=AX.X, op=ALU.add), reads=[kg], writes=['gsum'])
        tk.op('dve', lambda e: e.reciprocal(out=sm[:, 0:8], in_=sm[:, 0:8]), reads=['gsum'], writes=['gsum'])
        tk.op('dve', lambda e: e.tensor_tensor(out=G3, in0=G3, in1=sm[:, 0:8].unsqueeze(2).to_broadcast([128, 8, 16]), op=ALU.mult),
              reads=[kg, 'gsum'], writes=[kg])
        yield

    def u_slot(T, sl):
        p, p3 = T % 2, T % 3
        b = st['gi'] % NB
        st['gi'] += 1
        IDXp, h2p, ACTp = IDX[p3], h2[p], ACT_[p]
        tk.dma('pool', lambda e: e.indirect_dma_start(
            out=UB[b], out_offset=None, in_=eu[:, :], in_offset=bass.IndirectOffsetOnAxis(ap=IDXp[:, sl:sl + 1], axis=0)),
            'ub%d' % b, reads=[('IDX', p3)], writes=[('UB', b)])
        tk.op('dve', lambda e: e.scalar_tensor_tensor(out=UB[b], in0=UB[b], scalar=1.0, in1=h2p, op0=ALU.mult,
                                                      op1=ALU.mult, accum_out=ACTp[:, sl:sl + 1]),
              reads=[('UB', b), ('h2', p)], writes=[('UB', b), ('ACT', p, sl)])

    def u_finish(T):
        p, p3 = T % 2, T % 3
        ACTp, COEFp, GATEp = ACT_[p], COEF[p], GATE[p3]
        tk.op('act', lambda e: e.activation(out=COEFp, in_=ACTp, func=AF.Gelu), reads=[('ACT', p, sl) for sl in range(128)],
              writes=[('COEF', p)])
        tk.op('dve', lambda e: e.tensor_tensor(out=COEFp, in0=COEFp, in1=GATEp, op=ALU.mult),
              reads=[('COEF', p), ('GATE', p3)], writes=[('COEF', p)])

    def v_slot(T, sl):
        p, p3 = T % 2, T % 3
        b = st['gi'] % NB
        st['gi'] += 1
        IDXp, COEFp = IDX[p3], COEF[p]
        tk.dma('pool', lambda e: e.indirect_dma_start(
            out=UB[b], out_offset=None, in_=ev[:, :], in_offset=bass.IndirectOffsetOnAxis(ap=IDXp[:, sl:sl + 1], axis=0)),
            'ub%d' % b, reads=[('IDX', p3)], writes=[('UB', b)])
        if sl % 4 == 3:
            accd = x1[1]
            if sl == 3:
                tk.op('dve', lambda e: e.tensor_scalar(out=accd, in0=UB[b], scalar1=COEFp[:, sl:sl + 1], scalar2=None, op0=ALU.mult),
                      reads=[('UB', b), ('COEF', p)], writes=[('x1', 1)])
            else:
                tk.op('dve', lambda e: e.scalar_tensor_tensor(out=accd, in0=UB[b], scalar=COEFp[:, sl:sl + 1], in1=accd,
                                                              op0=ALU.mult, op1=ALU.add),
                      reads=[('UB', b), ('COEF', p), ('x1', 1)], writes=[('x1', 1)])
            return
        dj = sl % 4
        tk.op('act', lambda e: e.activation(out=DG[dj], in_=ident, func=AF.Copy, scale=COEFp[:, sl:sl + 1]),
              reads=['ident', ('COEF', p)], writes=[('DG', dj)])
        for nb in range(4):
            tk.op('pe', lambda e, nb=nb: e.matmul(PSC[:, nb * 512:(nb + 1) * 512], lhsT=DG[dj],
                                                  rhs=UB[b][:, nb * 512:(nb + 1) * 512], start=(sl == 0), stop=(sl == 126)),
                  reads=[('DG', dj), ('UB', b)], writes=['psc'], inc=(nb == 3))

    def v_finish(T):
        x1f = x1[1]
        tk.op('dve', lambda e: e.tensor_tensor(out=acc, in0=PSC, in1=x1f, op=ALU.add), reads=['psc', ('x1', 1)], writes=['acc'])
        tk.dma('sp', lambda e: e.dma_start(out=x1f, in_=X1S[T * 128:(T + 1) * 128, :]), 'x1l1', reads=[('X1S', T)], writes=[('x1', 1)])
        tk.op('dve', lambda e: e.tensor_tensor(out=acc, in0=acc, in1=g2b, op=ALU.mult), reads=['acc', 'g2b'], writes=['acc'])
        tk.op('dve', lambda e: e.scalar_tensor_tensor(out=acc, in0=x1f, scalar=ALPHA, in1=acc, op0=ALU.mult, op1=ALU.add),
              reads=[('x1', 1), 'acc'], writes=['acc'])
        layer_norm(tk, acc, 'acc', sm, l2g, 'l2g', l2b, 'l2b', PSC, ['psc'])
        tk.dma('sp', lambda e: e.dma_start(out=out[T * 128:(T + 1) * 128, :], in_=acc), 'outst',
               reads=['acc'], writes=[('out', T)])

    for i in range(NT + 2):
        genA = stage_A(i) if i < NT else iter(())
        tu = i - 1 if 0 <= i - 1 < NT else None
        tv = i - 2 if 0 <= i - 2 < NT else None
        if tu is None and tv is None:
            for _ in genA:
                pass
            continue
        for sl in range(128):
            if tv is not None:
                v_slot(tv, sl)
            if tu is not None:
                u_slot(tu, sl)
            if sl % 2 == 1 or (sl % 10 == 0 and sl > 0):
                next(genA, None)
        for _ in genA:
            pass
        if tu is not None:
            u_finish(tu)
        if tv is not None:
            v_finish(tv)


_CACHE = {}


def make_inputs(inputs, core):
    b, half = core // 2, core % 2
    flip = half == 1
    f = np.float32
    x = np.asarray(inputs['x'])[b]
    if flip:
        x = x[::-1]
    xp = np.zeros((4098, 2048), f)
    xp[1:4097] = x
    w_in = np.ascontiguousarray(np.asarray(inputs['w_in'])[0], dtype=f)
    wd = w_in[:, ODT:ODT + 64]
    cw = np.asarray(inputs['conv_ssd_w'])[0]
    scw = np.asarray(inputs['short_conv_w'])[0]
    dbf, dbb = np.asarray(inputs['dt_bias_f'])[0], np.asarray(inputs['dt_bias_b'])[0]
    alf, alb = np.asarray(inputs['a_log_f'])[0], np.asarray(inputs['a_log_b'])[0]
    if flip:
        wd = np.concatenate([wd[:, 32:64], wd[:, 0:32]], axis=1)
        cw = cw[::-1]
        scw = scw[::-1]
        dtb = np.concatenate([dbb, dbf])
        alog = np.concatenate([alb, alf])
    else:
        dtb = np.concatenate([dbf, dbb])
        alog = np.concatenate([alf, alb])

    def bc(v):
        v = np.asarray(v, dtype=f).reshape(1, -1)
        return np.ascontiguousarray(np.broadcast_to(v, (128, v.shape[1])))

    def fm(v):
        return np.ascontiguousarray(np.asarray(v, dtype=f).reshape(16, 128).T)

    sk = np.asarray(inputs['sub_keys'])[0]
    kT = sk.reshape(8, 2, 128, 2, 128).transpose(4, 0, 1, 3, 2).reshape(128, 4096)
    return {
        'xp': xp,
        'cT': fm(np.asarray(inputs['c'])[b]),
        'w_ada': np.ascontiguousarray(np.asarray(inputs['w_ada'])[0], dtype=f),
        'b_ada': np.ascontiguousarray(np.asarray(inputs['b_ada'])[0:1], dtype=f),
        'w_in': w_in,
        'w_dt': np.ascontiguousarray(wd, dtype=f),
        'cwb': bc(np.ascontiguousarray(cw).reshape(-1)),
        'scwb': bc(np.ascontiguousarray(scw).reshape(-1)),
        'cbias': np.ascontiguousarray(np.asarray(inputs['conv_ssd_b'])[0:1], dtype=f),
        'dtb_b': bc(dtb),
        'alog_b': bc(alog),
        'dsk_b': bc(np.asarray(inputs['d_skip'])[0]),
        'ssdgT': fm(np.asarray(inputs['ssd_norm_g'])[0]),
        'scgT': fm(np.asarray(inputs['sc_norm_g'])[0]),
        'w_out': np.ascontiguousarray(np.asarray(inputs['w_out'])[0], dtype=f),
        'ln1g_b': bc(np.asarray(inputs['ln1_g'])[0]),
        'ln1b_b': bc(np.asarray(inputs['ln1_b'])[0]),
        'w_query': np.ascontiguousarray(np.asarray(inputs['w_query'])[0], dtype=f),
        'kT': np.ascontiguousarray(kT, dtype=f),
        'eu': np.ascontiguousarray(np.asarray(inputs['expert_u'])[0], dtype=f),
        'ev': np.ascontiguousarray(np.asarray(inputs['expert_v'])[0], dtype=f),
        'ln2g_b': bc(np.asarray(inputs['ln2_g'])[0]),
        'ln2b_b': bc(np.asarray(inputs['ln2_b'])[0]),
    }


def kernel(_stage=99, _cores=8, **inputs):
    if _stage not in _CACHE:
        _CACHE[_stage] = build(_stage)
    nc = _CACHE[_stage]
    in_maps = [make_inputs(inputs, c) for c in range(_cores)]
    res = run_bass_kernel_spmd(nc, in_maps, core_ids=list(range(_cores)))
    outp = np.zeros((4, 4096, 2048), np.float32)
    for c in range(_cores):
        b, half = c // 2, c % 2
        o = np.asarray(res.results[c]['out'])
        if half == 0:
            outp[b, 0:2048] = o
        else:
            outp[b, 2048:4096] = o[::-1]
    return outp
```
